# Optimizing a Trainium2 kernel written in Bass

```python
import jax, jax.numpy as jnp
from jax import lax
import numpy as np

D_MODEL = 1024
BATCH = 4
SEQ = 8192
DEPTH = 2

GRID_W = 64
CTX_LEN = 256
MIX_W = D_MODEL
LRU_W = MIX_W // 2
LRU_HEADS = 8
LRU_HD = LRU_W // LRU_HEADS
CONV_W = MIX_W // 4
FFT_W = MIX_W - LRU_W - CONV_W
FFT_GROUPS = 4
FFT_GD = FFT_W // FFT_GROUPS
LRU_CONV_K = 4
LRU_CONV_LEFT = 1
SHORT_CONV_K = 3
SHORT_CONV_LEFT = 1
LRU_C = 8.0
IN_COLS = 2 * LRU_W + 3 * CONV_W + FFT_W
N_EXPERTS = 16
EXPERT_FF = D_MODEL
CAPACITY_FACTOR = 2
EPS = 1e-6

kernel_name = 'hybrid_lru_conv_fourier_ec_moe_dit'


def rmsnorm(x, g):
    xf = x.astype(jnp.float32)
    y = xf * lax.rsqrt(jnp.mean(xf * xf, axis=-1, keepdims=True) + EPS)
    return (y * g.astype(jnp.float32)).astype(x.dtype)


def modulate(h, shift, scale):
    return h * (1 + scale) + shift


def shift_conv(u, w, left, axis):
    k_w = w.shape[0]
    n = u.shape[axis]
    pad = [(0, 0)] * u.ndim
    pad[axis] = (left, k_w - 1 - left)
    up = jnp.pad(u, pad)
    out = w[0] * lax.slice_in_dim(up, 0, n, axis=axis)
    for k in range(1, k_w):
        out = out + w[k] * lax.slice_in_dim(up, k, k + n, axis=axis)
    return out


def conv_seq(u, w, left):
    return shift_conv(u, w, left, axis=1)


def conv_grid(u, w, left):
    b, n, ch = u.shape
    rows = n // GRID_W
    return shift_conv(u.reshape(b, rows, GRID_W, ch), w, left, axis=2).reshape(b, n, ch)


def rglru_coeffs(u, w_r, b_r, w_i, b_i, lam):
    b, n, _ = u.shape
    uh = u.astype(jnp.float32).reshape(b, n, LRU_HEADS, LRU_HD)
    r = jax.nn.sigmoid(jnp.einsum('blhi,hij->blhj', uh, w_r.astype(jnp.float32)).reshape(b, n, LRU_W) + b_r.astype(jnp.float32))
    i = jax.nn.sigmoid(jnp.einsum('blhi,hij->blhj', uh, w_i.astype(jnp.float32)).reshape(b, n, LRU_W) + b_i.astype(jnp.float32))
    log_a = -LRU_C * r * jax.nn.softplus(-lam.astype(jnp.float32))
    a = jnp.exp(log_a)
    inp = jnp.sqrt(-jnp.expm1(2.0 * log_a)) * (i * uh.reshape(b, n, LRU_W))
    return a, inp


def _combine(left, right):
    a1, b1 = left
    a2, b2 = right
    return a1 * a2, a2 * b1 + b2


def linear_scan(a, b, h0):
    b = b.at[:, 0].add(a[:, 0] * h0)
    _, h = lax.associative_scan(_combine, (a, b), axis=1)
    return h


def lru_states(u, w_r, b_r, w_i, b_i, lam, h0_f, h0_b):
    a, b = rglru_coeffs(u, w_r[0], b_r[0], w_i[0], b_i[0], lam[0])
    h_f = linear_scan(a, b, h0_f)
    a, b = rglru_coeffs(u, w_r[1], b_r[1], w_i[1], b_i[1], lam[1])
    h_b = jnp.flip(linear_scan(jnp.flip(a, 1), jnp.flip(b, 1), h0_b), 1)
    return h_f, h_b


def fourier_mix(u):
    b, n, w = u.shape
    uf = u.astype(jnp.float32).reshape(b, n, FFT_GROUPS, FFT_GD)
    y = jnp.real(jnp.fft.fftn(uf, axes=(1, 3), norm='ortho'))
    return y.reshape(b, n, w).astype(u.dtype)


def mixer_out(p, h_f, h_b, conv_fn, sc_w, g_out, w_out):
    o = 2 * LRU_W
    lru_g = p[..., LRU_W:o]
    sc_b = p[..., o:o + CONV_W]
    sc_c = p[..., o + CONV_W:o + 2 * CONV_W]
    sc_x = p[..., o + 2 * CONV_W:o + 3 * CONV_W]
    fft_x = p[..., o + 3 * CONV_W:]
    y_lru = ((h_f + h_b) * jax.nn.gelu(lru_g.astype(jnp.float32))).astype(p.dtype)
    y_sc = sc_b * conv_fn(sc_c * sc_x, sc_w, SHORT_CONV_LEFT)
    y_fft = fourier_mix(fft_x)
    y = jnp.concatenate([
        rmsnorm(y_lru, g_out[:LRU_W]),
        rmsnorm(y_sc, g_out[LRU_W:LRU_W + CONV_W]),
        rmsnorm(y_fft, g_out[LRU_W + CONV_W:]),
    ], axis=-1)
    return y @ w_out


def expert_choice_ffn(h, w_router, w_gate, w_up, w_down):
    b, n, d = h.shape
    cap = CAPACITY_FACTOR * n // N_EXPERTS
    logits = jnp.einsum('bld,de->ble', h, w_router).astype(jnp.float32)
    aff = jax.nn.softmax(logits, axis=-1)
    g, idx = lax.top_k(jnp.swapaxes(aff, 1, 2), cap)
    xs = jax.vmap(lambda hb, ib: hb[ib])(h, idx)
    hid = jax.nn.silu(jnp.einsum('becd,edf->becf', xs, w_gate)) * jnp.einsum('becd,edf->becf', xs, w_up)
    ys = jnp.einsum('becf,efd->becd', hid, w_down) * g[..., None].astype(hid.dtype)
    return jax.vmap(lambda ib, yb: jnp.zeros((n, d), yb.dtype).at[ib.reshape(-1)].add(yb.reshape(-1, d)))(idx, ys)


def setup_inputs(seed: int = 0) -> dict:
    key = jax.random.key(seed)
    ks = jax.random.split(key, 26)
    f32 = jnp.float32
    nrm = lambda k, shape, s: jax.random.normal(k, shape, f32) * s
    u = jax.random.uniform(ks[14], (DEPTH, 2, LRU_W), f32, 0.9, 0.999)
    a0 = u ** (1.0 / LRU_C)
    lam = jnp.log(a0) - jnp.log1p(-a0)
    return {
        'x': nrm(ks[0], (BATCH, SEQ, D_MODEL), 1.0),
        'c': nrm(ks[1], (BATCH, D_MODEL), 1.0),
        'ctx': nrm(ks[2], (BATCH, CTX_LEN, D_MODEL), 1.0),
        'c_ctx': nrm(ks[3], (D_MODEL,), 1.0),
        'w_ada': nrm(ks[4], (DEPTH, D_MODEL, 6 * D_MODEL), 0.5 * D_MODEL ** -0.5),
        'b_ada': nrm(ks[5], (DEPTH, 6 * D_MODEL), 0.02),
        'g_norm1': 1.0 + nrm(ks[6], (DEPTH, D_MODEL), 0.05),
        'w_in': nrm(ks[7], (DEPTH, D_MODEL, IN_COLS), D_MODEL ** -0.5),
        'lru_conv_w': nrm(ks[8], (DEPTH, LRU_CONV_K, LRU_W), LRU_CONV_K ** -0.5),
        'lru_conv_b': nrm(ks[9], (DEPTH, LRU_W), 0.02),
        'lru_wr': nrm(ks[10], (DEPTH, 2, LRU_HEADS, LRU_HD, LRU_HD), LRU_HD ** -0.5),
        'lru_br': nrm(ks[11], (DEPTH, 2, LRU_W), 0.02),
        'lru_wi': nrm(ks[12], (DEPTH, 2, LRU_HEADS, LRU_HD, LRU_HD), LRU_HD ** -0.5),
        'lru_bi': nrm(ks[13], (DEPTH, 2, LRU_W), 0.02),
        'lru_lam': lam,
        'sc_conv_w': nrm(ks[15], (DEPTH, SHORT_CONV_K, CONV_W), SHORT_CONV_K ** -0.5),
        'g_out': 1.0 + nrm(ks[16], (DEPTH, MIX_W), 0.05),
        'w_out': nrm(ks[17], (DEPTH, MIX_W, D_MODEL), MIX_W ** -0.5),
        'g_norm2': 1.0 + nrm(ks[18], (DEPTH, D_MODEL), 0.05),
        'w_router': nrm(ks[19], (DEPTH, D_MODEL, N_EXPERTS), D_MODEL ** -0.5),
        'w_gate_e': nrm(ks[20], (DEPTH, N_EXPERTS, D_MODEL, EXPERT_FF), D_MODEL ** -0.5),
        'w_up_e': nrm(ks[21], (DEPTH, N_EXPERTS, D_MODEL, EXPERT_FF), D_MODEL ** -0.5),
        'w_down_e': nrm(ks[22], (DEPTH, N_EXPERTS, EXPERT_FF, D_MODEL), EXPERT_FF ** -0.5),
        'g_final': 1.0 + nrm(ks[23], (D_MODEL,), 0.05),
    }


def reference(x, c, ctx, c_ctx, w_ada, b_ada, g_norm1, w_in, lru_conv_w, lru_conv_b, lru_wr, lru_br,
              lru_wi, lru_bi, lru_lam, sc_conv_w, g_out, w_out, g_norm2, w_router, w_gate_e, w_up_e,
              w_down_e, g_final):
    b = x.shape[0]
    for l in range(DEPTH):
        last = l == DEPTH - 1
        mx = jnp.split((jax.nn.silu(c) @ w_ada[l] + b_ada[l])[:, None, :], 6, axis=-1)
        mc = jnp.split(jax.nn.silu(c_ctx) @ w_ada[l] + b_ada[l], 6, axis=-1)
        lru_p = (lru_wr[l], lru_br[l], lru_wi[l], lru_bi[l], lru_lam[l])

        hc = modulate(rmsnorm(ctx, g_norm1[l]), mc[0], mc[1])
        pc = hc @ (w_in[l][:, :LRU_W] if last else w_in[l])
        uc = conv_seq(pc[..., :LRU_W], lru_conv_w[l], LRU_CONV_LEFT) + lru_conv_b[l]
        h0 = jnp.zeros((b, LRU_W), jnp.float32)
        hf_c, hb_c = lru_states(uc, *lru_p, h0, h0)
        state_f = hf_c[:, -1]
        state_b = hb_c[:, 0]

        hx = modulate(rmsnorm(x, g_norm1[l]), mx[0], mx[1])
        px = hx @ w_in[l]
        ux = conv_grid(px[..., :LRU_W], lru_conv_w[l], LRU_CONV_LEFT) + lru_conv_b[l]
        hf_x, hb_x = lru_states(ux, *lru_p, state_f, state_b)
        x = x + mx[2] * mixer_out(px, hf_x, hb_x, conv_grid, sc_conv_w[l], g_out[l], w_out[l])

        if not last:
            ctx = ctx + mc[2] * mixer_out(pc, hf_c, hb_c, conv_seq, sc_conv_w[l], g_out[l], w_out[l])
            hc2 = modulate(rmsnorm(ctx, g_norm2[l]), mc[3], mc[4])
            ctx = ctx + mc[5] * expert_choice_ffn(hc2, w_router[l], w_gate_e[l], w_up_e[l], w_down_e[l])

        hx2 = modulate(rmsnorm(x, g_norm2[l]), mx[3], mx[4])
        x = x + mx[5] * expert_choice_ffn(hx2, w_router[l], w_gate_e[l], w_up_e[l], w_down_e[l])
    return rmsnorm(x, g_final)
```

```python
import numpy as np
import ml_dtypes
from contextlib import ExitStack
import concourse.bass as bass
import concourse.mybir as mybir
from concourse.bass_utils import run_bass_kernel_spmd

F32 = mybir.dt.float32
BF16 = mybir.dt.bfloat16
I32 = mybir.dt.int32
ALU = mybir.AluOpType
AF = mybir.ActivationFunctionType
AX = mybir.AxisListType

ENGS = ['pe', 'act', 'dve', 'pool', 'sp']
NDMASEM = 8
SAME_ENGINE_SYNC = True
SCHEDULE = True
TABLE_AWARE = True
ASET = {AF.Sigmoid: 'sig', AF.Silu: 'silu', AF.Exp: 'exp', AF.Ln: 'exp', AF.Sqrt: 'sqrt', AF.Gelu_apprx_tanh: 'gelu'}
PE_GHZ = 1.9
SCHED_WINDOW = 48
SCHED_MODE = 'fifo'
INTERLEAVE = True
LT_POOLS, LT_DEPTH, LT_BY_BLOCK = 2, 7, False
YL_BUFS = 1
MOE_STG = 2
STORE_ENG = 'pool'
PS_A, PS_T, PS_S = 5, 1, 2
EPS = 1e-6
T_X = 8192
T_C = 256
NTOK = T_X + T_C


class Prog:
    def __init__(self, nc):
        self.nc = nc
        self.pending = []
        self.eng_seq = {e: 0 for e in ENGS}
        self.dma_cnt = {e: 0 for e in ENGS}
        self.dma_val = {}
        self.known = {e: {} for e in ENGS}
        self.stack = ExitStack()
        self.sems = {}
        for e in ENGS:
            k = 'eng:' + e
            self.sems[k] = self.stack.enter_context(nc.semaphore(k.replace(':', '_')))
            for j in range(NDMASEM):
                k = 'dma:%s:%d' % (e, j)
                self.sems[k] = self.stack.enter_context(nc.semaphore(k.replace(':', '_')))
        self.uid = 0
        self.sim_time = 0.0

    def sb(self, name, shape, dt, stack=None):
        self.uid += 1
        return (stack or self.stack).enter_context(self.nc.sbuf_tensor("%s_%d" % (name, self.uid), list(shape), dt))

    def ps(self, name, shape, dt=F32):
        return self.stack.enter_context(self.nc.psum_tensor(name, list(shape), dt))

    @staticmethod
    def _tok(t):
        if isinstance(t, (str, int)):
            return t
        return t.tensor.name

    def op(self, eng, fn, reads=(), writes=(), cost=0.1, aset=None):
        r = [self._tok(t) for t in reads if t is not None and not isinstance(t, (int, float))]
        w = [self._tok(t) for t in writes]
        self.pending.append((eng, False, fn, r, w, cost, aset))

    def dma(self, eng, fn, reads=(), writes=(), cost=3.0):
        r = [self._tok(t) for t in reads]
        w = [self._tok(t) for t in writes]
        self.pending.append((eng, True, fn, r, w, cost, None))

    def barrier(self):
        pass

    def mark(self):
        return len(self.pending)

    def interleave(self, i0, i1, i2):
        a, b = self.pending[i0:i1], self.pending[i1:i2]
        if not a or not b:
            return
        out = []
        ia = ib = 0
        while ia < len(a) or ib < len(b):
            if ib >= len(b) or (ia < len(a) and ia * len(b) <= ib * len(a)):
                out.append(a[ia])
                ia += 1
            else:
                out.append(b[ib])
                ib += 1
        self.pending[i0:i2] = out

    def _schedule(self, ops):
        n = len(ops)
        last_w = {}
        readers = {}
        deps = []
        for i, (eng, isdma, fn, r, w, cost, aset_) in enumerate(ops):
            d = set()
            for t in r:
                if t in last_w:
                    d.add(last_w[t])
            for t in w:
                if t in last_w:
                    d.add(last_w[t])
                rl = readers.get(t)
                if rl:
                    d.update(rl)
            d.discard(i)
            deps.append(d)
            for t in r:
                readers.setdefault(t, []).append(i)
            for t in w:
                last_w[t] = i
                readers[t] = []
        succ = [[] for _ in range(n)]
        ndep = [len(d) for d in deps]
        for i, d in enumerate(deps):
            for j in d:
                succ[j].append(i)
        ready = [0.0] * n
        blev = [0.0] * n
        for i in range(n - 1, -1, -1):
            m = 0.0
            for s_ in succ[i]:
                if blev[s_] > m:
                    m = blev[s_]
            blev[i] = m + ops[i][5] + 0.3
        queues = {e: [] for e in ENGS}
        for i, o in enumerate(ops):
            queues[o[0]].append(i)
        free = {e: 0.0 for e in ENGS}
        dma_fin = {e: [] for e in ENGS}
        order = {e: [] for e in ENGS}
        remaining = n
        W = SCHED_WINDOW
        cur_set = [None]
        while remaining:
            best = None
            for e in ENGS:
                q = queues[e]
                if not q:
                    continue
                fe = free[e]
                lim = min(W, len(q))
                first = None
                cand = None
                if e == 'act' and TABLE_AWARE:
                    for p in range(lim):
                        i = q[p]
                        if ndep[i] or ready[i] > fe:
                            continue
                        a_ = ops[i][6]
                        if a_ is None or a_ == cur_set[0]:
                            cand = (fe, i, e, p)
                            break
                    if cand is not None:
                        if best is None or cand[0] < best[0] or (cand[0] == best[0] and cand[1] < best[1]):
                            best = cand
                        continue
                for p in range(lim):
                    i = q[p]
                    if ndep[i]:
                        continue
                    st = ready[i] if ready[i] > fe else fe
                    if ops[i][1]:
                        k = len(dma_fin[e])
                        if k >= NDMASEM and dma_fin[e][k - NDMASEM] > st:
                            st = dma_fin[e][k - NDMASEM]
                    if SCHED_MODE == 'blev':
                        key = (max(st, fe), -blev[i])
                        if cand is None or key < ckey:
                            cand = (st, i, e, p)
                            ckey = key
                        first = cand
                        continue
                    if first is None:
                        first = (st, i, e, p)
                        cand = first
                        if st <= fe:
                            break
                    else:
                        c = ops[i][5] if not ops[i][1] else 0.05
                        if st + c <= first[0] and st < cand[0]:
                            cand = (st, i, e, p)
                            if st <= fe:
                                break
                if cand is not None and (best is None or cand[0] < best[0] or (cand[0] == best[0] and cand[1] < best[1])):
                    best = cand
            st, i, e, p = best
            queues[e].pop(p)
            o = ops[i]
            f = st + o[5]
            if e == 'act' and o[6] is not None:
                if o[6] != cur_set[0]:
                    f += 1.28
                cur_set[0] = o[6]
            if o[1]:
                free[e] = st + 0.05
                dma_fin[e].append(f)
            else:
                free[e] = f
            for s_ in succ[i]:
                ndep[s_] -= 1
                lat = 0.0 if (ops[s_][0] == e and not o[1]) else 0.3
                if f + lat > ready[s_]:
                    ready[s_] = f + lat
            order[e].append(i)
            remaining -= 1
        tend = max(list(free.values()) + [f_ for v in dma_fin.values() for f_ in v] + [0.0])
        return deps, order, tend

    def emit(self):
        ops = self.pending
        self.pending = []
        nc = self.nc
        sems = self.sems
        if ops:
            if SCHEDULE:
                deps, order, tend = self._schedule(ops)
            else:
                deps, order, tend = self._schedule_inorder(ops)
            self.sim_time += tend
        else:
            deps, order = [], {e: [] for e in ENGS}
        ev = [None] * len(ops)
        for e in ENGS:
            for i in order[e]:
                if ops[i][1]:
                    k = self.dma_cnt[e]
                    self.dma_cnt[e] += 1
                    key = 'dma:%s:%d' % (e, k % NDMASEM)
                    prev = self.dma_val.get(key, 0)
                    self.dma_val[key] = prev + 16
                    ev[i] = (key, prev + 16, prev)
                else:
                    self.eng_seq[e] += 1
                    ev[i] = ('eng:' + e, self.eng_seq[e], 0)
        prog = {e: [] for e in ENGS}
        for e in ENGS:
            kn = self.known[e]
            for i in order[e]:
                need = {}
                for d in deps[i]:
                    k, v, _ = ev[d]
                    if k == 'eng:' + e and (e == 'pe' or not SAME_ENGINE_SYNC):
                        continue
                    if need.get(k, 0) < v:
                        need[k] = v
                k, v, prev = ev[i]
                if ops[i][1] and prev > 0 and need.get(k, 0) < prev:
                    need[k] = prev
                waits = []
                for k2, v2 in need.items():
                    if kn.get(k2, 0) >= v2:
                        continue
                    kn[k2] = v2
                    waits.append((k2, v2))
                prog[e].append((waits, ops[i][2], k, 16 if ops[i][1] else 1))
        allv = {}
        for e in ENGS:
            if self.eng_seq[e]:
                allv['eng:' + e] = self.eng_seq[e]
        for k, v in self.dma_val.items():
            allv[k] = v
        for e in ENGS:
            kn = self.known[e]
            waits = []
            for k, v in allv.items():
                if k == 'eng:' + e or kn.get(k, 0) >= v:
                    continue
                kn[k] = v
                waits.append((k, v))
            kn['eng:' + e] = self.eng_seq[e]
            prog[e].append((waits, None, None, 0))

        def run(h, lst):
            for waits, fn, k, amt in lst:
                for (wk, wv) in waits:
                    h.wait_ge(sems[wk], wv)
                if fn is not None:
                    fn(h).then_inc(sems[k], amt)
        with nc.Block() as block:
            @block.tensor
            def _(e):
                run(e, prog['pe'])

            @block.scalar
            def _(e):
                run(e, prog['act'])

            @block.vector
            def _(e):
                run(e, prog['dve'])

            @block.gpsimd
            def _(e):
                run(e, prog['pool'])

            @block.sync
            def _(e):
                run(e, prog['sp'])

    def _schedule_inorder(self, ops):
        n = len(ops)
        W_save = None
        global SCHED_WINDOW
        W_save, SCHED_WINDOW = SCHED_WINDOW, 1
        try:
            return self._schedule(ops)
        finally:
            SCHED_WINDOW = W_save

    def mm(self, out, lhsT, rhs, start=True, stop=True, rt=None):
        c = max(64, rhs.free_size()) / PE_GHZ / 1000.0 * (4.0 if rhs.dtype == F32 else 1.0) + 0.02
        self.op('pe', lambda e: e.matmul(out, lhsT=lhsT, rhs=rhs, start=start, stop=stop), [lhsT, rhs] + list(rt or []), [out], c)

    def tr(self, out, in_, ident):
        self.op('pe', lambda e: e.transpose(out, in_, ident), [in_, ident], [out], 0.11)

    def act(self, out, in_, func, bias=None, scale=None, accum=None, wt=None):
        kw = {}
        if bias is not None:
            kw['bias'] = bias
        if scale is not None:
            kw['scale'] = scale
        if accum is not None:
            kw['accum_out'] = accum
        r = [in_] + [a for a in (bias, scale) if a is not None and not isinstance(a, (int, float))]
        w = (list(wt) if wt else [out]) + ([accum] if accum is not None else [])
        c = 0.22 + in_.free_size() / 1400.0
        self.op('act', lambda e: e.activation(out=out, in_=in_, func=func, **kw), r, w, c, ASET.get(func))

    def _vc(self, eng, n, f=1.0):
        f = 1.0 + (f - 1.0) * 0.3
        return (0.07 + f * n / 960.0) if eng == 'dve' else (0.3 + n / 300.0)

    def ts(self, eng, out, in0, s1, s2=None, op0=ALU.mult, op1=None, accum=None, wt=None):
        kw = {}
        if op1 is not None:
            kw['op1'] = op1
        if accum is not None:
            kw['accum_out'] = accum
        r = [in0] + [a for a in (s1, s2) if a is not None and not isinstance(a, (int, float))]
        w = (list(wt) if wt else [out]) + ([accum] if accum is not None else [])
        self.op(eng, lambda e: e.tensor_scalar(out=out, in0=in0, scalar1=s1, scalar2=s2, op0=op0, **kw), r, w,
                self._vc(eng, in0.free_size()))

    def tt(self, eng, out, in0, in1, op):
        self.op(eng, lambda e: e.tensor_tensor(out=out, in0=in0, in1=in1, op=op), [in0, in1], [out],
                self._vc(eng, in0.free_size(), 1.5))

    def stt(self, out, in0, scalar, in1, op0, op1):
        r = [in0, in1] + ([scalar] if not isinstance(scalar, (int, float)) else [])
        self.op('dve', lambda e: e.scalar_tensor_tensor(out=out, in0=in0, scalar=scalar, in1=in1, op0=op0, op1=op1), r, [out],
                self._vc('dve', in0.free_size(), 1.5))

    def cp(self, eng, out, in_):
        if eng == 'act':
            self.act(out, in_, AF.Copy)
        else:
            self.op(eng, lambda e: e.tensor_copy(out=out, in_=in_), [in_], [out], self._vc(eng, in_.free_size()))

    def ms(self, eng, ap, val):
        self.op(eng, lambda e: e.memset(ap, val), [], [ap], self._vc(eng, ap.free_size()))

    def scan(self, out, d0, d1, init):
        r = [d0, d1] + ([init] if not isinstance(init, (int, float)) else [])
        self.op('dve', lambda e: e.tensor_tensor_scan(out=out, data0=d0, data1=d1, initial=init, op0=ALU.mult, op1=ALU.add), r, [out],
                self._vc('dve', d0.free_size(), 2.0))

    def red(self, out, in_, op):
        self.op('dve', lambda e: e.tensor_reduce(out=out, in_=in_, axis=AX.X, op=op), [in_], [out], self._vc('dve', in_.free_size()))

    def recip(self, out, in_):
        self.op('dve', lambda e: e.reciprocal(out=out, in_=in_), [in_], [out], self._vc('dve', in_.free_size(), 8.0))

    def ld(self, out, in_, eng='sp', wt=None, rt=None, **kw):
        c = 2.0 + out.nbytes() / 150000.0
        self.dma(eng, lambda e: e.dma_start(out=out, in_=in_, **kw), [in_] + list(rt or []), list(wt) if wt else [out], c)

    def gather(self, out, table, idx):
        self.dma('pool', lambda e: e.indirect_dma_start(out=out, out_offset=None, in_=table,
                                                        in_offset=bass.IndirectOffsetOnAxis(ap=idx, axis=0)),
                 [table, idx], [out], 3.0 + out.nbytes() / 100000.0)

    def scatter_add(self, table, idx, in_, rt=None, wt=None):
        self.dma('pool', lambda e: e.indirect_dma_start(out=table, out_offset=bass.IndirectOffsetOnAxis(ap=idx, axis=0),
                                                        in_=in_, in_offset=None, compute_op=ALU.add),
                 [idx, in_] + (list(rt) if rt is not None else [table]), list(wt) if wt else [table], 4.0 + in_.nbytes() / 80000.0)


class MultiRot:
    def __init__(self, pools):
        self.pools = pools
        self.cur = 0

    def set(self, j):
        self.cur = j % len(self.pools)

    def next(self):
        return self.pools[self.cur].next()


class Rot:
    def __init__(self, items):
        self.items = items
        self.i = 0

    def next(self):
        t = self.items[self.i]
        self.i = (self.i + 1) % len(self.items)
        return t


def host_consts():
    bf = ml_dtypes.bfloat16
    k = {}
    k['k_ident_f'] = np.eye(128, dtype=np.float32)
    k['k_ident_b'] = np.eye(128).astype(bf)
    k['k_ones_b'] = np.ones((128, 128)).astype(bf)
    k['k_ones_f'] = np.ones((128, 128), np.float32)
    k['k_triu'] = np.triu(np.ones((128, 128), np.float32), 1)
    sel = np.zeros((2, 256), np.float32)
    sel[0, :128] = 1
    sel[1, 128:] = 1
    k['k_sel'] = sel
    k['k_iota_c'] = np.tile(np.arange(1024, dtype=np.float32)[None], (128, 1))
    k['k_cidx'] = (np.arange(8)[None, :] * 128 + np.arange(128)[:, None]).astype(np.float32)
    k['k_tstart'] = (np.arange(128) * 128).astype(np.float32)[:, None].copy()
    scale = 1.0 / np.sqrt(8192.0 * 64.0)
    cc = np.arange(64)
    ang = 2 * np.pi * np.outer(cc, cc) / 64.0
    bd = np.zeros((2, 128, 512), np.float64)
    for q in range(2):
        for gl in range(2):
            r0 = gl * 64
            c0 = q * 128 + gl * 64
            bd[q, r0:r0 + 64, c0:c0 + 64] = np.cos(ang) * scale
            bd[q, r0:r0 + 64, 256 + c0:256 + c0 + 64] = -np.sin(ang) * scale
    k['k_bd'] = np.ascontiguousarray(bd.transpose(1, 0, 2)).astype(bf)
    n1 = np.arange(128)
    a1 = 2 * np.pi * np.outer(n1, n1) / 128.0
    C1, S1 = np.cos(a1), np.sin(a1)
    cs = np.stack([np.concatenate([C1, -S1], 1), np.concatenate([S1, C1], 1)], 1)
    k['k_cs1'] = cs.astype(bf)
    n2 = np.arange(64)
    k1 = np.arange(128)
    k2 = np.arange(64)
    kk = k1[:, None] + 128 * k2[None, :]
    a3 = 2 * np.pi * n2[:, None, None] * kk[None] / 8192.0
    e3 = np.stack([np.cos(a3), np.sin(a3)], 2)
    k['k_e3'] = np.concatenate([e3, e3], 0).astype(bf)
    n = np.arange(256)
    ac = 2 * np.pi * np.outer(n, n) / 256.0
    f = np.sqrt(32.0)
    dc = np.stack([np.cos(ac) * f, np.sin(ac) * f], 1)
    k['k_dc'] = np.ascontiguousarray(dc.reshape(2, 128, 2, 256).transpose(1, 0, 2, 3)).astype(bf)
    return k


CONST_SHAPES = {
    'k_ident_f': ([128, 128], F32), 'k_ident_b': ([128, 128], BF16), 'k_ones_b': ([128, 128], BF16),
    'k_ones_f': ([128, 128], F32), 'k_triu': ([128, 128], F32), 'k_sel': ([2, 256], F32),
    'k_iota_c': ([128, 1024], F32), 'k_cidx': ([128, 8], F32), 'k_tstart': ([128, 1], F32),
    'k_bd': ([128, 2, 512], BF16), 'k_cs1': ([128, 2, 256], BF16), 'k_e3': ([128, 128, 2, 64], BF16),
    'k_dc': ([128, 2, 2, 256], BF16),
}

IN_SHAPES = {
    'x': [T_X, 1024], 'c': [1, 1024], 'ctx': [T_C, 1024], 'c_ctx': [1, 1024],
    'w_ada': [2, 1024, 6144], 'b_ada': [2, 6144], 'g_norm1': [2, 1024], 'w_in': [2, 1024, 2048],
    'lru_conv_w': [2, 4, 512], 'lru_conv_b': [2, 512], 'lru_wr': [2, 2, 8, 64, 64], 'lru_br': [2, 2, 512],
    'lru_wi': [2, 2, 8, 64, 64], 'lru_bi': [2, 2, 512], 'lru_lam': [2, 2, 512], 'sc_conv_w': [2, 3, 256],
    'g_out': [2, 1024], 'w_out': [2, 1024, 1024], 'g_norm2': [2, 1024], 'w_router': [2, 1024, 16],
    'w_gate_e': [2, 16, 1024, 1024], 'w_up_e': [2, 16, 1024, 1024], 'w_down_e': [2, 16, 1024, 1024],
    'g_final': [1, 1024],
}

C_G1, C_CW, C_CB, C_BR, C_BI, C_LAM, C_SCW, C_GOUT, C_NROWS = 0, 8, 24, 28, 36, 44, 52, 58, 66


def build(depth=2, stop_after=None, dbg=None):
    nc = bass.Bass("TRN2", target_bir_lowering=False)
    P = Prog(nc)
    D = {}
    for name, shp in IN_SHAPES.items():
        D[name] = nc.dram_tensor(name, shp, F32, kind="ExternalInput").ap()
    K = {}
    for name, (shp, dt) in CONST_SHAPES.items():
        K[name] = nc.dram_tensor(name, shp, dt, kind="ExternalInput").ap()
    out_d = nc.dram_tensor("out", [T_X, 1024], F32, kind="ExternalOutput").ap()
    xres = [nc.dram_tensor("xres%d" % i, [NTOK, 1024], F32, kind="Internal").ap() for i in range(2)]
    hx2ext = nc.dram_tensor("hx2ext", [NTOK, 1056], BF16, kind="Internal").ap()
    Vd = nc.dram_tensor("Vd", [2, NTOK, 256], BF16, kind="Internal").ap()
    hbd = nc.dram_tensor("hbd", [4, 128, NTOK], BF16, kind="Internal").ap()
    yfd = nc.dram_tensor("yfd", [2, 128, NTOK], BF16, kind="Internal").ap()
    affTd = nc.dram_tensor("affTd", [16, NTOK], F32, kind="Internal").ap()
    hxd = nc.dram_tensor("hxd", [8, 128, NTOK], BF16, kind="Internal").ap()
    dbg_out = {}
    if dbg:
        for name, shp in dbg.items():
            dbg_out[name] = nc.dram_tensor("dbg_" + name, shp, F32, kind="ExternalOutput").ap()

    ident_f = P.sb("ident_f", [128, 128], F32)
    ident_b = P.sb("ident_b", [128, 128], BF16)
    ones_b = P.sb("ones_b", [128, 128], BF16)
    ones_f = P.sb("ones_f", [128, 128], F32)
    triu = P.sb("triu", [128, 128], F32)
    sel = P.sb("sel", [2, 256], F32)
    cidx = P.sb("cidx", [128, 8], F32)
    tstart = P.sb("tstart", [128, 1], F32)
    bd = P.sb("bd", [128, 2, 512], BF16)
    cs1 = P.sb("cs1", [128, 2, 256], BF16)
    dc = P.sb("dc", [128, 2, 2, 256], BF16)
    for t, n in ((ident_f, 'k_ident_f'), (ident_b, 'k_ident_b'), (ones_b, 'k_ones_b'), (ones_f, 'k_ones_f'),
                 (triu, 'k_triu'), (sel, 'k_sel'), (cidx, 'k_cidx'), (tstart, 'k_tstart'), (bd, 'k_bd'),
                 (cs1, 'k_cs1'), (dc, 'k_dc')):
        P.ld(t[:], K[n])
    wr_b = P.sb("wr_b", [128, 8, 16], BF16)
    gates_b = P.sb("gates_b", [128, 2, 2, 4, 128], BF16)
    bcn = ['sh2', 'gs2', 'gate1', 'gate2']
    BC = {}
    bcd = nc.dram_tensor("bcd", [8, 128, 1024], F32, kind="Internal").ap()

    def bc_load(n_, v_, st_, rows=128):
        t_ = P.sb("bc_%s%d" % (n_, v_), [128, 1024], F32, st_)
        P.ld(t_[0:rows, :], bcd[bcn.index(n_) * 2 + v_, 0:rows, :])
        BC[(n_, v_)] = t_
    CL = P.sb("CL", [128, C_NROWS], F32)
    nsp = P.sb("nsp", [128, 8], F32)
    gs1 = P.sb("gs1", [128, 8, 2], F32)
    sh1 = P.sb("sh1", [128, 8, 2], F32)
    st_f = P.sb("st_f", [128, 4], F32)
    st_b = P.sb("st_b", [128, 4], F32)
    idx_all = P.sb("idx_all", [128, 16, 9], I32)
    val_all = P.sb("val_all", [128, 16, 9], F32)
    psA_banks = [P.ps("psA%d" % i, [128, 512], F32) for i in range(PS_A)]
    psA = Rot(psA_banks)
    psA_all, psA_lo, psA_hi = psA, Rot(psA_banks[0:PS_A - 2]), Rot(psA_banks[PS_A - 2:PS_A])
    psT = Rot([P.ps("psT%d" % i, [128, 1024], BF16) for i in range(PS_T)])
    psS = Rot([P.ps("psS%d" % i, [128, 512], F32) for i in range(PS_S)])

    cast_rr = Rot(['act', 'dve'])

    colpool = {}

    def col(st):
        if not hasattr(st, '_colpool'):
            st._colpool = Rot([P.sb("col%d" % i, [128, 1], F32, st) for i in range(32)])
        return st._colpool.next()

    def rsqrt_col(out, ss, n, st):
        t = col(st)
        P.ts('dve', t[:], ss, 1.0 / n, EPS, op0=ALU.mult, op1=ALU.add)
        P.act(t[:], t[:], AF.Ln)
        P.act(out, t[:], AF.Exp, scale=-0.5)

    def dump(name, ap_src, rows=None):
        if name in dbg_out:
            P.ld(dbg_out[name] if rows is None else dbg_out[name][rows], ap_src)

    for l in range(depth):
        last = (l == depth - 1)
        xin = D['x'] if l == 0 else xres[(l - 1) % 2][0:T_X, :]
        cin = D['ctx'] if l == 0 else xres[(l - 1) % 2][T_X:NTOK, :]
        xo = xres[l % 2]
        with ExitStack() as st:
            stg = Rot([P.sb("stg%d" % i, [128, 4096], F32, st) for i in range(2)])
            rows = P.sb("rows", [C_NROWS, 128], F32, st)
            P.ld(rows[C_G1:C_G1 + 8, :], D['g_norm1'][l].rearrange("(j p) -> j p", p=128))
            P.ld(rows[C_CW:C_CW + 16, :], D['lru_conv_w'][l].rearrange("k (j p) -> (k j) p", p=128))
            P.ld(rows[C_CB:C_CB + 4, :], D['lru_conv_b'][l].rearrange("(j p) -> j p", p=128))
            P.ld(rows[C_BR:C_BR + 8, :], D['lru_br'][l].rearrange("d (j p) -> (d j) p", p=128))
            P.ld(rows[C_BI:C_BI + 8, :], D['lru_bi'][l].rearrange("d (j p) -> (d j) p", p=128))
            P.ld(rows[C_LAM:C_LAM + 8, :], D['lru_lam'][l].rearrange("d (j p) -> (d j) p", p=128))
            P.ld(rows[C_SCW:C_SCW + 6, :], D['sc_conv_w'][l].rearrange("k (j p) -> (k j) p", p=128))
            P.ld(rows[C_GOUT:C_GOUT + 8, :], D['g_out'][l].rearrange("(j p) -> j p", p=128))
            pss = psS.next()
            P.tr(pss[:, 0:C_NROWS], rows[:, :], ident_f[0:C_NROWS, 0:C_NROWS])
            P.cp('dve', CL[:, :], pss[:, 0:C_NROWS])
            tmp8 = P.sb("tmp8", [128, 8], F32, st)
            P.act(tmp8[:], CL[:, C_LAM:C_LAM + 8], AF.Exp, scale=-1.0)
            P.act(tmp8[:], tmp8[:], AF.Ln, bias=1.0)
            P.ts('dve', nsp[:], tmp8[:], -8.0)
            gst = P.sb("gst", [128, 2, 2, 4, 128], F32, st)
            P.ms('pool', gst[:], 0.0)
            for d in range(2):
                for kind, nm_ in ((0, 'lru_wr'), (1, 'lru_wi')):
                    src = D[nm_][l, d].rearrange("(j two) i o -> two i j o", two=2)
                    P.ld(gst[0:64, d, kind, :, 0:64], src[0])
                    P.ld(gst[64:128, d, kind, :, 64:128], src[1])
            P.cp('pool', gates_b[:], gst[:])
            crow = P.sb("crow", [2, 1024], F32, st)
            P.ld(crow[0:1, :], D['c'])
            P.ld(crow[1:2, :], D['c_ctx'])
            pss = psS.next()
            for j in range(8):
                P.tr(pss[:, 2 * j:2 * j + 2], crow[0:2, j * 128:(j + 1) * 128], ident_f[0:2, 0:2])
            cvec = P.sb("cvec", [128, 8, 2], F32, st)
            P.act(cvec[:].rearrange("p k v -> p (k v)"), pss[:, 0:16], AF.Silu)
            badar = P.sb("badar", [2, 6144], F32, st)
            P.ld(badar[0:1, :], D['b_ada'][l:l + 1, :])
            P.ld(badar[1:2, :], D['b_ada'][l:l + 1, :])
            mods = P.sb("mods", [2, 6144], F32, st)
            for nb in range(12):
                s = stg.next()
                sv = s[:].rearrange("p (k n) -> p k n", k=8)
                P.ld(sv, D['w_ada'][l][:, nb * 512:(nb + 1) * 512].rearrange("(k p) n -> p k n", p=128))
                ps = psA.next()
                for k in range(8):
                    P.mm(ps[0:2, :], cvec[:, k, :], sv[:, k, :], start=(k == 0), stop=(k == 7))
                P.tt('dve', mods[0:2, nb * 512:(nb + 1) * 512], ps[0:2, :], badar[0:2, nb * 512:(nb + 1) * 512], ALU.add)
            pss = psS.next()
            for q in range(2):
                for j in range(8):
                    o = (q * 8 + j) * 2
                    P.tr(pss[:, o:o + 2], mods[0:2, q * 1024 + j * 128:q * 1024 + (j + 1) * 128], ident_f[0:2, 0:2])
            modc = P.sb("modc", [128, 2, 8, 2], F32, st)
            P.cp('dve', modc[:].rearrange("p q k v -> p (q k v)"), pss[:, 0:32])
            for v in range(2):
                P.stt(gs1[:, :, v], modc[:, 1, :, v], 1.0, CL[:, C_G1:C_G1 + 8], ALU.add, ALU.mult)
                P.cp('dve', sh1[:, :, v], modc[:, 0, :, v])
            g2r = P.sb("g2r", [2, 1024], F32, st)
            P.ld(g2r[0:1, :], D['g_norm2'][l:l + 1, :])
            P.ld(g2r[1:2, :], D['g_norm2'][l:l + 1, :])
            g2bc = P.sb("g2bc", [128, 1024], F32, st)

            def bcast(dst, src_rows, c0, v):
                for h in range(2):
                    ps = psA.next()
                    P.mm(ps[:, :], sel[0:2, v * 128:(v + 1) * 128], src_rows[0:2, c0 + h * 512:c0 + (h + 1) * 512])
                    P.cp('act', dst[:, h * 512:(h + 1) * 512], ps[:, :])
            bcast(g2bc, g2r, 0, 0)
            for n_ in bcn:
                for v in range(2):
                    BC[(n_, v)] = P.sb("bcp_%s%d" % (n_, v), [128, 1024], F32, st)
            for v in range(2):
                bcast(BC[('gate1', v)], mods, 2 * 1024, v)
                bcast(BC[('sh2', v)], mods, 3 * 1024, v)
                bcast(BC[('gs2', v)], mods, 4 * 1024, v)
                bcast(BC[('gate2', v)], mods, 5 * 1024, v)
                P.stt(BC[('gs2', v)][:], BC[('gs2', v)][:], 1.0, g2bc[:], ALU.add, ALU.mult)
            for n_ in bcn:
                for v in range(2):
                    P.ld(bcd[bcn.index(n_) * 2 + v], BC[(n_, v)][:])
            P.barrier()
            P.emit()
        lst = ExitStack()
        Win = P.sb("Win", [128, 8, 2048], BF16, lst)
        with ExitStack() as st:
            stg = Rot([P.sb("stg%d" % i, [128, 4096], F32, st) for i in range(2)])
            for kk in range(4):
                s = stg.next()
                sv = s[:].rearrange("p (k n) -> p k n", k=2)
                P.ld(sv, D['w_in'][l][kk * 256:(kk + 1) * 256, :].rearrange("(k p) n -> p k n", p=128))
                for k2 in range(2):
                    P.cp(cast_rr.next(), Win[:, kk * 2 + k2, :], sv[:, k2, :])
            wrs = P.sb("wrs", [128, 8, 16], F32, st)
            P.ld(wrs[:], D['w_router'][l].rearrange("(k p) e -> p k e", p=128))
            P.cp('dve', wr_b[:], wrs[:])
            P.barrier()
            P.emit()
        if stop_after == ('prep', l):
            break

        def prep_wfft(st_):
            Wf = P.sb("Wfft", [128, 8, 512], BF16, st_)
            WT = [P.sb("WT%d" % q, [128, 1024], BF16, st_) for q in range(2)]
            for q in range(2):
                pt = psT.next()
                for k in range(8):
                    P.tr(pt[:, k * 128:(k + 1) * 128], Win[:, k, 1792 + q * 128:1792 + (q + 1) * 128], ident_b[:, :])
                P.cp('act', WT[q][:], pt[:])
            for k in range(8):
                ps = psA.next()
                for q in range(2):
                    P.mm(ps[:, :], WT[q][:, k * 128:(k + 1) * 128], bd[:, q, :], start=(q == 0), stop=(q == 1))
                P.cp('dve', Wf[:, k, :], ps[:, :])
            return Wf

        def prep_wout(st_):
            Wo = P.sb("Wout", [128, 8, 1024], BF16, st_)
            for kk in range(2):
                P.ld(Wo[:, kk * 4:(kk + 1) * 4, :], D['w_out'][l][kk * 512:(kk + 1) * 512, :].rearrange("(k p) n -> p k n", p=128),
                     eng='pool')
            for k in range(8):
                P.act(Wo[:, k, :], Wo[:, k, :], AF.Copy, scale=CL[:, C_GOUT + k:C_GOUT + k + 1])
            return Wo

        def load_block(st, src, tok0, TB, v, hxT):
            for i in range(TB // 128):
                xt = xt_rot.next()
                P.ld(xt[:], src[tok0 + i * 128:tok0 + (i + 1) * 128, :])
                junk = junk_rot.next()
                ss = col(st)
                P.act(junk[:], xt[:], AF.Square, accum=ss[:])
                rstd = col(st)
                rsqrt_col(rstd[:], ss[:], 1024.0, st)
                xn = xn_rot.next()
                P.act(xn[:], xt[:], AF.Copy, scale=rstd[:, 0:1])
                pt = psT.next()
                for k in range(8):
                    P.tr(pt[:, k * 128:(k + 1) * 128], xn[:, k * 128:(k + 1) * 128], ident_b[:, :])
                for k in range(8):
                    o = hxT[:, k, i * 128:(i + 1) * 128]
                    wt = ["%s:t%d:%d" % (hxT.name, i, k % 2)]
                    if i % 2 == 0:
                        P.act(o, pt[:, k * 128:(k + 1) * 128], AF.Identity, scale=gs1[:, k, v:v + 1], bias=sh1[:, k, v:v + 1], wt=wt)
                    else:
                        P.ts('dve', o, pt[:, k * 128:(k + 1) * 128], gs1[:, k, v:v + 1], sh1[:, k, v:v + 1], op0=ALU.mult, op1=ALU.add, wt=wt)

        def inproj(hxT, TB, m):
            ps = psA.next()
            rt = ["%s:t%d:%d" % (hxT.name, i, p_) for i in range(TB // 128) for p_ in range(2)]
            for k in range(8):
                P.mm(ps[:, 0:TB], Win[:, k, m * 128:(m + 1) * 128], hxT[:, k, 0:TB], start=(k == 0), stop=(k == 7), rt=rt)
            return ps

        def conv_taps(u, p, w_cols, b_col, TB, RW, left):
            u3 = u.rearrange("p (r w) -> p r w", w=RW)
            p3 = p.rearrange("p (r w) -> p r w", w=RW)
            if b_col is None:
                P.act(u, p, AF.Copy, scale=w_cols[left])
            else:
                P.act(u, p, AF.Identity, scale=w_cols[left], bias=b_col)
            for kx in range(len(w_cols)):
                off = kx - left
                if off == 0:
                    continue
                if off < 0:
                    o, i_ = u3[:, :, -off:RW], p3[:, :, 0:RW + off]
                else:
                    o, i_ = u3[:, :, 0:RW - off], p3[:, :, off:RW]
                P.stt(o, i_, w_cols[kx], o, ALU.mult, ALU.add)

        def lru_dir(st, d, u, ub, TB, j, init, h_out, reverse):
            psr = psA.next()
            P.mm(psr[:, 0:TB], gates_b[:, d, 0, j, :], ub, start=True, stop=True)
            psi = psA.next()
            P.mm(psi[:, 0:TB], gates_b[:, d, 1, j, :], ub, start=True, stop=True)
            a = lt_rot.next()
            P.act(a[:, 0:TB], psr[:, 0:TB], AF.Sigmoid, bias=CL[:, C_BR + d * 4 + j:C_BR + d * 4 + j + 1])
            ig = lt_rot.next()
            P.act(ig[:, 0:TB], psi[:, 0:TB], AF.Sigmoid, bias=CL[:, C_BI + d * 4 + j:C_BI + d * 4 + j + 1])
            P.act(a[:, 0:TB], a[:, 0:TB], AF.Exp, scale=nsp[:, d * 4 + j:d * 4 + j + 1])
            sq = lt_rot.next()
            P.act(sq[:, 0:TB], a[:, 0:TB], AF.Square)
            P.act(sq[:, 0:TB], sq[:, 0:TB], AF.Ln, scale=-1.0, bias=1.0)
            P.act(sq[:, 0:TB], sq[:, 0:TB], AF.Exp, scale=0.5)
            P.tt('dve', ig[:, 0:TB], ig[:, 0:TB], sq[:, 0:TB], ALU.mult)
            P.tt('dve', ig[:, 0:TB], ig[:, 0:TB], u, ALU.mult)
            if reverse:
                P.scan(h_out[:, ::-1], a[:, 0:TB][:, ::-1], ig[:, 0:TB][:, ::-1], init)
            else:
                P.scan(h_out, a[:, 0:TB], ig[:, 0:TB], init)

        def lru_front(st, hxT, TB, RW, j):
            ps = inproj(hxT, TB, j)
            p = lt_rot.next()
            P.cp('act', p[:, 0:TB], ps[:, 0:TB])
            u = lt_rot.next()
            conv_taps(u[:, 0:TB], p[:, 0:TB], [CL[:, C_CW + kx * 4 + j:C_CW + kx * 4 + j + 1] for kx in range(4)],
                      CL[:, C_CB + j:C_CB + j + 1], TB, RW, 1)
            ub = ub_rot.next()
            P.cp('act', ub[:, 0:TB], u[:, 0:TB])
            return u, ub

        def group_norm(st, ys, TB, nch):
            ps = psA.next()
            for i, y in enumerate(ys):
                sq = sqb_rot.next()
                P.act(sq[:, 0:TB], y, AF.Square)
                P.mm(ps[:, 0:TB], ones_b[:, :], sq[:, 0:TB], start=(i == 0), stop=(i == len(ys) - 1))
            r = lt_rot.next()
            P.act(r[:, 0:TB], ps[:, 0:TB], AF.Ln, scale=1.0 / nch, bias=EPS)
            P.act(r[:, 0:TB], r[:, 0:TB], AF.Exp, scale=-0.5)
            return r

        for (v, src, T, TB, RW, off, full) in ((1, cin, T_C, 256, 256, T_X, not last), (0, xin, T_X, 512, 64, 0, True)):
            nblk = T // TB
            with ExitStack() as st:
                xt_rot = Rot([P.sb("xt%d" % i, [128, 1024], F32, st) for i in range(2)])
                junk_rot = Rot([P.sb("junk%d" % i, [128, 1024], BF16, st) for i in range(1)])
                xn_rot = Rot([P.sb("xn%d" % i, [128, 1024], BF16, st) for i in range(1)])
                hx_rot = Rot([P.sb("hxT%d" % i, [128, 8, 512], BF16, st) for i in range(2)])
                lt_rot = Rot([P.sb("lt%d" % i, [128, TB], F32, st) for i in range(7)])
                ub_rot = Rot([P.sb("ub%d" % i, [128, 512], BF16, st) for i in range(1)])
                sqb_rot = Rot([P.sb("sqb%d" % i, [128, 512], BF16, st) for i in range(2)])
                hbb_rot = Rot([P.sb("hbb%d" % i, [128, 512], BF16, st) for i in range(1)])
                if full:
                    Wfft = prep_wfft(st)
                    for b in range(nblk):
                        hxT = hx_rot.next()
                        load_block(st, src, b * TB, TB, v, hxT)
                        P.ld(hxd[:, :, off + b * TB:off + (b + 1) * TB].rearrange("k p t -> p k t"), hxT[:, :, 0:TB],
                             eng=STORE_ENG, wt=["hxd:%d" % b], rt=["%s:t%d:%d" % (hxT.name, i, p_) for i in range(TB // 128) for p_ in range(2)])
                        for i in range(TB // 128):
                            ps = psA.next()
                            for k in range(8):
                                P.mm(ps[:, :], hxT[:, k, i * 128:(i + 1) * 128], Wfft[:, k, :], start=(k == 0), stop=(k == 7),
                                     rt=["%s:t%d:%d" % (hxT.name, i, p_) for p_ in range(2)])
                            vt = sqb_rot.next()
                            P.cp('act', vt[:, :], ps[:, :])
                            r_ = slice(off + b * TB + i * 128, off + b * TB + (i + 1) * 128)
                            for q in range(2):
                                P.ld(Vd[q, r_, :].rearrange("t (part c) -> t part c", part=2),
                                     vt[:, :].rearrange("t (part qc) -> t part qc", part=2)[:, :, q * 128:(q + 1) * 128],
                                     eng=STORE_ENG, wt=["Vd:%d:%d:%d" % (b, i, q)])
                m0 = P.mark()
                if full:
                    psA = psA_lo
                P.ms('dve', st_b[:], 0.0) if v == 1 else None
                if v == 1:
                    P.ms('dve', st_f[:], 0.0)
                def fetch_block(hxT, b):
                    wt = ["%s:t%d:%d" % (hxT.name, i, p_) for i in range(TB // 128) for p_ in range(2)]
                    P.dma('sp', lambda e, o_=hxT[:, :, 0:TB], i_=hxd[:, :, off + b * TB:off + (b + 1) * TB].rearrange("k p t -> p k t"):
                          e.dma_start(out=o_, in_=i_), ["hxd:%d" % b], wt, 2.0 + TB * 16 * 128 / 150000.0)
                for b in reversed(range(nblk)):
                    hxT = hx_rot.next()
                    if full:
                        fetch_block(hxT, b)
                    else:
                        load_block(st, src, b * TB, TB, v, hxT)
                    for j in range(4):
                        u, ub = lru_front(st, hxT, TB, RW, j)
                        h = lt_rot.next()
                        lru_dir(st, 1, u[:, 0:TB], ub[:, 0:TB], TB, j, st_b[:, j:j + 1], h[:, 0:TB], True)
                        stn = col(st)
                        P.cp('dve', stn[:], h[:, 0:1])
                        P.cp('dve', st_b[:, j:j + 1], stn[:])
                        if full:
                            hb = hbb_rot.next()
                            P.cp('act', hb[:, 0:TB], h[:, 0:TB])
                            P.ld(hbd[j, :, off + b * TB:off + (b + 1) * TB], hb[:, 0:TB], eng=STORE_ENG, wt=["hbd:%d:%d" % (b, j)])
                m1 = P.mark()
                if full:
                    psA = psA_hi
                    yraw = [P.sb("yraw%d" % q, [128, T], BF16, st) for q in range(2)]
                    if v == 1:
                        Vc = P.sb("Vc", [128, 2, 2, 256], BF16, st)
                        for q in range(2):
                            P.ld(Vc[:, q], Vd[q, off:off + T, :].rearrange("(i p) n -> p i n", p=128),
                                 rt=["Vd:%d:%d:%d" % (b_, i_, q) for b_ in range(nblk) for i_ in range(TB // 128)])
                        for q in range(2):
                            ps = psA.next()
                            first = True
                            for i in range(2):
                                for part in range(2):
                                    P.mm(ps[:, 0:256], Vc[:, q, i, part * 128:(part + 1) * 128], dc[:, i, part, :],
                                         start=first, stop=(i == 1 and part == 1))
                                    first = False
                            P.cp('act', yraw[q][:, :], ps[:, 0:256])
                    else:
                        e3_rot = Rot([P.sb("e3t%d" % i, [128, 8, 2, 64], BF16, st) for i in range(1)])
                        Vq = P.sb("Vq", [128, 64, 2, 128], BF16, st)
                        Bq = P.sb("Bq", [128, 64, 256], BF16, st)
                        for q in range(2):
                            P.ld(Vq[:].rearrange("p n part c -> p (n part c)"),
                                 Vd[q, 0:T_X, :].rearrange("(n1 n2) pc -> n1 (n2 pc)", n2=64),
                                 rt=["Vd:%d:%d:%d" % (b_, i_, q) for b_ in range(nblk) for i_ in range(TB // 128)])
                            for cp2 in range(32):
                                ps = psA.next()
                                for c_ in range(2):
                                    cp_ = cp2 * 2 + c_
                                    for c2 in range(2):
                                        for part in range(2):
                                            lhsT = Vq[:, :, part, cp_ + 64 * c2]
                                            P.mm(ps[c2 * 64:(c2 + 1) * 64, c_ * 256:(c_ + 1) * 256], lhsT, cs1[:, part, :],
                                                 start=(part == 0), stop=(part == 1))
                                P.cp('act' if cp2 % 2 == 0 else 'dve', Bq[:, cp2 * 2:cp2 * 2 + 2, :].rearrange("p a b -> p (a b)"), ps[:, :])
                            for kb in range(16):
                                e3t = e3_rot.next()
                                P.ld(e3t[:], K['k_e3'][:, kb * 8:(kb + 1) * 8, :, :])
                                ps = psA.next()
                                for k1l in range(8):
                                    k1 = kb * 8 + k1l
                                    for c2 in range(2):
                                        pr = slice(c2 * 64, (c2 + 1) * 64)
                                        for part in range(2):
                                            P.mm(ps[pr, k1l * 64:(k1l + 1) * 64], Bq[pr, :, part * 128 + k1], e3t[pr, k1l, part, :],
                                                 start=(part == 0), stop=(part == 1))
                                o = yraw[q][:, :].rearrange("p (k2 k1) -> p k1 k2", k1=128)[:, kb * 8:(kb + 1) * 8, :]
                                P.cp('act' if kb % 2 == 0 else 'dve', o, ps[:, :].rearrange("p (a b) -> p a b", a=8))
                    NB = 512 if v == 0 else 256
                    sqf_rot = Rot([P.sb("sqf%d" % i, [128, 512], BF16, st) for i in range(2)])
                    rf_rot = Rot([P.sb("rf%d" % i, [128, 512], F32, st) for i in range(1)])
                    for b in range(T // NB):
                        ps = psA.next()
                        for q in range(2):
                            sq = sqf_rot.next()
                            P.act(sq[:, 0:NB], yraw[q][:, b * NB:(b + 1) * NB], AF.Square)
                            P.mm(ps[:, 0:NB], ones_b[:, :], sq[:, 0:NB], start=(q == 0), stop=(q == 1))
                        r = rf_rot.next()
                        P.act(r[:, 0:NB], ps[:, 0:NB], AF.Ln, scale=1.0 / 256, bias=EPS)
                        P.act(r[:, 0:NB], r[:, 0:NB], AF.Exp, scale=-0.5)
                        for q in range(2):
                            yo = sqf_rot.next()
                            P.tt('dve', yo[:, 0:NB], yraw[q][:, b * NB:(b + 1) * NB], r[:, 0:NB], ALU.mult)
                            P.ld(yfd[q, :, off + b * NB:off + (b + 1) * NB], yo[:, 0:NB], eng=STORE_ENG, wt=["yfd:%d:%d" % (b, q)])
                    psA = psA_all
                    if INTERLEAVE:
                        P.interleave(m0, m1, P.mark())
                P.barrier()
                P.emit()
            if not full:
                with ExitStack() as st:
                    xt_rot = Rot([P.sb("xt%d" % i, [128, 1024], F32, st) for i in range(2)])
                    junk_rot = Rot([P.sb("junk%d" % i, [128, 1024], BF16, st) for i in range(1)])
                    xn_rot = Rot([P.sb("xn%d" % i, [128, 1024], BF16, st) for i in range(2)])
                    hx_rot = Rot([P.sb("hxT%d" % i, [128, 8, 512], BF16, st) for i in range(1)])
                    lt_rot = Rot([P.sb("lt%d" % i, [128, 512], F32, st) for i in range(10)])
                    ub_rot = Rot([P.sb("ub%d" % i, [128, 512], BF16, st) for i in range(2)])
                    hxT = hx_rot.next()
                    load_block(st, src, 0, TB, v, hxT)
                    for j in range(4):
                        u, ub = lru_front(st, hxT, TB, RW, j)
                        h = lt_rot.next()
                        lru_dir(st, 0, u[:, 0:TB], ub[:, 0:TB], TB, j, 0.0, h[:, 0:TB], False)
                        P.cp('dve', st_f[:, j:j + 1], h[:, TB - 1:TB])
                    P.barrier()
                    P.emit()
                continue
            with ExitStack() as st:
                xt_rot = Rot([P.sb("xt%d" % i, [128, 1024], F32, st) for i in range(2)])
                junk_rot = Rot([P.sb("junk%d" % i, [128, 1024], BF16, st) for i in range(1)])
                hx_rot = Rot([P.sb("hxT%d" % i, [128, 8, 512], BF16, st) for i in range(2)])
                lt_rot = MultiRot([Rot([P.sb("lt%d_%d" % (pp, i), [128, TB], F32, st) for i in range(LT_DEPTH)]) for pp in range(LT_POOLS)])
                Wout = prep_wout(st)
                for n_ in ('gate1', 'sh2', 'gs2'):
                    bc_load(n_, v, st)
                ylru_tt = [P.sb("ylru_t%d" % i, [128, 4, 512], F32, st) for i in range(YL_BUFS)]
                ysc_tt = [P.sb("ysc_t%d" % i, [128, 2, 512], F32, st) for i in range(YL_BUFS)]
                ub_rot = Rot([P.sb("ub%d" % i, [128, 512], BF16, st) for i in range(2)])
                sqb_rot = Rot([P.sb("sqb%d" % i, [128, 512], BF16, st) for i in range(2)])
                hbb_rot = Rot([P.sb("hbb%d" % i, [128, 512], BF16, st) for i in range(2)])
                ynb_rot = Rot([P.sb("ynb%d" % i, [128, 8, 512], BF16, st) for i in range(2)])
                x1_rot = Rot([P.sb("x1t%d" % i, [128, 1024], F32, st) for i in range(2)])
                hx2_rot = Rot([P.sb("hx2e%d" % i, [128, 1056], BF16, st) for i in range(2)])
                hx2T_rot = Rot([P.sb("hx2T%d" % i, [128, 1024], BF16, st) for i in range(1)])
                sm_rot = Rot([P.sb("sm%d" % i, [128, 16], F32, st) for i in range(6)])
                afs_rot = Rot([P.sb("afs%d" % i, [16, 128], F32, st) for i in range(2)])
                for b in range(nblk):
                    tok0 = b * TB
                    hxT = hx_rot.next()
                    fetch_block(hxT, b)
                    ynb = ynb_rot.next()
                    ylru_t, ysc_t = ylru_tt[b % YL_BUFS], ysc_tt[b % YL_BUFS]
                    ylru = []
                    for j in range(4):
                        lt_rot.set(b if LT_BY_BLOCK else j)
                        u, ub = lru_front(st, hxT, TB, RW, j)
                        h = lt_rot.next()
                        init = st_f[:, j:j + 1]
                        lru_dir(st, 0, u[:, 0:TB], ub[:, 0:TB], TB, j, init, h[:, 0:TB], False)
                        stn = col(st)
                        P.cp('dve', stn[:], h[:, TB - 1:TB])
                        P.cp('dve', st_f[:, j:j + 1], stn[:])
                        hb = hbb_rot.next()
                        P.ld(hb[:, 0:TB], hbd[j, :, off + tok0:off + tok0 + TB])
                        P.tt('dve', h[:, 0:TB], h[:, 0:TB], hb[:, 0:TB], ALU.add)
                        psg = inproj(hxT, TB, 4 + j)
                        gg = lt_rot.next()
                        P.act(gg[:, 0:TB], psg[:, 0:TB], AF.Gelu_apprx_tanh)
                        P.tt('dve', ylru_t[:, j, 0:TB], gg[:, 0:TB], h[:, 0:TB], ALU.mult)
                        ylru.append(ylru_t[:, j, 0:TB])
                    r = group_norm(st, ylru, TB, 512.0)
                    for j in range(4):
                        P.tt('dve', ynb[:, j, 0:TB], ylru[j], r[:, 0:TB], ALU.mult)
                    ysc = []
                    for j in range(2):
                        lt_rot.set(b if LT_BY_BLOCK else j)
                        psc = inproj(hxT, TB, 10 + j)
                        cc_ = lt_rot.next()
                        P.cp('act', cc_[:, 0:TB], psc[:, 0:TB])
                        psx = inproj(hxT, TB, 12 + j)
                        vv = lt_rot.next()
                        P.tt('dve', vv[:, 0:TB], psx[:, 0:TB], cc_[:, 0:TB], ALU.mult)
                        cv = lt_rot.next()
                        conv_taps(cv[:, 0:TB], vv[:, 0:TB], [CL[:, C_SCW + kx * 2 + j:C_SCW + kx * 2 + j + 1] for kx in range(3)],
                                  None, TB, RW, 1)
                        psb = inproj(hxT, TB, 8 + j)
                        P.tt('dve', ysc_t[:, j, 0:TB], psb[:, 0:TB], cv[:, 0:TB], ALU.mult)
                        ysc.append(ysc_t[:, j, 0:TB])
                    r = group_norm(st, ysc, TB, 256.0)
                    for j in range(2):
                        P.tt('dve', ynb[:, 4 + j, 0:TB], ysc[j], r[:, 0:TB], ALU.mult)
                    for q in range(2):
                        P.ld(ynb[:, 6 + q, 0:TB], yfd[q, :, off + tok0:off + tok0 + TB])
                    for i in range(TB // 128):
                        r0 = tok0 + i * 128
                        xt = xt_rot.next()
                        P.ld(xt[:], src[r0:r0 + 128, :])
                        x1 = x1_rot.next()
                        for hf in range(2):
                            ps = psA.next()
                            for kc in range(8):
                                P.mm(ps[:, :], ynb[:, kc, i * 128:(i + 1) * 128], Wout[:, kc, hf * 512:(hf + 1) * 512],
                                     start=(kc == 0), stop=(kc == 7))
                            P.tt('dve', x1[:, hf * 512:(hf + 1) * 512], ps[:, :], BC[('gate1', v)][:, hf * 512:(hf + 1) * 512], ALU.mult)
                        P.tt('dve', x1[:], x1[:], xt[:], ALU.add)
                        P.ld(xo[off + r0:off + r0 + 128, :], x1[:], eng=STORE_ENG, wt=["xo:%d" % r0])
                        junk = junk_rot.next()
                        ss = col(st)
                        P.act(junk[:], x1[:], AF.Square, accum=ss[:])
                        rstd = col(st)
                        rsqrt_col(rstd[:], ss[:], 1024.0, st)
                        P.stt(xt[:], x1[:], rstd[:, 0:1], BC[('gs2', v)][:], ALU.mult, ALU.mult)
                        hx2e = hx2_rot.next()
                        P.tt('dve', hx2e[:, 0:1024], xt[:], BC[('sh2', v)][:], ALU.add)
                        pt = psT.next()
                        for k in range(8):
                            P.tr(pt[:, k * 128:(k + 1) * 128], hx2e[:, k * 128:(k + 1) * 128], ident_b[:, :])
                        hx2T = hx2T_rot.next()
                        P.cp('act', hx2T[:], pt[:])
                        pss = psS.next()
                        for k in range(8):
                            P.mm(pss[:, 0:16], hx2T[:, k * 128:(k + 1) * 128], wr_b[:, k, :], start=(k == 0), stop=(k == 7))
                        mx = sm_rot.next()
                        P.red(mx[:, 0:1], pss[:, 0:16], ALU.max)
                        P.ts('dve', mx[:, 1:2], mx[:, 0:1], -1.0)
                        ex = sm_rot.next()
                        P.act(ex[:, :], pss[:, 0:16], AF.Exp, bias=mx[:, 1:2], accum=mx[:, 2:3])
                        P.recip(mx[:, 3:4], mx[:, 2:3])
                        aff = sm_rot.next()
                        P.ts('dve', aff[:, :], ex[:, :], mx[:, 3:4])
                        P.cp('dve', hx2e[:, 1024:1040], aff[:, :])
                        P.tt('dve', ex[:, :], aff[:, :], hx2e[:, 1024:1040], ALU.subtract)
                        P.cp('dve', hx2e[:, 1040:1056], ex[:, :])
                        pss2 = psS.next()
                        P.tr(pss2[0:16, 0:128], aff[:, :], ident_f[:, :])
                        afs = afs_rot.next()
                        P.cp('act', afs[0:16, :], pss2[0:16, 0:128])
                        P.ld(affTd[:, off + r0:off + r0 + 128], afs[0:16, :], eng=STORE_ENG, wt=["affTd:%d" % r0])
                        P.ld(hx2ext[off + r0:off + r0 + 128, :], hx2e[:, :], eng=STORE_ENG, wt=["hx2ext:%d" % r0])
                P.barrier()
                P.emit()
        lst.close()
        if stop_after == ('mixer', l):
            if 'x1' in dbg_out:
                P.ld(dbg_out['x1'], xo)
            break

        with ExitStack() as st:
            onesrow = P.sb("onesrow", [64, 128], F32, st)
            P.ms('dve', onesrow[:], 1.0)
            iota_c = P.sb("iota_c", [64, 1024], F32, st)
            P.ld(iota_c[:], K['k_iota_c'][0:64, :])
            sets = [(0, 64, 1024, 0)] + ([(1, 2, 32, T_X)] if not last else [])
            for (v, NT, cap, off) in sets:
                A = P.sb("A", [NT, 16, 128], F32, st)
                P.ld(A[:], affTd[:, off:off + NT * 128].rearrange("e (i t) -> i e t", t=128))
                lo = P.sb("lo", [NT, 16], F32, st)
                hi = P.sb("hi", [NT, 16], F32, st)
                mid = P.sb("mid", [NT, 16], F32, st)
                cmp_ = P.sb("cmp", [NT, 16, 128], F32, st)
                cnt = P.sb("cnt", [NT, 16], F32, st)
                ge = P.sb("ge", [NT, 16], F32, st)
                dd = P.sb("dd", [NT, 16], F32, st)
                if NT == 64:
                    NP, TW = 128, 64
                    Ab = P.sb("Ab", [128, 16, 64], F32, st)
                    src2 = affTd[:, off:off + NT * 128].rearrange("e (i h t) -> h i e t", h=2, t=64)
                    for h_ in range(2):
                        P.ld(Ab[h_ * 64:(h_ + 1) * 64, :, :], src2[h_])
                    lo = P.sb("lo2", [128, 16], F32, st)
                    mid = P.sb("mid2", [128, 16], F32, st)
                    cmpb = P.sb("cmp2", [128, 16, 64], F32, st)
                    cnt2 = P.sb("cnt2", [128, 16], F32, st)
                    ge = P.sb("ge2", [128, 16], F32, st)
                else:
                    NP, TW, Ab, cmpb, cnt2 = NT, 128, A, cmp_, cnt
                P.ms('dve', lo[:], 0.0)
                for it in range(30):
                    c_it = 0.5 ** (it + 1)
                    P.ts('dve', mid[:], lo[:], c_it, op0=ALU.add)
                    P.tt('dve', cmpb[:], Ab[:], mid[:].unsqueeze(2).to_broadcast([NP, 16, TW]), ALU.is_ge)
                    P.red(cnt2[:], cmpb[:], ALU.add)
                    pss = psS.next()
                    P.mm(pss[0:NP, 0:16], ones_f[0:NP, 0:NP], cnt2[:], start=True, stop=True)
                    P.ts('dve', ge[:], pss[0:NP, 0:16], float(cap) - 0.5, c_it, op0=ALU.is_ge, op1=ALU.mult)
                    P.tt('dve', lo[:], lo[:], ge[:], ALU.add)
                P.tt('dve', cmp_[:], A[:], lo[0:NT, :].unsqueeze(2).to_broadcast([NT, 16, 128]), ALU.is_ge)
                RT = P.sb("RT", [NT, 16, 132], F32, st)
                for e in range(16):
                    P.scan(RT[:, e, 0:128], onesrow[0:NT, :], cmp_[:, e, :], 0.0)
                P.cp('dve', cnt[:], RT[:, :, 127])
                pss = psS.next()
                P.mm(pss[0:NT, 0:16], triu[0:NT, 0:NT], cnt[:], start=True, stop=True)
                base = P.sb("base", [NT, 16], F32, st)
                incl = P.sb("incl", [NT, 16], F32, st)
                P.cp('dve', base[:], pss[0:NT, 0:16])
                P.tt('dve', incl[:], base[:], cnt[:], ALU.add)
                P.cp('dve', RT[:, :, 128], base[:])
                tso = P.sb("tso", [NT, 1], F32, st)
                P.ts('dve', tso[:], tstart[0:NT, :], float(off), op0=ALU.add)
                P.cp('dve', RT[:, :, 129], tso[:, 0:1].to_broadcast([NT, 16]))
                P.ms('dve', RT[:, :, 130:132], 1.0)
                oh1 = P.sb("oh1", [NT, 1024], F32, st)
                oh = P.sb("oh", [NT, 1024], F32, st)
                nslot = (cap + 127) // 128
                for e in range(16):
                    P.ts('dve', oh1[:, 0:cap], iota_c[0:NT, 0:cap], base[:, e:e + 1], op0=ALU.is_ge)
                    P.stt(oh[:, 0:cap], iota_c[0:NT, 0:cap], incl[:, e:e + 1], oh1[:, 0:cap], ALU.is_lt, ALU.mult)
                    for j in range(nslot):
                        M = min(128, cap - j * 128)
                        slot = j if v == 0 else 8
                        pss = psS.next()
                        P.mm(pss[0:M, 0:132], oh[:, j * 128:j * 128 + M], RT[:, e, :], start=True, stop=True)
                        thr = P.sb("thr", [128, 4], F32, st)
                        P.ts('dve', thr[0:M, 0:1], pss[0:M, 128:129], -1.0, cidx[0:M, j:j + 1], op0=ALU.mult, op1=ALU.add)
                        jk = P.sb("jk", [128, 128], F32, st)
                        P.ts('dve', jk[0:M, :], pss[0:M, 0:128], thr[0:M, 0:1], pss[0:M, 129:130], op0=ALU.is_le, op1=ALU.add,
                             accum=thr[0:M, 2:3])
                        P.cp('dve', idx_all[0:M, e, slot:slot + 1], thr[0:M, 2:3])
                        P.cp('dve', val_all[0:M, e, slot:slot + 1], pss[0:M, 130:131])
            if 'idx' in dbg_out:
                idf = P.sb("idf", [128, 16 * 9], F32, st)
                P.cp('dve', idf[:], idx_all[:].rearrange("p e s -> p (e s)"))
                dump('idx', idf[:])
            P.barrier()
            P.emit()
        if stop_after == ('select', l):
            break

        with ExitStack() as st:
            Wg = P.sb("Wg", [128, 8, 1024], BF16, st)
            Wu = P.sb("Wu", [128, 8, 1024], BF16, st)
            Wd = P.sb("Wd", [128, 8, 1024], BF16, st)
            stg = Rot([P.sb("stg%d" % i, [128, 4096], F32, st) for i in range(MOE_STG)])
            xs_rot = Rot([P.sb("xs%d" % i, [128, 1056], BF16, st) for i in range(6)])
            xsT_rot = Rot([P.sb("xsT%d" % i, [128, 8, 1056], BF16, st) for i in range(2)])
            hid_rot = Rot([P.sb("hid%d" % i, [128, 8, 1056], BF16, st) for i in range(1)])
            gv_all = P.sb("gv_all", [128, 16, 9], F32, st)
            bc_load('gate2', 0, st)
            if not last:
                bc_load('gate2', 1, st, rows=32)
            sg_rot = Rot([P.sb("sg%d" % i, [128, 512], F32, st) for i in range(2)])
            ys_rot = Rot([P.sb("ys%d" % i, [128, 1024], F32, st) for i in range(3)])
            nsl = 8 if last else 9
            for e in range(16):
                for (Wt, nm_) in ((Wg, 'w_gate_e'), (Wu, 'w_up_e'), (Wd, 'w_down_e')):
                    for hh in range(2):
                        s = stg.next()
                        sv = s[:].rearrange("p (k n) -> p k n", k=4)
                        P.ld(sv, D[nm_][l, e][hh * 512:(hh + 1) * 512, :].rearrange("(k p) n -> p k n", p=128))
                        for k4 in range(4):
                            P.cp('act' if k4 % 2 == 0 else 'dve', Wt[:, hh * 4 + k4, :], sv[:, k4, :])
                xsT = xsT_rot.next()
                hid = hid_rot.next()
                for s_ in range(nsl):
                    M = 128 if s_ < 8 else 32
                    c0 = s_ * 128
                    xs = xs_rot.next()
                    P.gather(xs[0:M, :], hx2ext, idx_all[0:M, e, s_:s_ + 1])
                    P.tt('dve', gv_all[0:M, e, s_:s_ + 1], xs[0:M, 1024 + e:1025 + e], xs[0:M, 1040 + e:1041 + e], ALU.add)
                    P.tt('dve', gv_all[0:M, e, s_:s_ + 1], gv_all[0:M, e, s_:s_ + 1], val_all[0:M, e, s_:s_ + 1], ALU.mult)
                    pt = psT.next()
                    for k in range(8):
                        P.tr(pt[:, k * 128:k * 128 + M], xs[0:M, k * 128:(k + 1) * 128], ident_b[0:M, 0:M])
                    P.cp('act' if s_ % 2 == 0 else 'dve', xsT[:, :, c0:c0 + M], pt[:, :].rearrange("p (k m) -> p k m", k=8)[:, :, 0:M])
                groups = [(0, 512), (512, 512)] + ([(1024, 32)] if not last else [])
                for (c0, N) in groups:
                    for f in range(8):
                        psg = psA.next()
                        for k in range(8):
                            P.mm(psg[:, 0:N], Wg[:, k, f * 128:(f + 1) * 128], xsT[:, k, c0:c0 + N], start=(k == 0), stop=(k == 7))
                        psu = psA.next()
                        for k in range(8):
                            P.mm(psu[:, 0:N], Wu[:, k, f * 128:(f + 1) * 128], xsT[:, k, c0:c0 + N], start=(k == 0), stop=(k == 7))
                        sg = sg_rot.next()
                        P.act(sg[:, 0:N], psg[:, 0:N], AF.Silu)
                        P.tt('dve', hid[:, f, c0:c0 + N], psu[:, 0:N], sg[:, 0:N], ALU.mult)
                for s_ in range(nsl):
                    M = 128 if s_ < 8 else 32
                    c0 = s_ * 128
                    v = 0 if s_ < 8 else 1
                    ys = ys_rot.next()
                    for hf in range(2):
                        ps = psA.next()
                        for f in range(8):
                            P.mm(ps[0:M, :], hid[:, f, c0:c0 + M], Wd[:, f, hf * 512:(hf + 1) * 512], start=(f == 0), stop=(f == 7))
                        P.stt(ys[0:M, hf * 512:(hf + 1) * 512], ps[0:M, :], gv_all[0:M, e, s_:s_ + 1],
                              BC[('gate2', v)][0:M, hf * 512:(hf + 1) * 512], ALU.mult, ALU.mult)
                    P.scatter_add(xo, idx_all[0:M, e, s_:s_ + 1], ys[0:M, :],
                                  rt=["sc:%d:%d" % (e - 1, q_) for q_ in range(nsl)] if e > 0 else [],
                                  wt=["sc:%d:%d" % (e, s_)])
            P.barrier()
            P.emit()
        if stop_after == ('moe', l):
            if 'x1' in dbg_out:
                P.ld(dbg_out['x1'], xo)
            break

    if stop_after is None:
        with ExitStack() as st:
            xt_rot = Rot([P.sb("xt%d" % i, [128, 1024], F32, st) for i in range(3)])
            junk_rot = Rot([P.sb("junk%d" % i, [128, 1024], BF16, st) for i in range(2)])
            o_rot = Rot([P.sb("ot%d" % i, [128, 1024], F32, st) for i in range(3)])
            xf = xres[(depth - 1) % 2]
            gfin = P.sb("gfin", [128, 1024], F32, st)
            gfr = P.sb("gfr", [2, 1024], F32, st)
            P.ld(gfr[0:1, :], D['g_final'])
            P.ld(gfr[1:2, :], D['g_final'])
            for h in range(2):
                ps = psA.next()
                P.mm(ps[:, :], sel[0:2, 0:128], gfr[0:2, h * 512:(h + 1) * 512])
                P.cp('act', gfin[:, h * 512:(h + 1) * 512], ps[:, :])
            for i in range(T_X // 128):
                xt = xt_rot.next()
                P.ld(xt[:], xf[i * 128:(i + 1) * 128, :])
                junk = junk_rot.next()
                ss = col(st)
                P.act(junk[:], xt[:], AF.Square, accum=ss[:])
                rstd = col(st)
                rsqrt_col(rstd[:], ss[:], 1024.0, st)
                ot = o_rot.next()
                P.stt(ot[:], xt[:], rstd[:, 0:1], gfin[:], ALU.mult, ALU.mult)
                P.ld(out_d[i * 128:(i + 1) * 128, :], ot[:], eng=STORE_ENG, wt=["out:%d" % i])
            P.barrier()
            P.emit()
    else:
        P.barrier()
        P.emit()
    P.stack.close()
    return nc


_CONSTS = None


def make_in_maps(inputs, n_cores=8):
    global _CONSTS
    if _CONSTS is None:
        _CONSTS = host_consts()
    maps = []
    f = lambda a: np.ascontiguousarray(np.asarray(a, dtype=np.float32))
    shared = {n: f(inputs[n]) for n in IN_SHAPES if n not in ('x', 'c', 'ctx', 'c_ctx', 'g_final')}
    shared['c_ctx'] = f(inputs['c_ctx']).reshape(1, 1024)
    shared['g_final'] = f(inputs['g_final']).reshape(1, 1024)
    shared.update(_CONSTS)
    for core in range(n_cores):
        b = core % 4
        m = dict(shared)
        m['x'] = f(inputs['x'][b])
        m['c'] = f(inputs['c'][b]).reshape(1, 1024)
        m['ctx'] = f(inputs['ctx'][b])
        maps.append(m)
    return maps


def kernel(**inputs):
    nc = build()
    maps = make_in_maps(inputs)
    res = run_bass_kernel_spmd(nc, maps, core_ids=list(range(8)))
    out = np.stack([np.asarray(res.results[b]["out"], dtype=np.float32) for b in range(4)], axis=0)
    return out
```

```python
import numpy as np
import ml_dtypes
from contextlib import ExitStack
import concourse.bass as bass
import concourse.mybir as mybir
from concourse.bass_utils import run_bass_kernel_spmd

F32 = mybir.dt.float32
BF16 = mybir.dt.bfloat16
I32 = mybir.dt.int32
ALU = mybir.AluOpType
AF = mybir.ActivationFunctionType
AX = mybir.AxisListType

ENGS = ['pe', 'act', 'dve', 'pool', 'sp']
NDMASEM = 8
SAME_ENGINE_SYNC = True
SCHEDULE = True
TABLE_AWARE = True
ASET = {AF.Sigmoid: 'sig', AF.Silu: 'silu', AF.Exp: 'exp', AF.Ln: 'exp', AF.Sqrt: 'sqrt', AF.Gelu_apprx_tanh: 'gelu'}
PE_GHZ = 1.9
SCHED_WINDOW = 48
SCHED_MODE = 'fifo'
INTERLEAVE = True
LT_POOLS, LT_DEPTH, LT_BY_BLOCK = 2, 7, False
YL_BUFS = 1
MOE_STG = 2
PS_A, PS_T, PS_S = 5, 1, 2
EPS = 1e-6
T_X = 8192
T_C = 256
NTOK = T_X + T_C


class Prog:
    def __init__(self, nc):
        self.nc = nc
        self.pending = []
        self.eng_seq = {e: 0 for e in ENGS}
        self.dma_cnt = {e: 0 for e in ENGS}
        self.dma_val = {}
        self.known = {e: {} for e in ENGS}
        self.stack = ExitStack()
        self.sems = {}
        for e in ENGS:
            k = 'eng:' + e
            self.sems[k] = self.stack.enter_context(nc.semaphore(k.replace(':', '_')))
            for j in range(NDMASEM):
                k = 'dma:%s:%d' % (e, j)
                self.sems[k] = self.stack.enter_context(nc.semaphore(k.replace(':', '_')))
        self.uid = 0
        self.sim_time = 0.0

    def sb(self, name, shape, dt, stack=None):
        self.uid += 1
        return (stack or self.stack).enter_context(self.nc.sbuf_tensor("%s_%d" % (name, self.uid), list(shape), dt))

    def ps(self, name, shape, dt=F32):
        return self.stack.enter_context(self.nc.psum_tensor(name, list(shape), dt))

    @staticmethod
    def _tok(t):
        if isinstance(t, (str, int)):
            return t
        return t.tensor.name

    def op(self, eng, fn, reads=(), writes=(), cost=0.1, aset=None):
        r = [self._tok(t) for t in reads if t is not None and not isinstance(t, (int, float))]
        w = [self._tok(t) for t in writes]
        self.pending.append((eng, False, fn, r, w, cost, aset))

    def dma(self, eng, fn, reads=(), writes=(), cost=3.0):
        r = [self._tok(t) for t in reads]
        w = [self._tok(t) for t in writes]
        self.pending.append((eng, True, fn, r, w, cost, None))

    def barrier(self):
        pass

    def mark(self):
        return len(self.pending)

    def interleave(self, i0, i1, i2):
        a, b = self.pending[i0:i1], self.pending[i1:i2]
        if not a or not b:
            return
        out = []
        ia = ib = 0
        while ia < len(a) or ib < len(b):
            if ib >= len(b) or (ia < len(a) and ia * len(b) <= ib * len(a)):
                out.append(a[ia])
                ia += 1
            else:
                out.append(b[ib])
                ib += 1
        self.pending[i0:i2] = out

    def _schedule(self, ops):
        n = len(ops)
        last_w = {}
        readers = {}
        deps = []
        for i, (eng, isdma, fn, r, w, cost, aset_) in enumerate(ops):
            d = set()
            for t in r:
                if t in last_w:
                    d.add(last_w[t])
            for t in w:
                if t in last_w:
                    d.add(last_w[t])
                rl = readers.get(t)
                if rl:
                    d.update(rl)
            d.discard(i)
            deps.append(d)
            for t in r:
                readers.setdefault(t, []).append(i)
            for t in w:
                last_w[t] = i
                readers[t] = []
        succ = [[] for _ in range(n)]
        ndep = [len(d) for d in deps]
        for i, d in enumerate(deps):
            for j in d:
                succ[j].append(i)
        ready = [0.0] * n
        blev = [0.0] * n
        for i in range(n - 1, -1, -1):
            m = 0.0
            for s_ in succ[i]:
                if blev[s_] > m:
                    m = blev[s_]
            blev[i] = m + ops[i][5] + 0.3
        queues = {e: [] for e in ENGS}
        for i, o in enumerate(ops):
            queues[o[0]].append(i)
        free = {e: 0.0 for e in ENGS}
        dma_fin = {e: [] for e in ENGS}
        order = {e: [] for e in ENGS}
        remaining = n
        W = SCHED_WINDOW
        cur_set = [None]
        while remaining:
            best = None
            for e in ENGS:
                q = queues[e]
                if not q:
                    continue
                fe = free[e]
                lim = min(W, len(q))
                first = None
                cand = None
                if e == 'act' and TABLE_AWARE:
                    for p in range(lim):
                        i = q[p]
                        if ndep[i] or ready[i] > fe:
                            continue
                        a_ = ops[i][6]
                        if a_ is None or a_ == cur_set[0]:
                            cand = (fe, i, e, p)
                            break
                    if cand is not None:
                        if best is None or cand[0] < best[0] or (cand[0] == best[0] and cand[1] < best[1]):
                            best = cand
                        continue
                for p in range(lim):
                    i = q[p]
                    if ndep[i]:
                        continue
                    st = ready[i] if ready[i] > fe else fe
                    if ops[i][1]:
                        k = len(dma_fin[e])
                        if k >= NDMASEM and dma_fin[e][k - NDMASEM] > st:
                            st = dma_fin[e][k - NDMASEM]
                    if SCHED_MODE == 'blev':
                        key = (max(st, fe), -blev[i])
                        if cand is None or key < ckey:
                            cand = (st, i, e, p)
                            ckey = key
                        first = cand
                        continue
                    if first is None:
                        first = (st, i, e, p)
                        cand = first
                        if st <= fe:
                            break
                    else:
                        c = ops[i][5] if not ops[i][1] else 0.05
                        if st + c <= first[0] and st < cand[0]:
                            cand = (st, i, e, p)
                            if st <= fe:
                                break
                if cand is not None and (best is None or cand[0] < best[0] or (cand[0] == best[0] and cand[1] < best[1])):
                    best = cand
            st, i, e, p = best
            queues[e].pop(p)
            o = ops[i]
            f = st + o[5]
            if e == 'act' and o[6] is not None:
                if o[6] != cur_set[0]:
                    f += 1.28
                cur_set[0] = o[6]
            if o[1]:
                free[e] = st + 0.05
                dma_fin[e].append(f)
            else:
                free[e] = f
            for s_ in succ[i]:
                ndep[s_] -= 1
                lat = 0.0 if (ops[s_][0] == e and not o[1]) else 0.3
                if f + lat > ready[s_]:
                    ready[s_] = f + lat
            order[e].append(i)
            remaining -= 1
        tend = max(list(free.values()) + [f_ for v in dma_fin.values() for f_ in v] + [0.0])
        return deps, order, tend

    def emit(self):
        ops = self.pending
        self.pending = []
        nc = self.nc
        sems = self.sems
        if ops:
            if SCHEDULE:
                deps, order, tend = self._schedule(ops)
            else:
                deps, order, tend = self._schedule_inorder(ops)
            self.sim_time += tend
        else:
            deps, order = [], {e: [] for e in ENGS}
        ev = [None] * len(ops)
        for e in ENGS:
            for i in order[e]:
                if ops[i][1]:
                    k = self.dma_cnt[e]
                    self.dma_cnt[e] += 1
                    key = 'dma:%s:%d' % (e, k % NDMASEM)
                    prev = self.dma_val.get(key, 0)
                    self.dma_val[key] = prev + 16
                    ev[i] = (key, prev + 16, prev)
                else:
                    self.eng_seq[e] += 1
                    ev[i] = ('eng:' + e, self.eng_seq[e], 0)
        prog = {e: [] for e in ENGS}
        for e in ENGS:
            kn = self.known[e]
            for i in order[e]:
                need = {}
                for d in deps[i]:
                    k, v, _ = ev[d]
                    if k == 'eng:' + e and (e == 'pe' or not SAME_ENGINE_SYNC):
                        continue
                    if need.get(k, 0) < v:
                        need[k] = v
                k, v, prev = ev[i]
                if ops[i][1] and prev > 0 and need.get(k, 0) < prev:
                    need[k] = prev
                waits = []
                for k2, v2 in need.items():
                    if kn.get(k2, 0) >= v2:
                        continue
                    kn[k2] = v2
                    waits.append((k2, v2))
                prog[e].append((waits, ops[i][2], k, 16 if ops[i][1] else 1))
        allv = {}
        for e in ENGS:
            if self.eng_seq[e]:
                allv['eng:' + e] = self.eng_seq[e]
        for k, v in self.dma_val.items():
            allv[k] = v
        for e in ENGS:
            kn = self.known[e]
            waits = []
            for k, v in allv.items():
                if k == 'eng:' + e or kn.get(k, 0) >= v:
                    continue
                kn[k] = v
                waits.append((k, v))
            kn['eng:' + e] = self.eng_seq[e]
            prog[e].append((waits, None, None, 0))

        def run(h, lst):
            for waits, fn, k, amt in lst:
                for (wk, wv) in waits:
                    h.wait_ge(sems[wk], wv)
                if fn is not None:
                    fn(h).then_inc(sems[k], amt)
        with nc.Block() as block:
            @block.tensor
            def _(e):
                run(e, prog['pe'])

            @block.scalar
            def _(e):
                run(e, prog['act'])

            @block.vector
            def _(e):
                run(e, prog['dve'])

            @block.gpsimd
            def _(e):
                run(e, prog['pool'])

            @block.sync
            def _(e):
                run(e, prog['sp'])

    def _schedule_inorder(self, ops):
        n = len(ops)
        W_save = None
        global SCHED_WINDOW
        W_save, SCHED_WINDOW = SCHED_WINDOW, 1
        try:
            return self._schedule(ops)
        finally:
            SCHED_WINDOW = W_save

    def mm(self, out, lhsT, rhs, start=True, stop=True, rt=None):
        c = max(64, rhs.free_size()) / PE_GHZ / 1000.0 * (4.0 if rhs.dtype == F32 else 1.0) + 0.02
        self.op('pe', lambda e: e.matmul(out, lhsT=lhsT, rhs=rhs, start=start, stop=stop), [lhsT, rhs] + list(rt or []), [out], c)

    def tr(self, out, in_, ident):
        self.op('pe', lambda e: e.transpose(out, in_, ident), [in_, ident], [out], 0.11)

    def act(self, out, in_, func, bias=None, scale=None, accum=None, wt=None):
        kw = {}
        if bias is not None:
            kw['bias'] = bias
        if scale is not None:
            kw['scale'] = scale
        if accum is not None:
            kw['accum_out'] = accum
        r = [in_] + [a for a in (bias, scale) if a is not None and not isinstance(a, (int, float))]
        w = (list(wt) if wt else [out]) + ([accum] if accum is not None else [])
        c = 0.22 + in_.free_size() / 1400.0
        self.op('act', lambda e: e.activation(out=out, in_=in_, func=func, **kw), r, w, c, ASET.get(func))

    def _vc(self, eng, n, f=1.0):
        f = 1.0 + (f - 1.0) * 0.3
        return (0.07 + f * n / 960.0) if eng == 'dve' else (0.3 + n / 300.0)

    def ts(self, eng, out, in0, s1, s2=None, op0=ALU.mult, op1=None, accum=None, wt=None):
        kw = {}
        if op1 is not None:
            kw['op1'] = op1
        if accum is not None:
            kw['accum_out'] = accum
        r = [in0] + [a for a in (s1, s2) if a is not None and not isinstance(a, (int, float))]
        w = (list(wt) if wt else [out]) + ([accum] if accum is not None else [])
        self.op(eng, lambda e: e.tensor_scalar(out=out, in0=in0, scalar1=s1, scalar2=s2, op0=op0, **kw), r, w,
                self._vc(eng, in0.free_size()))

    def tt(self, eng, out, in0, in1, op):
        self.op(eng, lambda e: e.tensor_tensor(out=out, in0=in0, in1=in1, op=op), [in0, in1], [out],
                self._vc(eng, in0.free_size(), 1.5))

    def stt(self, out, in0, scalar, in1, op0, op1):
        r = [in0, in1] + ([scalar] if not isinstance(scalar, (int, float)) else [])
        self.op('dve', lambda e: e.scalar_tensor_tensor(out=out, in0=in0, scalar=scalar, in1=in1, op0=op0, op1=op1), r, [out],
                self._vc('dve', in0.free_size(), 1.5))

    def cp(self, eng, out, in_):
        if eng == 'act':
            self.act(out, in_, AF.Copy)
        else:
            self.op(eng, lambda e: e.tensor_copy(out=out, in_=in_), [in_], [out], self._vc(eng, in_.free_size()))

    def ms(self, eng, ap, val):
        self.op(eng, lambda e: e.memset(ap, val), [], [ap], self._vc(eng, ap.free_size()))

    def scan(self, out, d0, d1, init):
        r = [d0, d1] + ([init] if not isinstance(init, (int, float)) else [])
        self.op('dve', lambda e: e.tensor_tensor_scan(out=out, data0=d0, data1=d1, initial=init, op0=ALU.mult, op1=ALU.add), r, [out],
                self._vc('dve', d0.free_size(), 2.0))

    def red(self, out, in_, op):
        self.op('dve', lambda e: e.tensor_reduce(out=out, in_=in_, axis=AX.X, op=op), [in_], [out], self._vc('dve', in_.free_size()))

    def recip(self, out, in_):
        self.op('dve', lambda e: e.reciprocal(out=out, in_=in_), [in_], [out], self._vc('dve', in_.free_size(), 8.0))

    def ld(self, out, in_, eng='sp', wt=None, rt=None, **kw):
        c = 2.0 + out.nbytes() / 150000.0
        self.dma(eng, lambda e: e.dma_start(out=out, in_=in_, **kw), [in_] + list(rt or []), list(wt) if wt else [out], c)

    def gather(self, out, table, idx):
        self.dma('pool', lambda e: e.indirect_dma_start(out=out, out_offset=None, in_=table,
                                                        in_offset=bass.IndirectOffsetOnAxis(ap=idx, axis=0)),
                 [table, idx], [out], 3.0 + out.nbytes() / 100000.0)

    def scatter_add(self, table, idx, in_, rt=None, wt=None):
        self.dma('pool', lambda e: e.indirect_dma_start(out=table, out_offset=bass.IndirectOffsetOnAxis(ap=idx, axis=0),
                                                        in_=in_, in_offset=None, compute_op=ALU.add),
                 [idx, in_] + (list(rt) if rt is not None else [table]), list(wt) if wt else [table], 4.0 + in_.nbytes() / 80000.0)


class MultiRot:
    def __init__(self, pools):
        self.pools = pools
        self.cur = 0

    def set(self, j):
        self.cur = j % len(self.pools)

    def next(self):
        return self.pools[self.cur].next()


class Rot:
    def __init__(self, items):
        self.items = items
        self.i = 0

    def next(self):
        t = self.items[self.i]
        self.i = (self.i + 1) % len(self.items)
        return t


def host_consts():
    bf = ml_dtypes.bfloat16
    k = {}
    k['k_ident_f'] = np.eye(128, dtype=np.float32)
    k['k_ident_b'] = np.eye(128).astype(bf)
    k['k_ones_b'] = np.ones((128, 128)).astype(bf)
    k['k_ones_f'] = np.ones((128, 128), np.float32)
    k['k_triu'] = np.triu(np.ones((128, 128), np.float32), 1)
    sel = np.zeros((2, 256), np.float32)
    sel[0, :128] = 1
    sel[1, 128:] = 1
    k['k_sel'] = sel
    k['k_iota_c'] = np.tile(np.arange(1024, dtype=np.float32)[None], (128, 1))
    k['k_cidx'] = (np.arange(8)[None, :] * 128 + np.arange(128)[:, None]).astype(np.float32)
    k['k_tstart'] = (np.arange(128) * 128).astype(np.float32)[:, None].copy()
    scale = 1.0 / np.sqrt(8192.0 * 64.0)
    cc = np.arange(64)
    ang = 2 * np.pi * np.outer(cc, cc) / 64.0
    bd = np.zeros((2, 128, 512), np.float64)
    for q in range(2):
        for gl in range(2):
            r0 = gl * 64
            c0 = q * 128 + gl * 64
            bd[q, r0:r0 + 64, c0:c0 + 64] = np.cos(ang) * scale
            bd[q, r0:r0 + 64, 256 + c0:256 + c0 + 64] = -np.sin(ang) * scale
    k['k_bd'] = np.ascontiguousarray(bd.transpose(1, 0, 2)).astype(bf)
    n1 = np.arange(128)
    a1 = 2 * np.pi * np.outer(n1, n1) / 128.0
    C1, S1 = np.cos(a1), np.sin(a1)
    cs = np.stack([np.concatenate([C1, -S1], 1), np.concatenate([S1, C1], 1)], 1)
    k['k_cs1'] = cs.astype(bf)
    n2 = np.arange(64)
    k1 = np.arange(128)
    k2 = np.arange(64)
    kk = k1[:, None] + 128 * k2[None, :]
    a3 = 2 * np.pi * n2[:, None, None] * kk[None] / 8192.0
    e3 = np.stack([np.cos(a3), np.sin(a3)], 2)
    k['k_e3'] = np.concatenate([e3, e3], 0).astype(bf)
    n = np.arange(256)
    ac = 2 * np.pi * np.outer(n, n) / 256.0
    f = np.sqrt(32.0)
    dc = np.stack([np.cos(ac) * f, np.sin(ac) * f], 1)
    k['k_dc'] = np.ascontiguousarray(dc.reshape(2, 128, 2, 256).transpose(1, 0, 2, 3)).astype(bf)
    return k


CONST_SHAPES = {
    'k_ident_f': ([128, 128], F32), 'k_ident_b': ([128, 128], BF16), 'k_ones_b': ([128, 128], BF16),
    'k_ones_f': ([128, 128], F32), 'k_triu': ([128, 128], F32), 'k_sel': ([2, 256], F32),
    'k_iota_c': ([128, 1024], F32), 'k_cidx': ([128, 8], F32), 'k_tstart': ([128, 1], F32),
    'k_bd': ([128, 2, 512], BF16), 'k_cs1': ([128, 2, 256], BF16), 'k_e3': ([128, 128, 2, 64], BF16),
    'k_dc': ([128, 2, 2, 256], BF16),
}

IN_SHAPES = {
    'x': [T_X, 1024], 'c': [1, 1024], 'ctx': [T_C, 1024], 'c_ctx': [1, 1024],
    'w_ada': [2, 1024, 6144], 'b_ada': [2, 6144], 'g_norm1': [2, 1024], 'w_in': [2, 1024, 2048],
    'lru_conv_w': [2, 4, 512], 'lru_conv_b': [2, 512], 'lru_wr': [2, 2, 8, 64, 64], 'lru_br': [2, 2, 512],
    'lru_wi': [2, 2, 8, 64, 64], 'lru_bi': [2, 2, 512], 'lru_lam': [2, 2, 512], 'sc_conv_w': [2, 3, 256],
    'g_out': [2, 1024], 'w_out': [2, 1024, 1024], 'g_norm2': [2, 1024], 'w_router': [2, 1024, 16],
    'w_gate_e': [2, 16, 1024, 1024], 'w_up_e': [2, 16, 1024, 1024], 'w_down_e': [2, 16, 1024, 1024],
    'g_final': [1, 1024],
}

C_G1, C_CW, C_CB, C_BR, C_BI, C_LAM, C_SCW, C_GOUT, C_NROWS = 0, 8, 24, 28, 36, 44, 52, 58, 66


def build(depth=2, stop_after=None, dbg=None):
    nc = bass.Bass("TRN2", target_bir_lowering=False)
    P = Prog(nc)
    D = {}
    for name, shp in IN_SHAPES.items():
        D[name] = nc.dram_tensor(name, shp, F32, kind="ExternalInput").ap()
    K = {}
    for name, (shp, dt) in CONST_SHAPES.items():
        K[name] = nc.dram_tensor(name, shp, dt, kind="ExternalInput").ap()
    out_d = nc.dram_tensor("out", [T_X, 1024], F32, kind="ExternalOutput").ap()
    xres = [nc.dram_tensor("xres%d" % i, [NTOK, 1024], F32, kind="Internal").ap() for i in range(2)]
    hx2ext = nc.dram_tensor("hx2ext", [NTOK, 1056], BF16, kind="Internal").ap()
    Vd = nc.dram_tensor("Vd", [2, NTOK, 256], BF16, kind="Internal").ap()
    hbd = nc.dram_tensor("hbd", [4, 128, NTOK], BF16, kind="Internal").ap()
    yfd = nc.dram_tensor("yfd", [2, 128, NTOK], BF16, kind="Internal").ap()
    affTd = nc.dram_tensor("affTd", [16, NTOK], F32, kind="Internal").ap()
    hxd = nc.dram_tensor("hxd", [8, 128, NTOK], BF16, kind="Internal").ap()
    dbg_out = {}
    if dbg:
        for name, shp in dbg.items():
            dbg_out[name] = nc.dram_tensor("dbg_" + name, shp, F32, kind="ExternalOutput").ap()

    ident_f = P.sb("ident_f", [128, 128], F32)
    ident_b = P.sb("ident_b", [128, 128], BF16)
    ones_b = P.sb("ones_b", [128, 128], BF16)
    ones_f = P.sb("ones_f", [128, 128], F32)
    triu = P.sb("triu", [128, 128], F32)
    sel = P.sb("sel", [2, 256], F32)
    cidx = P.sb("cidx", [128, 8], F32)
    tstart = P.sb("tstart", [128, 1], F32)
    bd = P.sb("bd", [128, 2, 512], BF16)
    cs1 = P.sb("cs1", [128, 2, 256], BF16)
    dc = P.sb("dc", [128, 2, 2, 256], BF16)
    for t, n in ((ident_f, 'k_ident_f'), (ident_b, 'k_ident_b'), (ones_b, 'k_ones_b'), (ones_f, 'k_ones_f'),
                 (triu, 'k_triu'), (sel, 'k_sel'), (cidx, 'k_cidx'), (tstart, 'k_tstart'), (bd, 'k_bd'),
                 (cs1, 'k_cs1'), (dc, 'k_dc')):
        P.ld(t[:], K[n])
    wr_b = P.sb("wr_b", [128, 8, 16], BF16)
    gates_b = P.sb("gates_b", [128, 2, 2, 4, 128], BF16)
    bcn = ['sh2', 'gs2', 'gate1', 'gate2']
    BC = {}
    bcd = nc.dram_tensor("bcd", [8, 128, 1024], F32, kind="Internal").ap()

    def bc_load(n_, v_, st_, rows=128):
        t_ = P.sb("bc_%s%d" % (n_, v_), [128, 1024], F32, st_)
        P.ld(t_[0:rows, :], bcd[bcn.index(n_) * 2 + v_, 0:rows, :])
        BC[(n_, v_)] = t_
    CL = P.sb("CL", [128, C_NROWS], F32)
    nsp = P.sb("nsp", [128, 8], F32)
    gs1 = P.sb("gs1", [128, 8, 2], F32)
    sh1 = P.sb("sh1", [128, 8, 2], F32)
    st_f = P.sb("st_f", [128, 4], F32)
    st_b = P.sb("st_b", [128, 4], F32)
    idx_all = P.sb("idx_all", [128, 16, 9], I32)
    val_all = P.sb("val_all", [128, 16, 9], F32)
    psA_banks = [P.ps("psA%d" % i, [128, 512], F32) for i in range(PS_A)]
    psA = Rot(psA_banks)
    psA_all, psA_lo, psA_hi = psA, Rot(psA_banks[0:PS_A - 2]), Rot(psA_banks[PS_A - 2:PS_A])
    psT = Rot([P.ps("psT%d" % i, [128, 1024], BF16) for i in range(PS_T)])
    psS = Rot([P.ps("psS%d" % i, [128, 512], F32) for i in range(PS_S)])

    cast_rr = Rot(['act', 'dve'])

    colpool = {}

    def col(st):
        if not hasattr(st, '_colpool'):
            st._colpool = Rot([P.sb("col%d" % i, [128, 1], F32, st) for i in range(32)])
        return st._colpool.next()

    def rsqrt_col(out, ss, n, st):
        t = col(st)
        P.ts('dve', t[:], ss, 1.0 / n, EPS, op0=ALU.mult, op1=ALU.add)
        P.act(t[:], t[:], AF.Ln)
        P.act(out, t[:], AF.Exp, scale=-0.5)

    def dump(name, ap_src, rows=None):
        if name in dbg_out:
            P.ld(dbg_out[name] if rows is None else dbg_out[name][rows], ap_src)

    for l in range(depth):
        last = (l == depth - 1)
        xin = D['x'] if l == 0 else xres[(l - 1) % 2][0:T_X, :]
        cin = D['ctx'] if l == 0 else xres[(l - 1) % 2][T_X:NTOK, :]
        xo = xres[l % 2]
        with ExitStack() as st:
            stg = Rot([P.sb("stg%d" % i, [128, 4096], F32, st) for i in range(2)])
            rows = P.sb("rows", [C_NROWS, 128], F32, st)
            P.ld(rows[C_G1:C_G1 + 8, :], D['g_norm1'][l].rearrange("(j p) -> j p", p=128))
            P.ld(rows[C_CW:C_CW + 16, :], D['lru_conv_w'][l].rearrange("k (j p) -> (k j) p", p=128))
            P.ld(rows[C_CB:C_CB + 4, :], D['lru_conv_b'][l].rearrange("(j p) -> j p", p=128))
            P.ld(rows[C_BR:C_BR + 8, :], D['lru_br'][l].rearrange("d (j p) -> (d j) p", p=128))
            P.ld(rows[C_BI:C_BI + 8, :], D['lru_bi'][l].rearrange("d (j p) -> (d j) p", p=128))
            P.ld(rows[C_LAM:C_LAM + 8, :], D['lru_lam'][l].rearrange("d (j p) -> (d j) p", p=128))
            P.ld(rows[C_SCW:C_SCW + 6, :], D['sc_conv_w'][l].rearrange("k (j p) -> (k j) p", p=128))
            P.ld(rows[C_GOUT:C_GOUT + 8, :], D['g_out'][l].rearrange("(j p) -> j p", p=128))
            pss = psS.next()
            P.tr(pss[:, 0:C_NROWS], rows[:, :], ident_f[0:C_NROWS, 0:C_NROWS])
            P.cp('dve', CL[:, :], pss[:, 0:C_NROWS])
            tmp8 = P.sb("tmp8", [128, 8], F32, st)
            P.act(tmp8[:], CL[:, C_LAM:C_LAM + 8], AF.Exp, scale=-1.0)
            P.act(tmp8[:], tmp8[:], AF.Ln, bias=1.0)
            P.ts('dve', nsp[:], tmp8[:], -8.0)
            gst = P.sb("gst", [128, 2, 2, 4, 128], F32, st)
            P.ms('pool', gst[:], 0.0)
            for d in range(2):
                for kind, nm_ in ((0, 'lru_wr'), (1, 'lru_wi')):
                    src = D[nm_][l, d].rearrange("(j two) i o -> two i j o", two=2)
                    P.ld(gst[0:64, d, kind, :, 0:64], src[0])
                    P.ld(gst[64:128, d, kind, :, 64:128], src[1])
            P.cp('pool', gates_b[:], gst[:])
            crow = P.sb("crow", [2, 1024], F32, st)
            P.ld(crow[0:1, :], D['c'])
            P.ld(crow[1:2, :], D['c_ctx'])
            pss = psS.next()
            for j in range(8):
                P.tr(pss[:, 2 * j:2 * j + 2], crow[0:2, j * 128:(j + 1) * 128], ident_f[0:2, 0:2])
            cvec = P.sb("cvec", [128, 8, 2], F32, st)
            P.act(cvec[:].rearrange("p k v -> p (k v)"), pss[:, 0:16], AF.Silu)
            badar = P.sb("badar", [2, 6144], F32, st)
            P.ld(badar[0:1, :], D['b_ada'][l:l + 1, :])
            P.ld(badar[1:2, :], D['b_ada'][l:l + 1, :])
            mods = P.sb("mods", [2, 6144], F32, st)
            for nb in range(12):
                s = stg.next()
                sv = s[:].rearrange("p (k n) -> p k n", k=8)
                P.ld(sv, D['w_ada'][l][:, nb * 512:(nb + 1) * 512].rearrange("(k p) n -> p k n", p=128))
                ps = psA.next()
                for k in range(8):
                    P.mm(ps[0:2, :], cvec[:, k, :], sv[:, k, :], start=(k == 0), stop=(k == 7))
                P.tt('dve', mods[0:2, nb * 512:(nb + 1) * 512], ps[0:2, :], badar[0:2, nb * 512:(nb + 1) * 512], ALU.add)
            pss = psS.next()
            for q in range(2):
                for j in range(8):
                    o = (q * 8 + j) * 2
                    P.tr(pss[:, o:o + 2], mods[0:2, q * 1024 + j * 128:q * 1024 + (j + 1) * 128], ident_f[0:2, 0:2])
            modc = P.sb("modc", [128, 2, 8, 2], F32, st)
            P.cp('dve', modc[:].rearrange("p q k v -> p (q k v)"), pss[:, 0:32])
            for v in range(2):
                P.stt(gs1[:, :, v], modc[:, 1, :, v], 1.0, CL[:, C_G1:C_G1 + 8], ALU.add, ALU.mult)
                P.cp('dve', sh1[:, :, v], modc[:, 0, :, v])
            g2r = P.sb("g2r", [2, 1024], F32, st)
            P.ld(g2r[0:1, :], D['g_norm2'][l:l + 1, :])
            P.ld(g2r[1:2, :], D['g_norm2'][l:l + 1, :])
            g2bc = P.sb("g2bc", [128, 1024], F32, st)

            def bcast(dst, src_rows, c0, v):
                for h in range(2):
                    ps = psA.next()
                    P.mm(ps[:, :], sel[0:2, v * 128:(v + 1) * 128], src_rows[0:2, c0 + h * 512:c0 + (h + 1) * 512])
                    P.cp('act', dst[:, h * 512:(h + 1) * 512], ps[:, :])
            bcast(g2bc, g2r, 0, 0)
            for n_ in bcn:
                for v in range(2):
                    BC[(n_, v)] = P.sb("bcp_%s%d" % (n_, v), [128, 1024], F32, st)
            for v in range(2):
                bcast(BC[('gate1', v)], mods, 2 * 1024, v)
                bcast(BC[('sh2', v)], mods, 3 * 1024, v)
                bcast(BC[('gs2', v)], mods, 4 * 1024, v)
                bcast(BC[('gate2', v)], mods, 5 * 1024, v)
                P.stt(BC[('gs2', v)][:], BC[('gs2', v)][:], 1.0, g2bc[:], ALU.add, ALU.mult)
            for n_ in bcn:
                for v in range(2):
                    P.ld(bcd[bcn.index(n_) * 2 + v], BC[(n_, v)][:])
            P.barrier()
            P.emit()
        lst = ExitStack()
        Win = P.sb("Win", [128, 8, 2048], BF16, lst)
        with ExitStack() as st:
            stg = Rot([P.sb("stg%d" % i, [128, 4096], F32, st) for i in range(2)])
            for kk in range(4):
                s = stg.next()
                sv = s[:].rearrange("p (k n) -> p k n", k=2)
                P.ld(sv, D['w_in'][l][kk * 256:(kk + 1) * 256, :].rearrange("(k p) n -> p k n", p=128))
                for k2 in range(2):
                    P.cp(cast_rr.next(), Win[:, kk * 2 + k2, :], sv[:, k2, :])
            wrs = P.sb("wrs", [128, 8, 16], F32, st)
            P.ld(wrs[:], D['w_router'][l].rearrange("(k p) e -> p k e", p=128))
            P.cp('dve', wr_b[:], wrs[:])
            P.barrier()
            P.emit()
        if stop_after == ('prep', l):
            break

        def prep_wfft(st_):
            Wf = P.sb("Wfft", [128, 8, 512], BF16, st_)
            WT = [P.sb("WT%d" % q, [128, 1024], BF16, st_) for q in range(2)]
            for q in range(2):
                pt = psT.next()
                for k in range(8):
                    P.tr(pt[:, k * 128:(k + 1) * 128], Win[:, k, 1792 + q * 128:1792 + (q + 1) * 128], ident_b[:, :])
                P.cp('act', WT[q][:], pt[:])
            for k in range(8):
                ps = psA.next()
                for q in range(2):
                    P.mm(ps[:, :], WT[q][:, k * 128:(k + 1) * 128], bd[:, q, :], start=(q == 0), stop=(q == 1))
                P.cp('dve', Wf[:, k, :], ps[:, :])
            return Wf

        def prep_wout(st_):
            Wo = P.sb("Wout", [128, 8, 1024], BF16, st_)
            for kk in range(2):
                P.ld(Wo[:, kk * 4:(kk + 1) * 4, :], D['w_out'][l][kk * 512:(kk + 1) * 512, :].rearrange("(k p) n -> p k n", p=128),
                     eng='pool')
            for k in range(8):
                P.act(Wo[:, k, :], Wo[:, k, :], AF.Copy, scale=CL[:, C_GOUT + k:C_GOUT + k + 1])
            return Wo

        def load_block(st, src, tok0, TB, v, hxT):
            for i in range(TB // 128):
                xt = xt_rot.next()
                P.ld(xt[:], src[tok0 + i * 128:tok0 + (i + 1) * 128, :])
                junk = junk_rot.next()
                ss = col(st)
                P.act(junk[:], xt[:], AF.Square, accum=ss[:])
                rstd = col(st)
                rsqrt_col(rstd[:], ss[:], 1024.0, st)
                xn = xn_rot.next()
                P.act(xn[:], xt[:], AF.Copy, scale=rstd[:, 0:1])
                pt = psT.next()
                for k in range(8):
                    P.tr(pt[:, k * 128:(k + 1) * 128], xn[:, k * 128:(k + 1) * 128], ident_b[:, :])
                for k in range(8):
                    o = hxT[:, k, i * 128:(i + 1) * 128]
                    wt = ["%s:t%d:%d" % (hxT.name, i, k % 2)]
                    if i % 2 == 0:
                        P.act(o, pt[:, k * 128:(k + 1) * 128], AF.Identity, scale=gs1[:, k, v:v + 1], bias=sh1[:, k, v:v + 1], wt=wt)
                    else:
                        P.ts('dve', o, pt[:, k * 128:(k + 1) * 128], gs1[:, k, v:v + 1], sh1[:, k, v:v + 1], op0=ALU.mult, op1=ALU.add, wt=wt)

        def inproj(hxT, TB, m):
            ps = psA.next()
            rt = ["%s:t%d:%d" % (hxT.name, i, p_) for i in range(TB // 128) for p_ in range(2)]
            for k in range(8):
                P.mm(ps[:, 0:TB], Win[:, k, m * 128:(m + 1) * 128], hxT[:, k, 0:TB], start=(k == 0), stop=(k == 7), rt=rt)
            return ps

        def conv_taps(u, p, w_cols, b_col, TB, RW, left):
            u3 = u.rearrange("p (r w) -> p r w", w=RW)
            p3 = p.rearrange("p (r w) -> p r w", w=RW)
            if b_col is None:
                P.act(u, p, AF.Copy, scale=w_cols[left])
            else:
                P.act(u, p, AF.Identity, scale=w_cols[left], bias=b_col)
            for kx in range(len(w_cols)):
                off = kx - left
                if off == 0:
                    continue
                if off < 0:
                    o, i_ = u3[:, :, -off:RW], p3[:, :, 0:RW + off]
                else:
                    o, i_ = u3[:, :, 0:RW - off], p3[:, :, off:RW]
                P.stt(o, i_, w_cols[kx], o, ALU.mult, ALU.add)

        def lru_dir(st, d, u, ub, TB, j, init, h_out, reverse):
            psr = psA.next()
            P.mm(psr[:, 0:TB], gates_b[:, d, 0, j, :], ub, start=True, stop=True)
            psi = psA.next()
            P.mm(psi[:, 0:TB], gates_b[:, d, 1, j, :], ub, start=True, stop=True)
            a = lt_rot.next()
            P.act(a[:, 0:TB], psr[:, 0:TB], AF.Sigmoid, bias=CL[:, C_BR + d * 4 + j:C_BR + d * 4 + j + 1])
            ig = lt_rot.next()
            P.act(ig[:, 0:TB], psi[:, 0:TB], AF.Sigmoid, bias=CL[:, C_BI + d * 4 + j:C_BI + d * 4 + j + 1])
            P.act(a[:, 0:TB], a[:, 0:TB], AF.Exp, scale=nsp[:, d * 4 + j:d * 4 + j + 1])
            sq = lt_rot.next()
            P.act(sq[:, 0:TB], a[:, 0:TB], AF.Square)
            P.act(sq[:, 0:TB], sq[:, 0:TB], AF.Ln, scale=-1.0, bias=1.0)
            P.act(sq[:, 0:TB], sq[:, 0:TB], AF.Exp, scale=0.5)
            P.tt('dve', ig[:, 0:TB], ig[:, 0:TB], sq[:, 0:TB], ALU.mult)
            P.tt('dve', ig[:, 0:TB], ig[:, 0:TB], u, ALU.mult)
            if reverse:
                P.scan(h_out[:, ::-1], a[:, 0:TB][:, ::-1], ig[:, 0:TB][:, ::-1], init)
            else:
                P.scan(h_out, a[:, 0:TB], ig[:, 0:TB], init)

        def lru_front(st, hxT, TB, RW, j):
            ps = inproj(hxT, TB, j)
            p = lt_rot.next()
            P.cp('act', p[:, 0:TB], ps[:, 0:TB])
            u = lt_rot.next()
            conv_taps(u[:, 0:TB], p[:, 0:TB], [CL[:, C_CW + kx * 4 + j:C_CW + kx * 4 + j + 1] for kx in range(4)],
                      CL[:, C_CB + j:C_CB + j + 1], TB, RW, 1)
            ub = ub_rot.next()
            P.cp('act', ub[:, 0:TB], u[:, 0:TB])
            return u, ub

        def group_norm(st, ys, TB, nch):
            ps = psA.next()
            for i, y in enumerate(ys):
                sq = sqb_rot.next()
                P.act(sq[:, 0:TB], y, AF.Square)
                P.mm(ps[:, 0:TB], ones_b[:, :], sq[:, 0:TB], start=(i == 0), stop=(i == len(ys) - 1))
            r = lt_rot.next()
            P.act(r[:, 0:TB], ps[:, 0:TB], AF.Ln, scale=1.0 / nch, bias=EPS)
            P.act(r[:, 0:TB], r[:, 0:TB], AF.Exp, scale=-0.5)
            return r

        for (v, src, T, TB, RW, off, full) in ((1, cin, T_C, 256, 256, T_X, not last), (0, xin, T_X, 512, 64, 0, True)):
            nblk = T // TB
            with ExitStack() as st:
                xt_rot = Rot([P.sb("xt%d" % i, [128, 1024], F32, st) for i in range(2)])
                junk_rot = Rot([P.sb("junk%d" % i, [128, 1024], BF16, st) for i in range(1)])
                xn_rot = Rot([P.sb("xn%d" % i, [128, 1024], BF16, st) for i in range(1)])
                hx_rot = Rot([P.sb("hxT%d" % i, [128, 8, 512], BF16, st) for i in range(2)])
                lt_rot = Rot([P.sb("lt%d" % i, [128, TB], F32, st) for i in range(7)])
                ub_rot = Rot([P.sb("ub%d" % i, [128, 512], BF16, st) for i in range(1)])
                sqb_rot = Rot([P.sb("sqb%d" % i, [128, 512], BF16, st) for i in range(2)])
                hbb_rot = Rot([P.sb("hbb%d" % i, [128, 512], BF16, st) for i in range(1)])
                if full:
                    Wfft = prep_wfft(st)
                    for b in range(nblk):
                        hxT = hx_rot.next()
                        load_block(st, src, b * TB, TB, v, hxT)
                        P.ld(hxd[:, :, off + b * TB:off + (b + 1) * TB].rearrange("k p t -> p k t"), hxT[:, :, 0:TB],
                             wt=["hxd:%d" % b], rt=["%s:t%d:%d" % (hxT.name, i, p_) for i in range(TB // 128) for p_ in range(2)])
                        for i in range(TB // 128):
                            ps = psA.next()
                            for k in range(8):
                                P.mm(ps[:, :], hxT[:, k, i * 128:(i + 1) * 128], Wfft[:, k, :], start=(k == 0), stop=(k == 7),
                                     rt=["%s:t%d:%d" % (hxT.name, i, p_) for p_ in range(2)])
                            vt = sqb_rot.next()
                            P.cp('act', vt[:, :], ps[:, :])
                            r_ = slice(off + b * TB + i * 128, off + b * TB + (i + 1) * 128)
                            for q in range(2):
                                P.ld(Vd[q, r_, :].rearrange("t (part c) -> t part c", part=2),
                                     vt[:, :].rearrange("t (part qc) -> t part qc", part=2)[:, :, q * 128:(q + 1) * 128],
                                     wt=["Vd:%d:%d:%d" % (b, i, q)])
                m0 = P.mark()
                if full:
                    psA = psA_lo
                P.ms('dve', st_b[:], 0.0) if v == 1 else None
                if v == 1:
                    P.ms('dve', st_f[:], 0.0)
                def fetch_block(hxT, b):
                    wt = ["%s:t%d:%d" % (hxT.name, i, p_) for i in range(TB // 128) for p_ in range(2)]
                    P.dma('sp', lambda e, o_=hxT[:, :, 0:TB], i_=hxd[:, :, off + b * TB:off + (b + 1) * TB].rearrange("k p t -> p k t"):
                          e.dma_start(out=o_, in_=i_), ["hxd:%d" % b], wt, 2.0 + TB * 16 * 128 / 150000.0)
                for b in reversed(range(nblk)):
                    hxT = hx_rot.next()
                    if full:
                        fetch_block(hxT, b)
                    else:
                        load_block(st, src, b * TB, TB, v, hxT)
                    for j in range(4):
                        u, ub = lru_front(st, hxT, TB, RW, j)
                        h = lt_rot.next()
                        lru_dir(st, 1, u[:, 0:TB], ub[:, 0:TB], TB, j, st_b[:, j:j + 1], h[:, 0:TB], True)
                        stn = col(st)
                        P.cp('dve', stn[:], h[:, 0:1])
                        P.cp('dve', st_b[:, j:j + 1], stn[:])
                        if full:
                            hb = hbb_rot.next()
                            P.cp('act', hb[:, 0:TB], h[:, 0:TB])
                            P.ld(hbd[j, :, off + b * TB:off + (b + 1) * TB], hb[:, 0:TB], wt=["hbd:%d:%d" % (b, j)])
                m1 = P.mark()
                if full:
                    psA = psA_hi
                    yraw = [P.sb("yraw%d" % q, [128, T], BF16, st) for q in range(2)]
                    if v == 1:
                        Vc = P.sb("Vc", [128, 2, 2, 256], BF16, st)
                        for q in range(2):
                            P.ld(Vc[:, q], Vd[q, off:off + T, :].rearrange("(i p) n -> p i n", p=128),
                                 rt=["Vd:%d:%d:%d" % (b_, i_, q) for b_ in range(nblk) for i_ in range(TB // 128)])
                        for q in range(2):
                            ps = psA.next()
                            first = True
                            for i in range(2):
                                for part in range(2):
                                    P.mm(ps[:, 0:256], Vc[:, q, i, part * 128:(part + 1) * 128], dc[:, i, part, :],
                                         start=first, stop=(i == 1 and part == 1))
                                    first = False
                            P.cp('act', yraw[q][:, :], ps[:, 0:256])
                    else:
                        e3_rot = Rot([P.sb("e3t%d" % i, [128, 8, 2, 64], BF16, st) for i in range(1)])
                        Vq = P.sb("Vq", [128, 64, 2, 128], BF16, st)
                        Bq = P.sb("Bq", [128, 64, 256], BF16, st)
                        for q in range(2):
                            P.ld(Vq[:].rearrange("p n part c -> p (n part c)"),
                                 Vd[q, 0:T_X, :].rearrange("(n1 n2) pc -> n1 (n2 pc)", n2=64),
                                 rt=["Vd:%d:%d:%d" % (b_, i_, q) for b_ in range(nblk) for i_ in range(TB // 128)])
                            for cp2 in range(32):
                                ps = psA.next()
                                for c_ in range(2):
                                    cp_ = cp2 * 2 + c_
                                    for c2 in range(2):
                                        for part in range(2):
                                            lhsT = Vq[:, :, part, cp_ + 64 * c2]
                                            P.mm(ps[c2 * 64:(c2 + 1) * 64, c_ * 256:(c_ + 1) * 256], lhsT, cs1[:, part, :],
                                                 start=(part == 0), stop=(part == 1))
                                P.cp('act' if cp2 % 2 == 0 else 'dve', Bq[:, cp2 * 2:cp2 * 2 + 2, :].rearrange("p a b -> p (a b)"), ps[:, :])
                            for kb in range(16):
                                e3t = e3_rot.next()
                                P.ld(e3t[:], K['k_e3'][:, kb * 8:(kb + 1) * 8, :, :])
                                ps = psA.next()
                                for k1l in range(8):
                                    k1 = kb * 8 + k1l
                                    for c2 in range(2):
                                        pr = slice(c2 * 64, (c2 + 1) * 64)
                                        for part in range(2):
                                            P.mm(ps[pr, k1l * 64:(k1l + 1) * 64], Bq[pr, :, part * 128 + k1], e3t[pr, k1l, part, :],
                                                 start=(part == 0), stop=(part == 1))
                                o = yraw[q][:, :].rearrange("p (k2 k1) -> p k1 k2", k1=128)[:, kb * 8:(kb + 1) * 8, :]
                                P.cp('act' if kb % 2 == 0 else 'dve', o, ps[:, :].rearrange("p (a b) -> p a b", a=8))
                    NB = 512 if v == 0 else 256
                    sqf_rot = Rot([P.sb("sqf%d" % i, [128, 512], BF16, st) for i in range(2)])
                    rf_rot = Rot([P.sb("rf%d" % i, [128, 512], F32, st) for i in range(1)])
                    for b in range(T // NB):
                        ps = psA.next()
                        for q in range(2):
                            sq = sqf_rot.next()
                            P.act(sq[:, 0:NB], yraw[q][:, b * NB:(b + 1) * NB], AF.Square)
                            P.mm(ps[:, 0:NB], ones_b[:, :], sq[:, 0:NB], start=(q == 0), stop=(q == 1))
                        r = rf_rot.next()
                        P.act(r[:, 0:NB], ps[:, 0:NB], AF.Ln, scale=1.0 / 256, bias=EPS)
                        P.act(r[:, 0:NB], r[:, 0:NB], AF.Exp, scale=-0.5)
                        for q in range(2):
                            yo = sqf_rot.next()
                            P.tt('dve', yo[:, 0:NB], yraw[q][:, b * NB:(b + 1) * NB], r[:, 0:NB], ALU.mult)
                            P.ld(yfd[q, :, off + b * NB:off + (b + 1) * NB], yo[:, 0:NB], wt=["yfd:%d:%d" % (b, q)])
                    psA = psA_all
                    if INTERLEAVE:
                        P.interleave(m0, m1, P.mark())
                P.barrier()
                P.emit()
            if not full:
                with ExitStack() as st:
                    xt_rot = Rot([P.sb("xt%d" % i, [128, 1024], F32, st) for i in range(2)])
                    junk_rot = Rot([P.sb("junk%d" % i, [128, 1024], BF16, st) for i in range(1)])
                    xn_rot = Rot([P.sb("xn%d" % i, [128, 1024], BF16, st) for i in range(2)])
                    hx_rot = Rot([P.sb("hxT%d" % i, [128, 8, 512], BF16, st) for i in range(1)])
                    lt_rot = Rot([P.sb("lt%d" % i, [128, 512], F32, st) for i in range(10)])
                    ub_rot = Rot([P.sb("ub%d" % i, [128, 512], BF16, st) for i in range(2)])
                    hxT = hx_rot.next()
                    load_block(st, src, 0, TB, v, hxT)
                    for j in range(4):
                        u, ub = lru_front(st, hxT, TB, RW, j)
                        h = lt_rot.next()
                        lru_dir(st, 0, u[:, 0:TB], ub[:, 0:TB], TB, j, 0.0, h[:, 0:TB], False)
                        P.cp('dve', st_f[:, j:j + 1], h[:, TB - 1:TB])
                    P.barrier()
                    P.emit()
                continue
            with ExitStack() as st:
                xt_rot = Rot([P.sb("xt%d" % i, [128, 1024], F32, st) for i in range(2)])
                junk_rot = Rot([P.sb("junk%d" % i, [128, 1024], BF16, st) for i in range(1)])
                hx_rot = Rot([P.sb("hxT%d" % i, [128, 8, 512], BF16, st) for i in range(2)])
                lt_rot = MultiRot([Rot([P.sb("lt%d_%d" % (pp, i), [128, TB], F32, st) for i in range(LT_DEPTH)]) for pp in range(LT_POOLS)])
                Wout = prep_wout(st)
                for n_ in ('gate1', 'sh2', 'gs2'):
                    bc_load(n_, v, st)
                ylru_tt = [P.sb("ylru_t%d" % i, [128, 4, 512], F32, st) for i in range(YL_BUFS)]
                ysc_tt = [P.sb("ysc_t%d" % i, [128, 2, 512], F32, st) for i in range(YL_BUFS)]
                ub_rot = Rot([P.sb("ub%d" % i, [128, 512], BF16, st) for i in range(2)])
                sqb_rot = Rot([P.sb("sqb%d" % i, [128, 512], BF16, st) for i in range(2)])
                hbb_rot = Rot([P.sb("hbb%d" % i, [128, 512], BF16, st) for i in range(2)])
                ynb_rot = Rot([P.sb("ynb%d" % i, [128, 8, 512], BF16, st) for i in range(2)])
                x1_rot = Rot([P.sb("x1t%d" % i, [128, 1024], F32, st) for i in range(2)])
                hx2_rot = Rot([P.sb("hx2e%d" % i, [128, 1056], BF16, st) for i in range(2)])
                hx2T_rot = Rot([P.sb("hx2T%d" % i, [128, 1024], BF16, st) for i in range(1)])
                sm_rot = Rot([P.sb("sm%d" % i, [128, 16], F32, st) for i in range(6)])
                afs_rot = Rot([P.sb("afs%d" % i, [16, 128], F32, st) for i in range(2)])
                for b in range(nblk):
                    tok0 = b * TB
                    hxT = hx_rot.next()
                    fetch_block(hxT, b)
                    ynb = ynb_rot.next()
                    ylru_t, ysc_t = ylru_tt[b % YL_BUFS], ysc_tt[b % YL_BUFS]
                    ylru = []
                    for j in range(4):
                        lt_rot.set(b if LT_BY_BLOCK else j)
                        u, ub = lru_front(st, hxT, TB, RW, j)
                        h = lt_rot.next()
                        init = st_f[:, j:j + 1]
                        lru_dir(st, 0, u[:, 0:TB], ub[:, 0:TB], TB, j, init, h[:, 0:TB], False)
                        stn = col(st)
                        P.cp('dve', stn[:], h[:, TB - 1:TB])
                        P.cp('dve', st_f[:, j:j + 1], stn[:])
                        hb = hbb_rot.next()
                        P.ld(hb[:, 0:TB], hbd[j, :, off + tok0:off + tok0 + TB])
                        P.tt('dve', h[:, 0:TB], h[:, 0:TB], hb[:, 0:TB], ALU.add)
                        psg = inproj(hxT, TB, 4 + j)
                        gg = lt_rot.next()
                        P.act(gg[:, 0:TB], psg[:, 0:TB], AF.Gelu_apprx_tanh)
                        P.tt('dve', ylru_t[:, j, 0:TB], gg[:, 0:TB], h[:, 0:TB], ALU.mult)
                        ylru.append(ylru_t[:, j, 0:TB])
                    r = group_norm(st, ylru, TB, 512.0)
                    for j in range(4):
                        P.tt('dve', ynb[:, j, 0:TB], ylru[j], r[:, 0:TB], ALU.mult)
                    ysc = []
                    for j in range(2):
                        lt_rot.set(b if LT_BY_BLOCK else j)
                        psc = inproj(hxT, TB, 10 + j)
                        cc_ = lt_rot.next()
                        P.cp('act', cc_[:, 0:TB], psc[:, 0:TB])
                        psx = inproj(hxT, TB, 12 + j)
                        vv = lt_rot.next()
                        P.tt('dve', vv[:, 0:TB], psx[:, 0:TB], cc_[:, 0:TB], ALU.mult)
                        cv = lt_rot.next()
                        conv_taps(cv[:, 0:TB], vv[:, 0:TB], [CL[:, C_SCW + kx * 2 + j:C_SCW + kx * 2 + j + 1] for kx in range(3)],
                                  None, TB, RW, 1)
                        psb = inproj(hxT, TB, 8 + j)
                        P.tt('dve', ysc_t[:, j, 0:TB], psb[:, 0:TB], cv[:, 0:TB], ALU.mult)
                        ysc.append(ysc_t[:, j, 0:TB])
                    r = group_norm(st, ysc, TB, 256.0)
                    for j in range(2):
                        P.tt('dve', ynb[:, 4 + j, 0:TB], ysc[j], r[:, 0:TB], ALU.mult)
                    for q in range(2):
                        P.ld(ynb[:, 6 + q, 0:TB], yfd[q, :, off + tok0:off + tok0 + TB])
                    for i in range(TB // 128):
                        r0 = tok0 + i * 128
                        xt = xt_rot.next()
                        P.ld(xt[:], src[r0:r0 + 128, :])
                        x1 = x1_rot.next()
                        for hf in range(2):
                            ps = psA.next()
                            for kc in range(8):
                                P.mm(ps[:, :], ynb[:, kc, i * 128:(i + 1) * 128], Wout[:, kc, hf * 512:(hf + 1) * 512],
                                     start=(kc == 0), stop=(kc == 7))
                            P.tt('dve', x1[:, hf * 512:(hf + 1) * 512], ps[:, :], BC[('gate1', v)][:, hf * 512:(hf + 1) * 512], ALU.mult)
                        P.tt('dve', x1[:], x1[:], xt[:], ALU.add)
                        P.ld(xo[off + r0:off + r0 + 128, :], x1[:], wt=["xo:%d" % r0])
                        junk = junk_rot.next()
                        ss = col(st)
                        P.act(junk[:], x1[:], AF.Square, accum=ss[:])
                        rstd = col(st)
                        rsqrt_col(rstd[:], ss[:], 1024.0, st)
                        P.stt(xt[:], x1[:], rstd[:, 0:1], BC[('gs2', v)][:], ALU.mult, ALU.mult)
                        hx2e = hx2_rot.next()
                        P.tt('dve', hx2e[:, 0:1024], xt[:], BC[('sh2', v)][:], ALU.add)
                        pt = psT.next()
                        for k in range(8):
                            P.tr(pt[:, k * 128:(k + 1) * 128], hx2e[:, k * 128:(k + 1) * 128], ident_b[:, :])
                        hx2T = hx2T_rot.next()
                        P.cp('act', hx2T[:], pt[:])
                        pss = psS.next()
                        for k in range(8):
                            P.mm(pss[:, 0:16], hx2T[:, k * 128:(k + 1) * 128], wr_b[:, k, :], start=(k == 0), stop=(k == 7))
                        mx = sm_rot.next()
                        P.red(mx[:, 0:1], pss[:, 0:16], ALU.max)
                        P.ts('dve', mx[:, 1:2], mx[:, 0:1], -1.0)
                        ex = sm_rot.next()
                        P.act(ex[:, :], pss[:, 0:16], AF.Exp, bias=mx[:, 1:2], accum=mx[:, 2:3])
                        P.recip(mx[:, 3:4], mx[:, 2:3])
                        aff = sm_rot.next()
                        P.ts('dve', aff[:, :], ex[:, :], mx[:, 3:4])
                        P.cp('dve', hx2e[:, 1024:1040], aff[:, :])
                        P.tt('dve', ex[:, :], aff[:, :], hx2e[:, 1024:1040], ALU.subtract)
                        P.cp('dve', hx2e[:, 1040:1056], ex[:, :])
                        pss2 = psS.next()
                        P.tr(pss2[0:16, 0:128], aff[:, :], ident_f[:, :])
                        afs = afs_rot.next()
                        P.cp('act', afs[0:16, :], pss2[0:16, 0:128])
                        P.ld(affTd[:, off + r0:off + r0 + 128], afs[0:16, :], wt=["affTd:%d" % r0])
                        P.ld(hx2ext[off + r0:off + r0 + 128, :], hx2e[:, :], wt=["hx2ext:%d" % r0])
                P.barrier()
                P.emit()
        lst.close()
        if stop_after == ('mixer', l):
            if 'x1' in dbg_out:
                P.ld(dbg_out['x1'], xo)
            break

        with ExitStack() as st:
            onesrow = P.sb("onesrow", [64, 128], F32, st)
            P.ms('dve', onesrow[:], 1.0)
            iota_c = P.sb("iota_c", [64, 1024], F32, st)
            P.ld(iota_c[:], K['k_iota_c'][0:64, :])
            sets = [(0, 64, 1024, 0)] + ([(1, 2, 32, T_X)] if not last else [])
            for (v, NT, cap, off) in sets:
                A = P.sb("A", [NT, 16, 128], F32, st)
                P.ld(A[:], affTd[:, off:off + NT * 128].rearrange("e (i t) -> i e t", t=128))
                lo = P.sb("lo", [NT, 16], F32, st)
                hi = P.sb("hi", [NT, 16], F32, st)
                mid = P.sb("mid", [NT, 16], F32, st)
                cmp_ = P.sb("cmp", [NT, 16, 128], F32, st)
                cnt = P.sb("cnt", [NT, 16], F32, st)
                ge = P.sb("ge", [NT, 16], F32, st)
                dd = P.sb("dd", [NT, 16], F32, st)
                if NT == 64:
                    NP, TW = 128, 64
                    Ab = P.sb("Ab", [128, 16, 64], F32, st)
                    src2 = affTd[:, off:off + NT * 128].rearrange("e (i h t) -> h i e t", h=2, t=64)
                    for h_ in range(2):
                        P.ld(Ab[h_ * 64:(h_ + 1) * 64, :, :], src2[h_])
                    lo = P.sb("lo2", [128, 16], F32, st)
                    mid = P.sb("mid2", [128, 16], F32, st)
                    cmpb = P.sb("cmp2", [128, 16, 64], F32, st)
                    cnt2 = P.sb("cnt2", [128, 16], F32, st)
                    ge = P.sb("ge2", [128, 16], F32, st)
                else:
                    NP, TW, Ab, cmpb, cnt2 = NT, 128, A, cmp_, cnt
                P.ms('dve', lo[:], 0.0)
                for it in range(30):
                    c_it = 0.5 ** (it + 1)
                    P.ts('dve', mid[:], lo[:], c_it, op0=ALU.add)
                    P.tt('dve', cmpb[:], Ab[:], mid[:].unsqueeze(2).to_broadcast([NP, 16, TW]), ALU.is_ge)
                    P.red(cnt2[:], cmpb[:], ALU.add)
                    pss = psS.next()
                    P.mm(pss[0:NP, 0:16], ones_f[0:NP, 0:NP], cnt2[:], start=True, stop=True)
                    P.ts('dve', ge[:], pss[0:NP, 0:16], float(cap) - 0.5, c_it, op0=ALU.is_ge, op1=ALU.mult)
                    P.tt('dve', lo[:], lo[:], ge[:], ALU.add)
                P.tt('dve', cmp_[:], A[:], lo[0:NT, :].unsqueeze(2).to_broadcast([NT, 16, 128]), ALU.is_ge)
                RT = P.sb("RT", [NT, 16, 132], F32, st)
                for e in range(16):
                    P.scan(RT[:, e, 0:128], onesrow[0:NT, :], cmp_[:, e, :], 0.0)
                P.cp('dve', cnt[:], RT[:, :, 127])
                pss = psS.next()
                P.mm(pss[0:NT, 0:16], triu[0:NT, 0:NT], cnt[:], start=True, stop=True)
                base = P.sb("base", [NT, 16], F32, st)
                incl = P.sb("incl", [NT, 16], F32, st)
                P.cp('dve', base[:], pss[0:NT, 0:16])
                P.tt('dve', incl[:], base[:], cnt[:], ALU.add)
                P.cp('dve', RT[:, :, 128], base[:])
                tso = P.sb("tso", [NT, 1], F32, st)
                P.ts('dve', tso[:], tstart[0:NT, :], float(off), op0=ALU.add)
                P.cp('dve', RT[:, :, 129], tso[:, 0:1].to_broadcast([NT, 16]))
                P.ms('dve', RT[:, :, 130:132], 1.0)
                oh1 = P.sb("oh1", [NT, 1024], F32, st)
                oh = P.sb("oh", [NT, 1024], F32, st)
                nslot = (cap + 127) // 128
                for e in range(16):
                    P.ts('dve', oh1[:, 0:cap], iota_c[0:NT, 0:cap], base[:, e:e + 1], op0=ALU.is_ge)
                    P.stt(oh[:, 0:cap], iota_c[0:NT, 0:cap], incl[:, e:e + 1], oh1[:, 0:cap], ALU.is_lt, ALU.mult)
                    for j in range(nslot):
                        M = min(128, cap - j * 128)
                        slot = j if v == 0 else 8
                        pss = psS.next()
                        P.mm(pss[0:M, 0:132], oh[:, j * 128:j * 128 + M], RT[:, e, :], start=True, stop=True)
                        thr = P.sb("thr", [128, 4], F32, st)
                        P.ts('dve', thr[0:M, 0:1], pss[0:M, 128:129], -1.0, cidx[0:M, j:j + 1], op0=ALU.mult, op1=ALU.add)
                        jk = P.sb("jk", [128, 128], F32, st)
                        P.ts('dve', jk[0:M, :], pss[0:M, 0:128], thr[0:M, 0:1], pss[0:M, 129:130], op0=ALU.is_le, op1=ALU.add,
                             accum=thr[0:M, 2:3])
                        P.cp('dve', idx_all[0:M, e, slot:slot + 1], thr[0:M, 2:3])
                        P.cp('dve', val_all[0:M, e, slot:slot + 1], pss[0:M, 130:131])
            if 'idx' in dbg_out:
                idf = P.sb("idf", [128, 16 * 9], F32, st)
                P.cp('dve', idf[:], idx_all[:].rearrange("p e s -> p (e s)"))
                dump('idx', idf[:])
            P.barrier()
            P.emit()
        if stop_after == ('select', l):
            break

        with ExitStack() as st:
            Wg = P.sb("Wg", [128, 8, 1024], BF16, st)
            Wu = P.sb("Wu", [128, 8, 1024], BF16, st)
            Wd = P.sb("Wd", [128, 8, 1024], BF16, st)
            stg = Rot([P.sb("stg%d" % i, [128, 4096], F32, st) for i in range(MOE_STG)])
            xs_rot = Rot([P.sb("xs%d" % i, [128, 1056], BF16, st) for i in range(6)])
            xsT_rot = Rot([P.sb("xsT%d" % i, [128, 8, 1056], BF16, st) for i in range(2)])
            hid_rot = Rot([P.sb("hid%d" % i, [128, 8, 1056], BF16, st) for i in range(1)])
            gv_all = P.sb("gv_all", [128, 16, 9], F32, st)
            bc_load('gate2', 0, st)
            if not last:
                bc_load('gate2', 1, st, rows=32)
            sg_rot = Rot([P.sb("sg%d" % i, [128, 512], F32, st) for i in range(3)])
            ys_rot = Rot([P.sb("ys%d" % i, [128, 1024], F32, st) for i in range(3)])
            nsl = 8 if last else 9
            for e in range(16):
                for (Wt, nm_) in ((Wg, 'w_gate_e'), (Wu, 'w_up_e'), (Wd, 'w_down_e')):
                    for hh in range(2):
                        s = stg.next()
                        sv = s[:].rearrange("p (k n) -> p k n", k=4)
                        P.ld(sv, D[nm_][l, e][hh * 512:(hh + 1) * 512, :].rearrange("(k p) n -> p k n", p=128))
                        for k4 in range(4):
                            P.cp('act' if k4 % 2 == 0 else 'dve', Wt[:, hh * 4 + k4, :], sv[:, k4, :])
                xsT = xsT_rot.next()
                hid = hid_rot.next()
                for s_ in range(nsl):
                    M = 128 if s_ < 8 else 32
                    c0 = s_ * 128
                    xs = xs_rot.next()
                    P.gather(xs[0:M, :], hx2ext, idx_all[0:M, e, s_:s_ + 1])
                    P.tt('dve', gv_all[0:M, e, s_:s_ + 1], xs[0:M, 1024 + e:1025 + e], xs[0:M, 1040 + e:1041 + e], ALU.add)
                    P.tt('dve', gv_all[0:M, e, s_:s_ + 1], gv_all[0:M, e, s_:s_ + 1], val_all[0:M, e, s_:s_ + 1], ALU.mult)
                    pt = psT.next()
                    for k in range(8):
                        P.tr(pt[:, k * 128:k * 128 + M], xs[0:M, k * 128:(k + 1) * 128], ident_b[0:M, 0:M])
                    P.cp('act' if s_ % 2 == 0 else 'dve', xsT[:, :, c0:c0 + M], pt[:, :].rearrange("p (k m) -> p k m", k=8)[:, :, 0:M])
                groups = [(0, 512), (512, 512)] + ([(1024, 32)] if not last else [])
                for (c0, N) in groups:
                    for f in range(8):
                        psg = psA.next()
                        for k in range(8):
                            P.mm(psg[:, 0:N], Wg[:, k, f * 128:(f + 1) * 128], xsT[:, k, c0:c0 + N], start=(k == 0), stop=(k == 7))
                        psu = psA.next()
                        for k in range(8):
                            P.mm(psu[:, 0:N], Wu[:, k, f * 128:(f + 1) * 128], xsT[:, k, c0:c0 + N], start=(k == 0), stop=(k == 7))
                        sg = sg_rot.next()
                        P.act(sg[:, 0:N], psg[:, 0:N], AF.Silu)
                        P.tt('dve', hid[:, f, c0:c0 + N], psu[:, 0:N], sg[:, 0:N], ALU.mult)
                for s_ in range(nsl):
                    M = 128 if s_ < 8 else 32
                    c0 = s_ * 128
                    v = 0 if s_ < 8 else 1
                    ys = ys_rot.next()
                    for hf in range(2):
                        ps = psA.next()
                        for f in range(8):
                            P.mm(ps[0:M, :], hid[:, f, c0:c0 + M], Wd[:, f, hf * 512:(hf + 1) * 512], start=(f == 0), stop=(f == 7))
                        P.stt(ys[0:M, hf * 512:(hf + 1) * 512], ps[0:M, :], gv_all[0:M, e, s_:s_ + 1],
                              BC[('gate2', v)][0:M, hf * 512:(hf + 1) * 512], ALU.mult, ALU.mult)
                    P.scatter_add(xo, idx_all[0:M, e, s_:s_ + 1], ys[0:M, :],
                                  rt=["sc:%d:%d" % (e - 1, q_) for q_ in range(nsl)] if e > 0 else [],
                                  wt=["sc:%d:%d" % (e, s_)])
            P.barrier()
            P.emit()
        if stop_after == ('moe', l):
            if 'x1' in dbg_out:
                P.ld(dbg_out['x1'], xo)
            break

    if stop_after is None:
        with ExitStack() as st:
            xt_rot = Rot([P.sb("xt%d" % i, [128, 1024], F32, st) for i in range(3)])
            junk_rot = Rot([P.sb("junk%d" % i, [128, 1024], BF16, st) for i in range(2)])
            o_rot = Rot([P.sb("ot%d" % i, [128, 1024], F32, st) for i in range(3)])
            xf = xres[(depth - 1) % 2]
            gfin = P.sb("gfin", [128, 1024], F32, st)
            gfr = P.sb("gfr", [2, 1024], F32, st)
            P.ld(gfr[0:1, :], D['g_final'])
            P.ld(gfr[1:2, :], D['g_final'])
            for h in range(2):
                ps = psA.next()
                P.mm(ps[:, :], sel[0:2, 0:128], gfr[0:2, h * 512:(h + 1) * 512])
                P.cp('act', gfin[:, h * 512:(h + 1) * 512], ps[:, :])
            for i in range(T_X // 128):
                xt = xt_rot.next()
                P.ld(xt[:], xf[i * 128:(i + 1) * 128, :])
                junk = junk_rot.next()
                ss = col(st)
                P.act(junk[:], xt[:], AF.Square, accum=ss[:])
                rstd = col(st)
                rsqrt_col(rstd[:], ss[:], 1024.0, st)
                ot = o_rot.next()
                P.stt(ot[:], xt[:], rstd[:, 0:1], gfin[:], ALU.mult, ALU.mult)
                P.ld(out_d[i * 128:(i + 1) * 128, :], ot[:], wt=["out:%d" % i])
            P.barrier()
            P.emit()
    else:
        P.barrier()
        P.emit()
    P.stack.close()
    return nc


_CONSTS = None


def make_in_maps(inputs, n_cores=8):
    global _CONSTS
    if _CONSTS is None:
        _CONSTS = host_consts()
    maps = []
    f = lambda a: np.ascontiguousarray(np.asarray(a, dtype=np.float32))
    shared = {n: f(inputs[n]) for n in IN_SHAPES if n not in ('x', 'c', 'ctx', 'c_ctx', 'g_final')}
    shared['c_ctx'] = f(inputs['c_ctx']).reshape(1, 1024)
    shared['g_final'] = f(inputs['g_final']).reshape(1, 1024)
    shared.update(_CONSTS)
    for core in range(n_cores):
        b = core % 4
        m = dict(shared)
        m['x'] = f(inputs['x'][b])
        m['c'] = f(inputs['c'][b]).reshape(1, 1024)
        m['ctx'] = f(inputs['ctx'][b])
        maps.append(m)
    return maps


def kernel(**inputs):
    nc = build()
    maps = make_in_maps(inputs)
    res = run_bass_kernel_spmd(nc, maps, core_ids=list(range(8)))
    out = np.stack([np.asarray(res.results[b]["out"], dtype=np.float32) for b in range(4)], axis=0)
    return out
```

```python
import numpy as np
import ml_dtypes
from contextlib import ExitStack
import concourse.bass as bass
import concourse.mybir as mybir
from concourse.bass_utils import run_bass_kernel_spmd

F32 = mybir.dt.float32
BF16 = mybir.dt.bfloat16
I32 = mybir.dt.int32
ALU = mybir.AluOpType
AF = mybir.ActivationFunctionType
AX = mybir.AxisListType

ENGS = ['pe', 'act', 'dve', 'pool', 'sp']
NDMASEM = 8
SAME_ENGINE_SYNC = True
SCHEDULE = True
TABLE_AWARE = True
ASET = {AF.Sigmoid: 'sig', AF.Silu: 'silu', AF.Exp: 'exp', AF.Ln: 'exp', AF.Sqrt: 'sqrt', AF.Gelu_apprx_tanh: 'gelu'}
PE_GHZ = 1.9
SCHED_WINDOW = 48
SCHED_MODE = 'fifo'
INTERLEAVE = True
LT_POOLS, LT_DEPTH, LT_BY_BLOCK = 2, 7, False
YL_BUFS = 1
MOE_STG = 2
PS_A, PS_T, PS_S = 5, 1, 2
EPS = 1e-6
T_X = 8192
T_C = 256
NTOK = T_X + T_C


class Prog:
    def __init__(self, nc):
        self.nc = nc
        self.pending = []
        self.eng_seq = {e: 0 for e in ENGS}
        self.dma_cnt = {e: 0 for e in ENGS}
        self.dma_val = {}
        self.known = {e: {} for e in ENGS}
        self.stack = ExitStack()
        self.sems = {}
        for e in ENGS:
            k = 'eng:' + e
            self.sems[k] = self.stack.enter_context(nc.semaphore(k.replace(':', '_')))
            for j in range(NDMASEM):
                k = 'dma:%s:%d' % (e, j)
                self.sems[k] = self.stack.enter_context(nc.semaphore(k.replace(':', '_')))
        self.uid = 0
        self.sim_time = 0.0

    def sb(self, name, shape, dt, stack=None):
        self.uid += 1
        return (stack or self.stack).enter_context(self.nc.sbuf_tensor("%s_%d" % (name, self.uid), list(shape), dt))

    def ps(self, name, shape, dt=F32):
        return self.stack.enter_context(self.nc.psum_tensor(name, list(shape), dt))

    @staticmethod
    def _tok(t):
        if isinstance(t, (str, int)):
            return t
        return t.tensor.name

    def op(self, eng, fn, reads=(), writes=(), cost=0.1, aset=None):
        r = [self._tok(t) for t in reads if t is not None and not isinstance(t, (int, float))]
        w = [self._tok(t) for t in writes]
        self.pending.append((eng, False, fn, r, w, cost, aset))

    def dma(self, eng, fn, reads=(), writes=(), cost=3.0):
        r = [self._tok(t) for t in reads]
        w = [self._tok(t) for t in writes]
        self.pending.append((eng, True, fn, r, w, cost, None))

    def barrier(self):
        pass

    def mark(self):
        return len(self.pending)

    def interleave(self, i0, i1, i2):
        a, b = self.pending[i0:i1], self.pending[i1:i2]
        if not a or not b:
            return
        out = []
        ia = ib = 0
        while ia < len(a) or ib < len(b):
            if ib >= len(b) or (ia < len(a) and ia * len(b) <= ib * len(a)):
                out.append(a[ia])
                ia += 1
            else:
                out.append(b[ib])
                ib += 1
        self.pending[i0:i2] = out

    def _schedule(self, ops):
        n = len(ops)
        last_w = {}
        readers = {}
        deps = []
        for i, (eng, isdma, fn, r, w, cost, aset_) in enumerate(ops):
            d = set()
            for t in r:
                if t in last_w:
                    d.add(last_w[t])
            for t in w:
                if t in last_w:
                    d.add(last_w[t])
                rl = readers.get(t)
                if rl:
                    d.update(rl)
            d.discard(i)
            deps.append(d)
            for t in r:
                readers.setdefault(t, []).append(i)
            for t in w:
                last_w[t] = i
                readers[t] = []
        succ = [[] for _ in range(n)]
        ndep = [len(d) for d in deps]
        for i, d in enumerate(deps):
            for j in d:
                succ[j].append(i)
        ready = [0.0] * n
        blev = [0.0] * n
        for i in range(n - 1, -1, -1):
            m = 0.0
            for s_ in succ[i]:
                if blev[s_] > m:
                    m = blev[s_]
            blev[i] = m + ops[i][5] + 0.3
        queues = {e: [] for e in ENGS}
        for i, o in enumerate(ops):
            queues[o[0]].append(i)
        free = {e: 0.0 for e in ENGS}
        dma_fin = {e: [] for e in ENGS}
        order = {e: [] for e in ENGS}
        remaining = n
        W = SCHED_WINDOW
        cur_set = [None]
        while remaining:
            best = None
            for e in ENGS:
                q = queues[e]
                if not q:
                    continue
                fe = free[e]
                lim = min(W, len(q))
                first = None
                cand = None
                if e == 'act' and TABLE_AWARE:
                    for p in range(lim):
                        i = q[p]
                        if ndep[i] or ready[i] > fe:
                            continue
                        a_ = ops[i][6]
                        if a_ is None or a_ == cur_set[0]:
                            cand = (fe, i, e, p)
                            break
                    if cand is not None:
                        if best is None or cand[0] < best[0] or (cand[0] == best[0] and cand[1] < best[1]):
                            best = cand
                        continue
                for p in range(lim):
                    i = q[p]
                    if ndep[i]:
                        continue
                    st = ready[i] if ready[i] > fe else fe
                    if ops[i][1]:
                        k = len(dma_fin[e])
                        if k >= NDMASEM and dma_fin[e][k - NDMASEM] > st:
                            st = dma_fin[e][k - NDMASEM]
                    if SCHED_MODE == 'blev':
                        key = (max(st, fe), -blev[i])
                        if cand is None or key < ckey:
                            cand = (st, i, e, p)
                            ckey = key
                        first = cand
                        continue
                    if first is None:
                        first = (st, i, e, p)
                        cand = first
                        if st <= fe:
                            break
                    else:
                        c = ops[i][5] if not ops[i][1] else 0.05
                        if st + c <= first[0] and st < cand[0]:
                            cand = (st, i, e, p)
                            if st <= fe:
                                break
                if cand is not None and (best is None or cand[0] < best[0] or (cand[0] == best[0] and cand[1] < best[1])):
                    best = cand
            st, i, e, p = best
            queues[e].pop(p)
            o = ops[i]
            f = st + o[5]
            if e == 'act' and o[6] is not None:
                if o[6] != cur_set[0]:
                    f += 1.28
                cur_set[0] = o[6]
            if o[1]:
                free[e] = st + 0.05
                dma_fin[e].append(f)
            else:
                free[e] = f
            for s_ in succ[i]:
                ndep[s_] -= 1
                lat = 0.0 if (ops[s_][0] == e and not o[1]) else 0.3
                if f + lat > ready[s_]:
                    ready[s_] = f + lat
            order[e].append(i)
            remaining -= 1
        tend = max(list(free.values()) + [f_ for v in dma_fin.values() for f_ in v] + [0.0])
        return deps, order, tend

    def emit(self):
        ops = self.pending
        self.pending = []
        nc = self.nc
        sems = self.sems
        if ops:
            if SCHEDULE:
                deps, order, tend = self._schedule(ops)
            else:
                deps, order, tend = self._schedule_inorder(ops)
            self.sim_time += tend
        else:
            deps, order = [], {e: [] for e in ENGS}
        ev = [None] * len(ops)
        for e in ENGS:
            for i in order[e]:
                if ops[i][1]:
                    k = self.dma_cnt[e]
                    self.dma_cnt[e] += 1
                    key = 'dma:%s:%d' % (e, k % NDMASEM)
                    prev = self.dma_val.get(key, 0)
                    self.dma_val[key] = prev + 16
                    ev[i] = (key, prev + 16, prev)
                else:
                    self.eng_seq[e] += 1
                    ev[i] = ('eng:' + e, self.eng_seq[e], 0)
        prog = {e: [] for e in ENGS}
        for e in ENGS:
            kn = self.known[e]
            for i in order[e]:
                need = {}
                for d in deps[i]:
                    k, v, _ = ev[d]
                    if k == 'eng:' + e and (e == 'pe' or not SAME_ENGINE_SYNC):
                        continue
                    if need.get(k, 0) < v:
                        need[k] = v
                k, v, prev = ev[i]
                if ops[i][1] and prev > 0 and need.get(k, 0) < prev:
                    need[k] = prev
                waits = []
                for k2, v2 in need.items():
                    if kn.get(k2, 0) >= v2:
                        continue
                    kn[k2] = v2
                    waits.append((k2, v2))
                prog[e].append((waits, ops[i][2], k, 16 if ops[i][1] else 1))
        allv = {}
        for e in ENGS:
            if self.eng_seq[e]:
                allv['eng:' + e] = self.eng_seq[e]
        for k, v in self.dma_val.items():
            allv[k] = v
        for e in ENGS:
            kn = self.known[e]
            waits = []
            for k, v in allv.items():
                if k == 'eng:' + e or kn.get(k, 0) >= v:
                    continue
                kn[k] = v
                waits.append((k, v))
            kn['eng:' + e] = self.eng_seq[e]
            prog[e].append((waits, None, None, 0))

        def run(h, lst):
            for waits, fn, k, amt in lst:
                for (wk, wv) in waits:
                    h.wait_ge(sems[wk], wv)
                if fn is not None:
                    fn(h).then_inc(sems[k], amt)
        with nc.Block() as block:
            @block.tensor
            def _(e):
                run(e, prog['pe'])

            @block.scalar
            def _(e):
                run(e, prog['act'])

            @block.vector
            def _(e):
                run(e, prog['dve'])

            @block.gpsimd
            def _(e):
                run(e, prog['pool'])

            @block.sync
            def _(e):
                run(e, prog['sp'])

    def _schedule_inorder(self, ops):
        n = len(ops)
        W_save = None
        global SCHED_WINDOW
        W_save, SCHED_WINDOW = SCHED_WINDOW, 1
        try:
            return self._schedule(ops)
        finally:
            SCHED_WINDOW = W_save

    def mm(self, out, lhsT, rhs, start=True, stop=True, rt=None):
        c = max(64, rhs.free_size()) / PE_GHZ / 1000.0 * (4.0 if rhs.dtype == F32 else 1.0) + 0.02
        self.op('pe', lambda e: e.matmul(out, lhsT=lhsT, rhs=rhs, start=start, stop=stop), [lhsT, rhs] + list(rt or []), [out], c)

    def tr(self, out, in_, ident):
        self.op('pe', lambda e: e.transpose(out, in_, ident), [in_, ident], [out], 0.11)

    def act(self, out, in_, func, bias=None, scale=None, accum=None, wt=None):
        kw = {}
        if bias is not None:
            kw['bias'] = bias
        if scale is not None:
            kw['scale'] = scale
        if accum is not None:
            kw['accum_out'] = accum
        r = [in_] + [a for a in (bias, scale) if a is not None and not isinstance(a, (int, float))]
        w = (list(wt) if wt else [out]) + ([accum] if accum is not None else [])
        c = 0.22 + in_.free_size() / 1400.0
        self.op('act', lambda e: e.activation(out=out, in_=in_, func=func, **kw), r, w, c, ASET.get(func))

    def _vc(self, eng, n, f=1.0):
        f = 1.0 + (f - 1.0) * 0.3
        return (0.07 + f * n / 960.0) if eng == 'dve' else (0.3 + n / 300.0)

    def ts(self, eng, out, in0, s1, s2=None, op0=ALU.mult, op1=None, accum=None, wt=None):
        kw = {}
        if op1 is not None:
            kw['op1'] = op1
        if accum is not None:
            kw['accum_out'] = accum
        r = [in0] + [a for a in (s1, s2) if a is not None and not isinstance(a, (int, float))]
        w = (list(wt) if wt else [out]) + ([accum] if accum is not None else [])
        self.op(eng, lambda e: e.tensor_scalar(out=out, in0=in0, scalar1=s1, scalar2=s2, op0=op0, **kw), r, w,
                self._vc(eng, in0.free_size()))

    def tt(self, eng, out, in0, in1, op):
        self.op(eng, lambda e: e.tensor_tensor(out=out, in0=in0, in1=in1, op=op), [in0, in1], [out],
                self._vc(eng, in0.free_size(), 1.5))

    def stt(self, out, in0, scalar, in1, op0, op1):
        r = [in0, in1] + ([scalar] if not isinstance(scalar, (int, float)) else [])
        self.op('dve', lambda e: e.scalar_tensor_tensor(out=out, in0=in0, scalar=scalar, in1=in1, op0=op0, op1=op1), r, [out],
                self._vc('dve', in0.free_size(), 1.5))

    def cp(self, eng, out, in_):
        if eng == 'act':
            self.act(out, in_, AF.Copy)
        else:
            self.op(eng, lambda e: e.tensor_copy(out=out, in_=in_), [in_], [out], self._vc(eng, in_.free_size()))

    def ms(self, eng, ap, val):
        self.op(eng, lambda e: e.memset(ap, val), [], [ap], self._vc(eng, ap.free_size()))

    def scan(self, out, d0, d1, init):
        r = [d0, d1] + ([init] if not isinstance(init, (int, float)) else [])
        self.op('dve', lambda e: e.tensor_tensor_scan(out=out, data0=d0, data1=d1, initial=init, op0=ALU.mult, op1=ALU.add), r, [out],
                self._vc('dve', d0.free_size(), 2.0))

    def red(self, out, in_, op):
        self.op('dve', lambda e: e.tensor_reduce(out=out, in_=in_, axis=AX.X, op=op), [in_], [out], self._vc('dve', in_.free_size()))

    def recip(self, out, in_):
        self.op('dve', lambda e: e.reciprocal(out=out, in_=in_), [in_], [out], self._vc('dve', in_.free_size(), 8.0))

    def ld(self, out, in_, eng='sp', wt=None, rt=None, **kw):
        c = 2.0 + out.nbytes() / 150000.0
        self.dma(eng, lambda e: e.dma_start(out=out, in_=in_, **kw), [in_] + list(rt or []), list(wt) if wt else [out], c)

    def gather(self, out, table, idx):
        self.dma('pool', lambda e: e.indirect_dma_start(out=out, out_offset=None, in_=table,
                                                        in_offset=bass.IndirectOffsetOnAxis(ap=idx, axis=0)),
                 [table, idx], [out], 3.0 + out.nbytes() / 100000.0)

    def scatter_add(self, table, idx, in_, rt=None, wt=None):
        self.dma('pool', lambda e: e.indirect_dma_start(out=table, out_offset=bass.IndirectOffsetOnAxis(ap=idx, axis=0),
                                                        in_=in_, in_offset=None, compute_op=ALU.add),
                 [idx, in_] + (list(rt) if rt is not None else [table]), list(wt) if wt else [table], 4.0 + in_.nbytes() / 80000.0)


class MultiRot:
    def __init__(self, pools):
        self.pools = pools
        self.cur = 0

    def set(self, j):
        self.cur = j % len(self.pools)

    def next(self):
        return self.pools[self.cur].next()


class Rot:
    def __init__(self, items):
        self.items = items
        self.i = 0

    def next(self):
        t = self.items[self.i]
        self.i = (self.i + 1) % len(self.items)
        return t


def host_consts():
    bf = ml_dtypes.bfloat16
    k = {}
    k['k_ident_f'] = np.eye(128, dtype=np.float32)
    k['k_ident_b'] = np.eye(128).astype(bf)
    k['k_ones_b'] = np.ones((128, 128)).astype(bf)
    k['k_ones_f'] = np.ones((128, 128), np.float32)
    k['k_triu'] = np.triu(np.ones((128, 128), np.float32), 1)
    sel = np.zeros((2, 256), np.float32)
    sel[0, :128] = 1
    sel[1, 128:] = 1
    k['k_sel'] = sel
    k['k_iota_c'] = np.tile(np.arange(1024, dtype=np.float32)[None], (128, 1))
    k['k_cidx'] = (np.arange(8)[None, :] * 128 + np.arange(128)[:, None]).astype(np.float32)
    k['k_tstart'] = (np.arange(128) * 128).astype(np.float32)[:, None].copy()
    scale = 1.0 / np.sqrt(8192.0 * 64.0)
    cc = np.arange(64)
    ang = 2 * np.pi * np.outer(cc, cc) / 64.0
    bd = np.zeros((2, 128, 512), np.float64)
    for q in range(2):
        for gl in range(2):
            r0 = gl * 64
            c0 = q * 128 + gl * 64
            bd[q, r0:r0 + 64, c0:c0 + 64] = np.cos(ang) * scale
            bd[q, r0:r0 + 64, 256 + c0:256 + c0 + 64] = -np.sin(ang) * scale
    k['k_bd'] = np.ascontiguousarray(bd.transpose(1, 0, 2)).astype(bf)
    n1 = np.arange(128)
    a1 = 2 * np.pi * np.outer(n1, n1) / 128.0
    C1, S1 = np.cos(a1), np.sin(a1)
    cs = np.stack([np.concatenate([C1, -S1], 1), np.concatenate([S1, C1], 1)], 1)
    k['k_cs1'] = cs.astype(bf)
    n2 = np.arange(64)
    k1 = np.arange(128)
    k2 = np.arange(64)
    kk = k1[:, None] + 128 * k2[None, :]
    a3 = 2 * np.pi * n2[:, None, None] * kk[None] / 8192.0
    e3 = np.stack([np.cos(a3), np.sin(a3)], 2)
    k['k_e3'] = np.concatenate([e3, e3], 0).astype(bf)
    n = np.arange(256)
    ac = 2 * np.pi * np.outer(n, n) / 256.0
    f = np.sqrt(32.0)
    dc = np.stack([np.cos(ac) * f, np.sin(ac) * f], 1)
    k['k_dc'] = np.ascontiguousarray(dc.reshape(2, 128, 2, 256).transpose(1, 0, 2, 3)).astype(bf)
    return k


CONST_SHAPES = {
    'k_ident_f': ([128, 128], F32), 'k_ident_b': ([128, 128], BF16), 'k_ones_b': ([128, 128], BF16),
    'k_ones_f': ([128, 128], F32), 'k_triu': ([128, 128], F32), 'k_sel': ([2, 256], F32),
    'k_iota_c': ([128, 1024], F32), 'k_cidx': ([128, 8], F32), 'k_tstart': ([128, 1], F32),
    'k_bd': ([128, 2, 512], BF16), 'k_cs1': ([128, 2, 256], BF16), 'k_e3': ([128, 128, 2, 64], BF16),
    'k_dc': ([128, 2, 2, 256], BF16),
}

IN_SHAPES = {
    'x': [T_X, 1024], 'c': [1, 1024], 'ctx': [T_C, 1024], 'c_ctx': [1, 1024],
    'w_ada': [2, 1024, 6144], 'b_ada': [2, 6144], 'g_norm1': [2, 1024], 'w_in': [2, 1024, 2048],
    'lru_conv_w': [2, 4, 512], 'lru_conv_b': [2, 512], 'lru_wr': [2, 2, 8, 64, 64], 'lru_br': [2, 2, 512],
    'lru_wi': [2, 2, 8, 64, 64], 'lru_bi': [2, 2, 512], 'lru_lam': [2, 2, 512], 'sc_conv_w': [2, 3, 256],
    'g_out': [2, 1024], 'w_out': [2, 1024, 1024], 'g_norm2': [2, 1024], 'w_router': [2, 1024, 16],
    'w_gate_e': [2, 16, 1024, 1024], 'w_up_e': [2, 16, 1024, 1024], 'w_down_e': [2, 16, 1024, 1024],
    'g_final': [1, 1024],
}

C_G1, C_CW, C_CB, C_BR, C_BI, C_LAM, C_SCW, C_GOUT, C_NROWS = 0, 8, 24, 28, 36, 44, 52, 58, 66


def build(depth=2, stop_after=None, dbg=None):
    nc = bass.Bass("TRN2", target_bir_lowering=False)
    P = Prog(nc)
    D = {}
    for name, shp in IN_SHAPES.items():
        D[name] = nc.dram_tensor(name, shp, F32, kind="ExternalInput").ap()
    K = {}
    for name, (shp, dt) in CONST_SHAPES.items():
        K[name] = nc.dram_tensor(name, shp, dt, kind="ExternalInput").ap()
    out_d = nc.dram_tensor("out", [T_X, 1024], F32, kind="ExternalOutput").ap()
    xres = [nc.dram_tensor("xres%d" % i, [NTOK, 1024], F32, kind="Internal").ap() for i in range(2)]
    hx2ext = nc.dram_tensor("hx2ext", [NTOK, 1056], BF16, kind="Internal").ap()
    Vd = nc.dram_tensor("Vd", [2, NTOK, 256], BF16, kind="Internal").ap()
    hbd = nc.dram_tensor("hbd", [4, 128, NTOK], BF16, kind="Internal").ap()
    yfd = nc.dram_tensor("yfd", [2, 128, NTOK], BF16, kind="Internal").ap()
    affTd = nc.dram_tensor("affTd", [16, NTOK], F32, kind="Internal").ap()
    hxd = nc.dram_tensor("hxd", [8, 128, NTOK], BF16, kind="Internal").ap()
    dbg_out = {}
    if dbg:
        for name, shp in dbg.items():
            dbg_out[name] = nc.dram_tensor("dbg_" + name, shp, F32, kind="ExternalOutput").ap()

    ident_f = P.sb("ident_f", [128, 128], F32)
    ident_b = P.sb("ident_b", [128, 128], BF16)
    ones_b = P.sb("ones_b", [128, 128], BF16)
    ones_f = P.sb("ones_f", [128, 128], F32)
    triu = P.sb("triu", [128, 128], F32)
    sel = P.sb("sel", [2, 256], F32)
    cidx = P.sb("cidx", [128, 8], F32)
    tstart = P.sb("tstart", [128, 1], F32)
    bd = P.sb("bd", [128, 2, 512], BF16)
    cs1 = P.sb("cs1", [128, 2, 256], BF16)
    dc = P.sb("dc", [128, 2, 2, 256], BF16)
    for t, n in ((ident_f, 'k_ident_f'), (ident_b, 'k_ident_b'), (ones_b, 'k_ones_b'), (ones_f, 'k_ones_f'),
                 (triu, 'k_triu'), (sel, 'k_sel'), (cidx, 'k_cidx'), (tstart, 'k_tstart'), (bd, 'k_bd'),
                 (cs1, 'k_cs1'), (dc, 'k_dc')):
        P.ld(t[:], K[n])
    wr_b = P.sb("wr_b", [128, 8, 16], BF16)
    gates_b = P.sb("gates_b", [128, 2, 2, 4, 128], BF16)
    bcn = ['sh2', 'gs2', 'gate1', 'gate2']
    BC = {}
    bcd = nc.dram_tensor("bcd", [8, 128, 1024], F32, kind="Internal").ap()

    def bc_load(n_, v_, st_, rows=128):
        t_ = P.sb("bc_%s%d" % (n_, v_), [128, 1024], F32, st_)
        P.ld(t_[0:rows, :], bcd[bcn.index(n_) * 2 + v_, 0:rows, :])
        BC[(n_, v_)] = t_
    CL = P.sb("CL", [128, C_NROWS], F32)
    nsp = P.sb("nsp", [128, 8], F32)
    gs1 = P.sb("gs1", [128, 8, 2], F32)
    sh1 = P.sb("sh1", [128, 8, 2], F32)
    st_f = P.sb("st_f", [128, 4], F32)
    st_b = P.sb("st_b", [128, 4], F32)
    idx_all = P.sb("idx_all", [128, 16, 9], I32)
    val_all = P.sb("val_all", [128, 16, 9], F32)
    psA_banks = [P.ps("psA%d" % i, [128, 512], F32) for i in range(PS_A)]
    psA = Rot(psA_banks)
    psA_all, psA_lo, psA_hi = psA, Rot(psA_banks[0:PS_A - 2]), Rot(psA_banks[PS_A - 2:PS_A])
    psT = Rot([P.ps("psT%d" % i, [128, 1024], BF16) for i in range(PS_T)])
    psS = Rot([P.ps("psS%d" % i, [128, 512], F32) for i in range(PS_S)])

    cast_rr = Rot(['act', 'dve'])

    colpool = {}

    def col(st):
        if not hasattr(st, '_colpool'):
            st._colpool = Rot([P.sb("col%d" % i, [128, 1], F32, st) for i in range(32)])
        return st._colpool.next()

    def rsqrt_col(out, ss, n, st):
        t = col(st)
        P.ts('dve', t[:], ss, 1.0 / n, EPS, op0=ALU.mult, op1=ALU.add)
        P.act(t[:], t[:], AF.Ln)
        P.act(out, t[:], AF.Exp, scale=-0.5)

    def dump(name, ap_src, rows=None):
        if name in dbg_out:
            P.ld(dbg_out[name] if rows is None else dbg_out[name][rows], ap_src)

    for l in range(depth):
        last = (l == depth - 1)
        xin = D['x'] if l == 0 else xres[(l - 1) % 2][0:T_X, :]
        cin = D['ctx'] if l == 0 else xres[(l - 1) % 2][T_X:NTOK, :]
        xo = xres[l % 2]
        with ExitStack() as st:
            stg = Rot([P.sb("stg%d" % i, [128, 4096], F32, st) for i in range(2)])
            rows = P.sb("rows", [C_NROWS, 128], F32, st)
            P.ld(rows[C_G1:C_G1 + 8, :], D['g_norm1'][l].rearrange("(j p) -> j p", p=128))
            P.ld(rows[C_CW:C_CW + 16, :], D['lru_conv_w'][l].rearrange("k (j p) -> (k j) p", p=128))
            P.ld(rows[C_CB:C_CB + 4, :], D['lru_conv_b'][l].rearrange("(j p) -> j p", p=128))
            P.ld(rows[C_BR:C_BR + 8, :], D['lru_br'][l].rearrange("d (j p) -> (d j) p", p=128))
            P.ld(rows[C_BI:C_BI + 8, :], D['lru_bi'][l].rearrange("d (j p) -> (d j) p", p=128))
            P.ld(rows[C_LAM:C_LAM + 8, :], D['lru_lam'][l].rearrange("d (j p) -> (d j) p", p=128))
            P.ld(rows[C_SCW:C_SCW + 6, :], D['sc_conv_w'][l].rearrange("k (j p) -> (k j) p", p=128))
            P.ld(rows[C_GOUT:C_GOUT + 8, :], D['g_out'][l].rearrange("(j p) -> j p", p=128))
            pss = psS.next()
            P.tr(pss[:, 0:C_NROWS], rows[:, :], ident_f[0:C_NROWS, 0:C_NROWS])
            P.cp('dve', CL[:, :], pss[:, 0:C_NROWS])
            tmp8 = P.sb("tmp8", [128, 8], F32, st)
            P.act(tmp8[:], CL[:, C_LAM:C_LAM + 8], AF.Exp, scale=-1.0)
            P.act(tmp8[:], tmp8[:], AF.Ln, bias=1.0)
            P.ts('dve', nsp[:], tmp8[:], -8.0)
            gst = P.sb("gst", [128, 2, 2, 4, 128], F32, st)
            P.ms('pool', gst[:], 0.0)
            for d in range(2):
                for kind, nm_ in ((0, 'lru_wr'), (1, 'lru_wi')):
                    src = D[nm_][l, d].rearrange("(j two) i o -> two i j o", two=2)
                    P.ld(gst[0:64, d, kind, :, 0:64], src[0])
                    P.ld(gst[64:128, d, kind, :, 64:128], src[1])
            P.cp('pool', gates_b[:], gst[:])
            crow = P.sb("crow", [2, 1024], F32, st)
            P.ld(crow[0:1, :], D['c'])
            P.ld(crow[1:2, :], D['c_ctx'])
            pss = psS.next()
            for j in range(8):
                P.tr(pss[:, 2 * j:2 * j + 2], crow[0:2, j * 128:(j + 1) * 128], ident_f[0:2, 0:2])
            cvec = P.sb("cvec", [128, 8, 2], F32, st)
            P.act(cvec[:].rearrange("p k v -> p (k v)"), pss[:, 0:16], AF.Silu)
            badar = P.sb("badar", [2, 6144], F32, st)
            P.ld(badar[0:1, :], D['b_ada'][l:l + 1, :])
            P.ld(badar[1:2, :], D['b_ada'][l:l + 1, :])
            mods = P.sb("mods", [2, 6144], F32, st)
            for nb in range(12):
                s = stg.next()
                sv = s[:].rearrange("p (k n) -> p k n", k=8)
                P.ld(sv, D['w_ada'][l][:, nb * 512:(nb + 1) * 512].rearrange("(k p) n -> p k n", p=128))
                ps = psA.next()
                for k in range(8):
                    P.mm(ps[0:2, :], cvec[:, k, :], sv[:, k, :], start=(k == 0), stop=(k == 7))
                P.tt('dve', mods[0:2, nb * 512:(nb + 1) * 512], ps[0:2, :], badar[0:2, nb * 512:(nb + 1) * 512], ALU.add)
            pss = psS.next()
            for q in range(2):
                for j in range(8):
                    o = (q * 8 + j) * 2
                    P.tr(pss[:, o:o + 2], mods[0:2, q * 1024 + j * 128:q * 1024 + (j + 1) * 128], ident_f[0:2, 0:2])
            modc = P.sb("modc", [128, 2, 8, 2], F32, st)
            P.cp('dve', modc[:].rearrange("p q k v -> p (q k v)"), pss[:, 0:32])
            for v in range(2):
                P.stt(gs1[:, :, v], modc[:, 1, :, v], 1.0, CL[:, C_G1:C_G1 + 8], ALU.add, ALU.mult)
                P.cp('dve', sh1[:, :, v], modc[:, 0, :, v])
            g2r = P.sb("g2r", [2, 1024], F32, st)
            P.ld(g2r[0:1, :], D['g_norm2'][l:l + 1, :])
            P.ld(g2r[1:2, :], D['g_norm2'][l:l + 1, :])
            g2bc = P.sb("g2bc", [128, 1024], F32, st)

            def bcast(dst, src_rows, c0, v):
                for h in range(2):
                    ps = psA.next()
                    P.mm(ps[:, :], sel[0:2, v * 128:(v + 1) * 128], src_rows[0:2, c0 + h * 512:c0 + (h + 1) * 512])
                    P.cp('act', dst[:, h * 512:(h + 1) * 512], ps[:, :])
            bcast(g2bc, g2r, 0, 0)
            for n_ in bcn:
                for v in range(2):
                    BC[(n_, v)] = P.sb("bcp_%s%d" % (n_, v), [128, 1024], F32, st)
            for v in range(2):
                bcast(BC[('gate1', v)], mods, 2 * 1024, v)
                bcast(BC[('sh2', v)], mods, 3 * 1024, v)
                bcast(BC[('gs2', v)], mods, 4 * 1024, v)
                bcast(BC[('gate2', v)], mods, 5 * 1024, v)
                P.stt(BC[('gs2', v)][:], BC[('gs2', v)][:], 1.0, g2bc[:], ALU.add, ALU.mult)
            for n_ in bcn:
                for v in range(2):
                    P.ld(bcd[bcn.index(n_) * 2 + v], BC[(n_, v)][:])
            P.barrier()
            P.emit()
        lst = ExitStack()
        Win = P.sb("Win", [128, 8, 2048], BF16, lst)
        with ExitStack() as st:
            stg = Rot([P.sb("stg%d" % i, [128, 4096], F32, st) for i in range(2)])
            for kk in range(4):
                s = stg.next()
                sv = s[:].rearrange("p (k n) -> p k n", k=2)
                P.ld(sv, D['w_in'][l][kk * 256:(kk + 1) * 256, :].rearrange("(k p) n -> p k n", p=128))
                for k2 in range(2):
                    P.cp(cast_rr.next(), Win[:, kk * 2 + k2, :], sv[:, k2, :])
            wrs = P.sb("wrs", [128, 8, 16], F32, st)
            P.ld(wrs[:], D['w_router'][l].rearrange("(k p) e -> p k e", p=128))
            P.cp('dve', wr_b[:], wrs[:])
            P.barrier()
            P.emit()
        if stop_after == ('prep', l):
            break

        def prep_wfft(st_):
            Wf = P.sb("Wfft", [128, 8, 512], BF16, st_)
            WT = [P.sb("WT%d" % q, [128, 1024], BF16, st_) for q in range(2)]
            for q in range(2):
                pt = psT.next()
                for k in range(8):
                    P.tr(pt[:, k * 128:(k + 1) * 128], Win[:, k, 1792 + q * 128:1792 + (q + 1) * 128], ident_b[:, :])
                P.cp('act', WT[q][:], pt[:])
            for k in range(8):
                ps = psA.next()
                for q in range(2):
                    P.mm(ps[:, :], WT[q][:, k * 128:(k + 1) * 128], bd[:, q, :], start=(q == 0), stop=(q == 1))
                P.cp('dve', Wf[:, k, :], ps[:, :])
            return Wf

        def prep_wout(st_):
            Wo = P.sb("Wout", [128, 8, 1024], BF16, st_)
            for kk in range(2):
                P.ld(Wo[:, kk * 4:(kk + 1) * 4, :], D['w_out'][l][kk * 512:(kk + 1) * 512, :].rearrange("(k p) n -> p k n", p=128),
                     eng='pool')
            for k in range(8):
                P.act(Wo[:, k, :], Wo[:, k, :], AF.Copy, scale=CL[:, C_GOUT + k:C_GOUT + k + 1])
            return Wo

        def load_block(st, src, tok0, TB, v, hxT):
            for i in range(TB // 128):
                xt = xt_rot.next()
                P.ld(xt[:], src[tok0 + i * 128:tok0 + (i + 1) * 128, :])
                junk = junk_rot.next()
                ss = col(st)
                P.act(junk[:], xt[:], AF.Square, accum=ss[:])
                rstd = col(st)
                rsqrt_col(rstd[:], ss[:], 1024.0, st)
                xn = xn_rot.next()
                P.act(xn[:], xt[:], AF.Copy, scale=rstd[:, 0:1])
                pt = psT.next()
                for k in range(8):
                    P.tr(pt[:, k * 128:(k + 1) * 128], xn[:, k * 128:(k + 1) * 128], ident_b[:, :])
                for k in range(8):
                    o = hxT[:, k, i * 128:(i + 1) * 128]
                    wt = ["%s:t%d:%d" % (hxT.name, i, k % 2)]
                    if i % 2 == 0:
                        P.act(o, pt[:, k * 128:(k + 1) * 128], AF.Identity, scale=gs1[:, k, v:v + 1], bias=sh1[:, k, v:v + 1], wt=wt)
                    else:
                        P.ts('dve', o, pt[:, k * 128:(k + 1) * 128], gs1[:, k, v:v + 1], sh1[:, k, v:v + 1], op0=ALU.mult, op1=ALU.add, wt=wt)

        def inproj(hxT, TB, m):
            ps = psA.next()
            rt = ["%s:t%d:%d" % (hxT.name, i, p_) for i in range(TB // 128) for p_ in range(2)]
            for k in range(8):
                P.mm(ps[:, 0:TB], Win[:, k, m * 128:(m + 1) * 128], hxT[:, k, 0:TB], start=(k == 0), stop=(k == 7), rt=rt)
            return ps

        def conv_taps(u, p, w_cols, b_col, TB, RW, left):
            u3 = u.rearrange("p (r w) -> p r w", w=RW)
            p3 = p.rearrange("p (r w) -> p r w", w=RW)
            if b_col is None:
                P.act(u, p, AF.Copy, scale=w_cols[left])
            else:
                P.act(u, p, AF.Identity, scale=w_cols[left], bias=b_col)
            for kx in range(len(w_cols)):
                off = kx - left
                if off == 0:
                    continue
                if off < 0:
                    o, i_ = u3[:, :, -off:RW], p3[:, :, 0:RW + off]
                else:
                    o, i_ = u3[:, :, 0:RW - off], p3[:, :, off:RW]
                P.stt(o, i_, w_cols[kx], o, ALU.mult, ALU.add)

        def lru_dir(st, d, u, ub, TB, j, init, h_out, reverse):
            psr = psA.next()
            P.mm(psr[:, 0:TB], gates_b[:, d, 0, j, :], ub, start=True, stop=True)
            psi = psA.next()
            P.mm(psi[:, 0:TB], gates_b[:, d, 1, j, :], ub, start=True, stop=True)
            a = lt_rot.next()
            P.act(a[:, 0:TB], psr[:, 0:TB], AF.Sigmoid, bias=CL[:, C_BR + d * 4 + j:C_BR + d * 4 + j + 1])
            ig = lt_rot.next()
            P.act(ig[:, 0:TB], psi[:, 0:TB], AF.Sigmoid, bias=CL[:, C_BI + d * 4 + j:C_BI + d * 4 + j + 1])
            P.act(a[:, 0:TB], a[:, 0:TB], AF.Exp, scale=nsp[:, d * 4 + j:d * 4 + j + 1])
            sq = lt_rot.next()
            P.act(sq[:, 0:TB], a[:, 0:TB], AF.Square)
            P.act(sq[:, 0:TB], sq[:, 0:TB], AF.Ln, scale=-1.0, bias=1.0)
            P.act(sq[:, 0:TB], sq[:, 0:TB], AF.Exp, scale=0.5)
            P.tt('dve', ig[:, 0:TB], ig[:, 0:TB], sq[:, 0:TB], ALU.mult)
            P.tt('dve', ig[:, 0:TB], ig[:, 0:TB], u, ALU.mult)
            if reverse:
                P.scan(h_out[:, ::-1], a[:, 0:TB][:, ::-1], ig[:, 0:TB][:, ::-1], init)
            else:
                P.scan(h_out, a[:, 0:TB], ig[:, 0:TB], init)

        def lru_front(st, hxT, TB, RW, j):
            ps = inproj(hxT, TB, j)
            p = lt_rot.next()
            P.cp('act', p[:, 0:TB], ps[:, 0:TB])
            u = lt_rot.next()
            conv_taps(u[:, 0:TB], p[:, 0:TB], [CL[:, C_CW + kx * 4 + j:C_CW + kx * 4 + j + 1] for kx in range(4)],
                      CL[:, C_CB + j:C_CB + j + 1], TB, RW, 1)
            ub = ub_rot.next()
            P.cp('act', ub[:, 0:TB], u[:, 0:TB])
            return u, ub

        def group_norm(st, ys, TB, nch):
            ps = psA.next()
            for i, y in enumerate(ys):
                sq = sqb_rot.next()
                P.act(sq[:, 0:TB], y, AF.Square)
                P.mm(ps[:, 0:TB], ones_b[:, :], sq[:, 0:TB], start=(i == 0), stop=(i == len(ys) - 1))
            r = lt_rot.next()
            P.act(r[:, 0:TB], ps[:, 0:TB], AF.Ln, scale=1.0 / nch, bias=EPS)
            P.act(r[:, 0:TB], r[:, 0:TB], AF.Exp, scale=-0.5)
            return r

        for (v, src, T, TB, RW, off, full) in ((1, cin, T_C, 256, 256, T_X, not last), (0, xin, T_X, 512, 64, 0, True)):
            nblk = T // TB
            with ExitStack() as st:
                xt_rot = Rot([P.sb("xt%d" % i, [128, 1024], F32, st) for i in range(2)])
                junk_rot = Rot([P.sb("junk%d" % i, [128, 1024], BF16, st) for i in range(1)])
                xn_rot = Rot([P.sb("xn%d" % i, [128, 1024], BF16, st) for i in range(1)])
                hx_rot = Rot([P.sb("hxT%d" % i, [128, 8, 512], BF16, st) for i in range(2)])
                lt_rot = Rot([P.sb("lt%d" % i, [128, TB], F32, st) for i in range(7)])
                ub_rot = Rot([P.sb("ub%d" % i, [128, 512], BF16, st) for i in range(1)])
                sqb_rot = Rot([P.sb("sqb%d" % i, [128, 512], BF16, st) for i in range(2)])
                hbb_rot = Rot([P.sb("hbb%d" % i, [128, 512], BF16, st) for i in range(1)])
                if full:
                    Wfft = prep_wfft(st)
                    for b in range(nblk):
                        hxT = hx_rot.next()
                        load_block(st, src, b * TB, TB, v, hxT)
                        P.ld(hxd[:, :, off + b * TB:off + (b + 1) * TB].rearrange("k p t -> p k t"), hxT[:, :, 0:TB],
                             wt=["hxd:%d" % b], rt=["%s:t%d:%d" % (hxT.name, i, p_) for i in range(TB // 128) for p_ in range(2)])
                        for i in range(TB // 128):
                            ps = psA.next()
                            for k in range(8):
                                P.mm(ps[:, :], hxT[:, k, i * 128:(i + 1) * 128], Wfft[:, k, :], start=(k == 0), stop=(k == 7),
                                     rt=["%s:t%d:%d" % (hxT.name, i, p_) for p_ in range(2)])
                            vt = sqb_rot.next()
                            P.cp('act', vt[:, :], ps[:, :])
                            r_ = slice(off + b * TB + i * 128, off + b * TB + (i + 1) * 128)
                            for q in range(2):
                                P.ld(Vd[q, r_, :].rearrange("t (part c) -> t part c", part=2),
                                     vt[:, :].rearrange("t (part qc) -> t part qc", part=2)[:, :, q * 128:(q + 1) * 128],
                                     wt=["Vd:%d:%d:%d" % (b, i, q)])
                m0 = P.mark()
                if full:
                    psA = psA_lo
                P.ms('dve', st_b[:], 0.0) if v == 1 else None
                if v == 1:
                    P.ms('dve', st_f[:], 0.0)
                def fetch_block(hxT, b):
                    wt = ["%s:t%d:%d" % (hxT.name, i, p_) for i in range(TB // 128) for p_ in range(2)]
                    P.dma('sp', lambda e, o_=hxT[:, :, 0:TB], i_=hxd[:, :, off + b * TB:off + (b + 1) * TB].rearrange("k p t -> p k t"):
                          e.dma_start(out=o_, in_=i_), ["hxd:%d" % b], wt, 2.0 + TB * 16 * 128 / 150000.0)
                for b in reversed(range(nblk)):
                    hxT = hx_rot.next()
                    if full:
                        fetch_block(hxT, b)
                    else:
                        load_block(st, src, b * TB, TB, v, hxT)
                    for j in range(4):
                        u, ub = lru_front(st, hxT, TB, RW, j)
                        h = lt_rot.next()
                        lru_dir(st, 1, u[:, 0:TB], ub[:, 0:TB], TB, j, st_b[:, j:j + 1], h[:, 0:TB], True)
                        stn = col(st)
                        P.cp('dve', stn[:], h[:, 0:1])
                        P.cp('dve', st_b[:, j:j + 1], stn[:])
                        if full:
                            hb = hbb_rot.next()
                            P.cp('act', hb[:, 0:TB], h[:, 0:TB])
                            P.ld(hbd[j, :, off + b * TB:off + (b + 1) * TB], hb[:, 0:TB], wt=["hbd:%d:%d" % (b, j)])
                m1 = P.mark()
                if full:
                    psA = psA_hi
                    yraw = [P.sb("yraw%d" % q, [128, T], BF16, st) for q in range(2)]
                    if v == 1:
                        Vc = P.sb("Vc", [128, 2, 2, 256], BF16, st)
                        for q in range(2):
                            P.ld(Vc[:, q], Vd[q, off:off + T, :].rearrange("(i p) n -> p i n", p=128),
                                 rt=["Vd:%d:%d:%d" % (b_, i_, q) for b_ in range(nblk) for i_ in range(TB // 128)])
                        for q in range(2):
                            ps = psA.next()
                            first = True
                            for i in range(2):
                                for part in range(2):
                                    P.mm(ps[:, 0:256], Vc[:, q, i, part * 128:(part + 1) * 128], dc[:, i, part, :],
                                         start=first, stop=(i == 1 and part == 1))
                                    first = False
                            P.cp('act', yraw[q][:, :], ps[:, 0:256])
                    else:
                        e3_rot = Rot([P.sb("e3t%d" % i, [128, 8, 2, 64], BF16, st) for i in range(1)])
                        Vq = P.sb("Vq", [128, 64, 2, 128], BF16, st)
                        Bq = P.sb("Bq", [128, 64, 256], BF16, st)
                        for q in range(2):
                            P.ld(Vq[:].rearrange("p n part c -> p (n part c)"),
                                 Vd[q, 0:T_X, :].rearrange("(n1 n2) pc -> n1 (n2 pc)", n2=64),
                                 rt=["Vd:%d:%d:%d" % (b_, i_, q) for b_ in range(nblk) for i_ in range(TB // 128)])
                            for cp2 in range(32):
                                ps = psA.next()
                                for c_ in range(2):
                                    cp_ = cp2 * 2 + c_
                                    for c2 in range(2):
                                        for part in range(2):
                                            lhsT = Vq[:, :, part, cp_ + 64 * c2]
                                            P.mm(ps[c2 * 64:(c2 + 1) * 64, c_ * 256:(c_ + 1) * 256], lhsT, cs1[:, part, :],
                                                 start=(part == 0), stop=(part == 1))
                                P.cp('act' if cp2 % 2 == 0 else 'dve', Bq[:, cp2 * 2:cp2 * 2 + 2, :].rearrange("p a b -> p (a b)"), ps[:, :])
                            for kb in range(16):
                                e3t = e3_rot.next()
                                P.ld(e3t[:], K['k_e3'][:, kb * 8:(kb + 1) * 8, :, :])
                                ps = psA.next()
                                for k1l in range(8):
                                    k1 = kb * 8 + k1l
                                    for c2 in range(2):
                                        pr = slice(c2 * 64, (c2 + 1) * 64)
                                        for part in range(2):
                                            P.mm(ps[pr, k1l * 64:(k1l + 1) * 64], Bq[pr, :, part * 128 + k1], e3t[pr, k1l, part, :],
                                                 start=(part == 0), stop=(part == 1))
                                o = yraw[q][:, :].rearrange("p (k2 k1) -> p k1 k2", k1=128)[:, kb * 8:(kb + 1) * 8, :]
                                P.cp('act' if kb % 2 == 0 else 'dve', o, ps[:, :].rearrange("p (a b) -> p a b", a=8))
                    NB = 512 if v == 0 else 256
                    sqf_rot = Rot([P.sb("sqf%d" % i, [128, 512], BF16, st) for i in range(2)])
                    rf_rot = Rot([P.sb("rf%d" % i, [128, 512], F32, st) for i in range(1)])
                    for b in range(T // NB):
                        ps = psA.next()
                        for q in range(2):
                            sq = sqf_rot.next()
                            P.act(sq[:, 0:NB], yraw[q][:, b * NB:(b + 1) * NB], AF.Square)
                            P.mm(ps[:, 0:NB], ones_b[:, :], sq[:, 0:NB], start=(q == 0), stop=(q == 1))
                        r = rf_rot.next()
                        P.act(r[:, 0:NB], ps[:, 0:NB], AF.Ln, scale=1.0 / 256, bias=EPS)
                        P.act(r[:, 0:NB], r[:, 0:NB], AF.Exp, scale=-0.5)
                        for q in range(2):
                            yo = sqf_rot.next()
                            P.tt('dve', yo[:, 0:NB], yraw[q][:, b * NB:(b + 1) * NB], r[:, 0:NB], ALU.mult)
                            P.ld(yfd[q, :, off + b * NB:off + (b + 1) * NB], yo[:, 0:NB], wt=["yfd:%d:%d" % (b, q)])
                    psA = psA_all
                    if INTERLEAVE:
                        P.interleave(m0, m1, P.mark())
                P.barrier()
                P.emit()
            if not full:
                with ExitStack() as st:
                    xt_rot = Rot([P.sb("xt%d" % i, [128, 1024], F32, st) for i in range(2)])
                    junk_rot = Rot([P.sb("junk%d" % i, [128, 1024], BF16, st) for i in range(1)])
                    xn_rot = Rot([P.sb("xn%d" % i, [128, 1024], BF16, st) for i in range(2)])
                    hx_rot = Rot([P.sb("hxT%d" % i, [128, 8, 512], BF16, st) for i in range(1)])
                    lt_rot = Rot([P.sb("lt%d" % i, [128, 512], F32, st) for i in range(10)])
                    ub_rot = Rot([P.sb("ub%d" % i, [128, 512], BF16, st) for i in range(2)])
                    hxT = hx_rot.next()
                    load_block(st, src, 0, TB, v, hxT)
                    for j in range(4):
                        u, ub = lru_front(st, hxT, TB, RW, j)
                        h = lt_rot.next()
                        lru_dir(st, 0, u[:, 0:TB], ub[:, 0:TB], TB, j, 0.0, h[:, 0:TB], False)
                        P.cp('dve', st_f[:, j:j + 1], h[:, TB - 1:TB])
                    P.barrier()
                    P.emit()
                continue
            with ExitStack() as st:
                xt_rot = Rot([P.sb("xt%d" % i, [128, 1024], F32, st) for i in range(2)])
                junk_rot = Rot([P.sb("junk%d" % i, [128, 1024], BF16, st) for i in range(1)])
                hx_rot = Rot([P.sb("hxT%d" % i, [128, 8, 512], BF16, st) for i in range(2)])
                lt_rot = MultiRot([Rot([P.sb("lt%d_%d" % (pp, i), [128, TB], F32, st) for i in range(LT_DEPTH)]) for pp in range(LT_POOLS)])
                Wout = prep_wout(st)
                for n_ in ('gate1', 'sh2', 'gs2'):
                    bc_load(n_, v, st)
                ylru_tt = [P.sb("ylru_t%d" % i, [128, 4, 512], F32, st) for i in range(YL_BUFS)]
                ysc_tt = [P.sb("ysc_t%d" % i, [128, 2, 512], F32, st) for i in range(YL_BUFS)]
                ub_rot = Rot([P.sb("ub%d" % i, [128, 512], BF16, st) for i in range(2)])
                sqb_rot = Rot([P.sb("sqb%d" % i, [128, 512], BF16, st) for i in range(2)])
                hbb_rot = Rot([P.sb("hbb%d" % i, [128, 512], BF16, st) for i in range(2)])
                ynb_rot = Rot([P.sb("ynb%d" % i, [128, 8, 512], BF16, st) for i in range(2)])
                x1_rot = Rot([P.sb("x1t%d" % i, [128, 1024], F32, st) for i in range(2)])
                hx2_rot = Rot([P.sb("hx2e%d" % i, [128, 1056], BF16, st) for i in range(2)])
                hx2T_rot = Rot([P.sb("hx2T%d" % i, [128, 1024], BF16, st) for i in range(2)])
                sm_rot = Rot([P.sb("sm%d" % i, [128, 16], F32, st) for i in range(6)])
                afs_rot = Rot([P.sb("afs%d" % i, [16, 128], F32, st) for i in range(2)])
                for b in range(nblk):
                    tok0 = b * TB
                    hxT = hx_rot.next()
                    fetch_block(hxT, b)
                    ynb = ynb_rot.next()
                    ylru_t, ysc_t = ylru_tt[b % YL_BUFS], ysc_tt[b % YL_BUFS]
                    ylru = []
                    for j in range(4):
                        lt_rot.set(b if LT_BY_BLOCK else j)
                        u, ub = lru_front(st, hxT, TB, RW, j)
                        h = lt_rot.next()
                        init = st_f[:, j:j + 1]
                        lru_dir(st, 0, u[:, 0:TB], ub[:, 0:TB], TB, j, init, h[:, 0:TB], False)
                        stn = col(st)
                        P.cp('dve', stn[:], h[:, TB - 1:TB])
                        P.cp('dve', st_f[:, j:j + 1], stn[:])
                        hb = hbb_rot.next()
                        P.ld(hb[:, 0:TB], hbd[j, :, off + tok0:off + tok0 + TB])
                        P.tt('dve', h[:, 0:TB], h[:, 0:TB], hb[:, 0:TB], ALU.add)
                        psg = inproj(hxT, TB, 4 + j)
                        gg = lt_rot.next()
                        P.act(gg[:, 0:TB], psg[:, 0:TB], AF.Gelu_apprx_tanh)
                        P.tt('dve', ylru_t[:, j, 0:TB], gg[:, 0:TB], h[:, 0:TB], ALU.mult)
                        ylru.append(ylru_t[:, j, 0:TB])
                    r = group_norm(st, ylru, TB, 512.0)
                    for j in range(4):
                        P.tt('dve', ynb[:, j, 0:TB], ylru[j], r[:, 0:TB], ALU.mult)
                    ysc = []
                    for j in range(2):
                        lt_rot.set(b if LT_BY_BLOCK else j)
                        psc = inproj(hxT, TB, 10 + j)
                        cc_ = lt_rot.next()
                        P.cp('act', cc_[:, 0:TB], psc[:, 0:TB])
                        psx = inproj(hxT, TB, 12 + j)
                        vv = lt_rot.next()
                        P.tt('dve', vv[:, 0:TB], psx[:, 0:TB], cc_[:, 0:TB], ALU.mult)
                        cv = lt_rot.next()
                        conv_taps(cv[:, 0:TB], vv[:, 0:TB], [CL[:, C_SCW + kx * 2 + j:C_SCW + kx * 2 + j + 1] for kx in range(3)],
                                  None, TB, RW, 1)
                        psb = inproj(hxT, TB, 8 + j)
                        P.tt('dve', ysc_t[:, j, 0:TB], psb[:, 0:TB], cv[:, 0:TB], ALU.mult)
                        ysc.append(ysc_t[:, j, 0:TB])
                    r = group_norm(st, ysc, TB, 256.0)
                    for j in range(2):
                        P.tt('dve', ynb[:, 4 + j, 0:TB], ysc[j], r[:, 0:TB], ALU.mult)
                    for q in range(2):
                        P.ld(ynb[:, 6 + q, 0:TB], yfd[q, :, off + tok0:off + tok0 + TB])
                    for i in range(TB // 128):
                        r0 = tok0 + i * 128
                        xt = xt_rot.next()
                        P.ld(xt[:], src[r0:r0 + 128, :])
                        x1 = x1_rot.next()
                        for hf in range(2):
                            ps = psA.next()
                            for kc in range(8):
                                P.mm(ps[:, :], ynb[:, kc, i * 128:(i + 1) * 128], Wout[:, kc, hf * 512:(hf + 1) * 512],
                                     start=(kc == 0), stop=(kc == 7))
                            P.tt('dve', x1[:, hf * 512:(hf + 1) * 512], ps[:, :], BC[('gate1', v)][:, hf * 512:(hf + 1) * 512], ALU.mult)
                        P.tt('dve', x1[:], x1[:], xt[:], ALU.add)
                        P.ld(xo[off + r0:off + r0 + 128, :], x1[:], wt=["xo:%d" % r0])
                        junk = junk_rot.next()
                        ss = col(st)
                        P.act(junk[:], x1[:], AF.Square, accum=ss[:])
                        rstd = col(st)
                        rsqrt_col(rstd[:], ss[:], 1024.0, st)
                        P.stt(xt[:], x1[:], rstd[:, 0:1], BC[('gs2', v)][:], ALU.mult, ALU.mult)
                        hx2e = hx2_rot.next()
                        P.tt('dve', hx2e[:, 0:1024], xt[:], BC[('sh2', v)][:], ALU.add)
                        pt = psT.next()
                        for k in range(8):
                            P.tr(pt[:, k * 128:(k + 1) * 128], hx2e[:, k * 128:(k + 1) * 128], ident_b[:, :])
                        hx2T = hx2T_rot.next()
                        P.cp('act', hx2T[:], pt[:])
                        pss = psS.next()
                        for k in range(8):
                            P.mm(pss[:, 0:16], hx2T[:, k * 128:(k + 1) * 128], wr_b[:, k, :], start=(k == 0), stop=(k == 7))
                        mx = sm_rot.next()
                        P.red(mx[:, 0:1], pss[:, 0:16], ALU.max)
                        P.ts('dve', mx[:, 1:2], mx[:, 0:1], -1.0)
                        ex = sm_rot.next()
                        P.act(ex[:, :], pss[:, 0:16], AF.Exp, bias=mx[:, 1:2], accum=mx[:, 2:3])
                        P.recip(mx[:, 3:4], mx[:, 2:3])
                        aff = sm_rot.next()
                        P.ts('dve', aff[:, :], ex[:, :], mx[:, 3:4])
                        P.cp('dve', hx2e[:, 1024:1040], aff[:, :])
                        P.tt('dve', ex[:, :], aff[:, :], hx2e[:, 1024:1040], ALU.subtract)
                        P.cp('dve', hx2e[:, 1040:1056], ex[:, :])
                        pss2 = psS.next()
                        P.tr(pss2[0:16, 0:128], aff[:, :], ident_f[:, :])
                        afs = afs_rot.next()
                        P.cp('act', afs[0:16, :], pss2[0:16, 0:128])
                        P.ld(affTd[:, off + r0:off + r0 + 128], afs[0:16, :], wt=["affTd:%d" % r0])
                        P.ld(hx2ext[off + r0:off + r0 + 128, :], hx2e[:, :], wt=["hx2ext:%d" % r0])
                P.barrier()
                P.emit()
        lst.close()
        if stop_after == ('mixer', l):
            if 'x1' in dbg_out:
                P.ld(dbg_out['x1'], xo)
            break

        with ExitStack() as st:
            onesrow = P.sb("onesrow", [64, 128], F32, st)
            P.ms('dve', onesrow[:], 1.0)
            iota_c = P.sb("iota_c", [64, 1024], F32, st)
            P.ld(iota_c[:], K['k_iota_c'][0:64, :])
            sets = [(0, 64, 1024, 0)] + ([(1, 2, 32, T_X)] if not last else [])
            for (v, NT, cap, off) in sets:
                A = P.sb("A", [NT, 16, 128], F32, st)
                P.ld(A[:], affTd[:, off:off + NT * 128].rearrange("e (i t) -> i e t", t=128))
                lo = P.sb("lo", [NT, 16], F32, st)
                hi = P.sb("hi", [NT, 16], F32, st)
                mid = P.sb("mid", [NT, 16], F32, st)
                cmp_ = P.sb("cmp", [NT, 16, 128], F32, st)
                cnt = P.sb("cnt", [NT, 16], F32, st)
                ge = P.sb("ge", [NT, 16], F32, st)
                dd = P.sb("dd", [NT, 16], F32, st)
                if NT == 64:
                    NP, TW = 128, 64
                    Ab = P.sb("Ab", [128, 16, 64], F32, st)
                    src2 = affTd[:, off:off + NT * 128].rearrange("e (i h t) -> h i e t", h=2, t=64)
                    for h_ in range(2):
                        P.ld(Ab[h_ * 64:(h_ + 1) * 64, :, :], src2[h_])
                    lo = P.sb("lo2", [128, 16], F32, st)
                    mid = P.sb("mid2", [128, 16], F32, st)
                    cmpb = P.sb("cmp2", [128, 16, 64], F32, st)
                    cnt2 = P.sb("cnt2", [128, 16], F32, st)
                    ge = P.sb("ge2", [128, 16], F32, st)
                else:
                    NP, TW, Ab, cmpb, cnt2 = NT, 128, A, cmp_, cnt
                P.ms('dve', lo[:], 0.0)
                for it in range(30):
                    c_it = 0.5 ** (it + 1)
                    P.ts('dve', mid[:], lo[:], c_it, op0=ALU.add)
                    P.tt('dve', cmpb[:], Ab[:], mid[:].unsqueeze(2).to_broadcast([NP, 16, TW]), ALU.is_ge)
                    P.red(cnt2[:], cmpb[:], ALU.add)
                    pss = psS.next()
                    P.mm(pss[0:NP, 0:16], ones_f[0:NP, 0:NP], cnt2[:], start=True, stop=True)
                    P.ts('dve', ge[:], pss[0:NP, 0:16], float(cap) - 0.5, c_it, op0=ALU.is_ge, op1=ALU.mult)
                    P.tt('dve', lo[:], lo[:], ge[:], ALU.add)
                P.tt('dve', cmp_[:], A[:], lo[0:NT, :].unsqueeze(2).to_broadcast([NT, 16, 128]), ALU.is_ge)
                RT = P.sb("RT", [NT, 16, 132], F32, st)
                for e in range(16):
                    P.scan(RT[:, e, 0:128], onesrow[0:NT, :], cmp_[:, e, :], 0.0)
                P.cp('dve', cnt[:], RT[:, :, 127])
                pss = psS.next()
                P.mm(pss[0:NT, 0:16], triu[0:NT, 0:NT], cnt[:], start=True, stop=True)
                base = P.sb("base", [NT, 16], F32, st)
                incl = P.sb("incl", [NT, 16], F32, st)
                P.cp('dve', base[:], pss[0:NT, 0:16])
                P.tt('dve', incl[:], base[:], cnt[:], ALU.add)
                P.cp('dve', RT[:, :, 128], base[:])
                tso = P.sb("tso", [NT, 1], F32, st)
                P.ts('dve', tso[:], tstart[0:NT, :], float(off), op0=ALU.add)
                P.cp('dve', RT[:, :, 129], tso[:, 0:1].to_broadcast([NT, 16]))
                P.ms('dve', RT[:, :, 130:132], 1.0)
                oh1 = P.sb("oh1", [NT, 1024], F32, st)
                oh = P.sb("oh", [NT, 1024], F32, st)
                nslot = (cap + 127) // 128
                for e in range(16):
                    P.ts('dve', oh1[:, 0:cap], iota_c[0:NT, 0:cap], base[:, e:e + 1], op0=ALU.is_ge)
                    P.stt(oh[:, 0:cap], iota_c[0:NT, 0:cap], incl[:, e:e + 1], oh1[:, 0:cap], ALU.is_lt, ALU.mult)
                    for j in range(nslot):
                        M = min(128, cap - j * 128)
                        slot = j if v == 0 else 8
                        pss = psS.next()
                        P.mm(pss[0:M, 0:132], oh[:, j * 128:j * 128 + M], RT[:, e, :], start=True, stop=True)
                        thr = P.sb("thr", [128, 4], F32, st)
                        P.ts('dve', thr[0:M, 0:1], pss[0:M, 128:129], -1.0, cidx[0:M, j:j + 1], op0=ALU.mult, op1=ALU.add)
                        jk = P.sb("jk", [128, 128], F32, st)
                        P.ts('dve', jk[0:M, :], pss[0:M, 0:128], thr[0:M, 0:1], pss[0:M, 129:130], op0=ALU.is_le, op1=ALU.add,
                             accum=thr[0:M, 2:3])
                        P.cp('dve', idx_all[0:M, e, slot:slot + 1], thr[0:M, 2:3])
                        P.cp('dve', val_all[0:M, e, slot:slot + 1], pss[0:M, 130:131])
            if 'idx' in dbg_out:
                idf = P.sb("idf", [128, 16 * 9], F32, st)
                P.cp('dve', idf[:], idx_all[:].rearrange("p e s -> p (e s)"))
                dump('idx', idf[:])
            P.barrier()
            P.emit()
        if stop_after == ('select', l):
            break

        with ExitStack() as st:
            Wg = P.sb("Wg", [128, 8, 1024], BF16, st)
            Wu = P.sb("Wu", [128, 8, 1024], BF16, st)
            Wd = P.sb("Wd", [128, 8, 1024], BF16, st)
            stg = Rot([P.sb("stg%d" % i, [128, 4096], F32, st) for i in range(MOE_STG)])
            xs_rot = Rot([P.sb("xs%d" % i, [128, 1056], BF16, st) for i in range(6)])
            xsT_rot = Rot([P.sb("xsT%d" % i, [128, 8, 1056], BF16, st) for i in range(2)])
            hid_rot = Rot([P.sb("hid%d" % i, [128, 8, 1056], BF16, st) for i in range(1)])
            gv_all = P.sb("gv_all", [128, 16, 9], F32, st)
            bc_load('gate2', 0, st)
            if not last:
                bc_load('gate2', 1, st, rows=32)
            sg_rot = Rot([P.sb("sg%d" % i, [128, 512], F32, st) for i in range(2)])
            ys_rot = Rot([P.sb("ys%d" % i, [128, 1024], F32, st) for i in range(3)])
            nsl = 8 if last else 9
            for e in range(16):
                for (Wt, nm_) in ((Wg, 'w_gate_e'), (Wu, 'w_up_e'), (Wd, 'w_down_e')):
                    for hh in range(2):
                        s = stg.next()
                        sv = s[:].rearrange("p (k n) -> p k n", k=4)
                        P.ld(sv, D[nm_][l, e][hh * 512:(hh + 1) * 512, :].rearrange("(k p) n -> p k n", p=128))
                        for k4 in range(4):
                            P.cp('act' if k4 % 2 == 0 else 'dve', Wt[:, hh * 4 + k4, :], sv[:, k4, :])
                xsT = xsT_rot.next()
                hid = hid_rot.next()
                for s_ in range(nsl):
                    M = 128 if s_ < 8 else 32
                    c0 = s_ * 128
                    xs = xs_rot.next()
                    P.gather(xs[0:M, :], hx2ext, idx_all[0:M, e, s_:s_ + 1])
                    P.tt('dve', gv_all[0:M, e, s_:s_ + 1], xs[0:M, 1024 + e:1025 + e], xs[0:M, 1040 + e:1041 + e], ALU.add)
                    P.tt('dve', gv_all[0:M, e, s_:s_ + 1], gv_all[0:M, e, s_:s_ + 1], val_all[0:M, e, s_:s_ + 1], ALU.mult)
                    pt = psT.next()
                    for k in range(8):
                        P.tr(pt[:, k * 128:k * 128 + M], xs[0:M, k * 128:(k + 1) * 128], ident_b[0:M, 0:M])
                    P.cp('act' if s_ % 2 == 0 else 'dve', xsT[:, :, c0:c0 + M], pt[:, :].rearrange("p (k m) -> p k m", k=8)[:, :, 0:M])
                groups = [(0, 512), (512, 512)] + ([(1024, 32)] if not last else [])
                for (c0, N) in groups:
                    for f in range(8):
                        psg = psA.next()
                        for k in range(8):
                            P.mm(psg[:, 0:N], Wg[:, k, f * 128:(f + 1) * 128], xsT[:, k, c0:c0 + N], start=(k == 0), stop=(k == 7))
                        psu = psA.next()
                        for k in range(8):
                            P.mm(psu[:, 0:N], Wu[:, k, f * 128:(f + 1) * 128], xsT[:, k, c0:c0 + N], start=(k == 0), stop=(k == 7))
                        sg = sg_rot.next()
                        P.act(sg[:, 0:N], psg[:, 0:N], AF.Silu)
                        P.tt('dve', hid[:, f, c0:c0 + N], psu[:, 0:N], sg[:, 0:N], ALU.mult)
                for s_ in range(nsl):
                    M = 128 if s_ < 8 else 32
                    c0 = s_ * 128
                    v = 0 if s_ < 8 else 1
                    ys = ys_rot.next()
                    for hf in range(2):
                        ps = psA.next()
                        for f in range(8):
                            P.mm(ps[0:M, :], hid[:, f, c0:c0 + M], Wd[:, f, hf * 512:(hf + 1) * 512], start=(f == 0), stop=(f == 7))
                        P.stt(ys[0:M, hf * 512:(hf + 1) * 512], ps[0:M, :], gv_all[0:M, e, s_:s_ + 1],
                              BC[('gate2', v)][0:M, hf * 512:(hf + 1) * 512], ALU.mult, ALU.mult)
                    P.scatter_add(xo, idx_all[0:M, e, s_:s_ + 1], ys[0:M, :],
                                  rt=["sc:%d:%d" % (e - 1, q_) for q_ in range(nsl)] if e > 0 else [],
                                  wt=["sc:%d:%d" % (e, s_)])
            P.barrier()
            P.emit()
        if stop_after == ('moe', l):
            if 'x1' in dbg_out:
                P.ld(dbg_out['x1'], xo)
            break

    if stop_after is None:
        with ExitStack() as st:
            xt_rot = Rot([P.sb("xt%d" % i, [128, 1024], F32, st) for i in range(3)])
            junk_rot = Rot([P.sb("junk%d" % i, [128, 1024], BF16, st) for i in range(2)])
            o_rot = Rot([P.sb("ot%d" % i, [128, 1024], F32, st) for i in range(3)])
            xf = xres[(depth - 1) % 2]
            gfin = P.sb("gfin", [128, 1024], F32, st)
            gfr = P.sb("gfr", [2, 1024], F32, st)
            P.ld(gfr[0:1, :], D['g_final'])
            P.ld(gfr[1:2, :], D['g_final'])
            for h in range(2):
                ps = psA.next()
                P.mm(ps[:, :], sel[0:2, 0:128], gfr[0:2, h * 512:(h + 1) * 512])
                P.cp('act', gfin[:, h * 512:(h + 1) * 512], ps[:, :])
            for i in range(T_X // 128):
                xt = xt_rot.next()
                P.ld(xt[:], xf[i * 128:(i + 1) * 128, :])
                junk = junk_rot.next()
                ss = col(st)
                P.act(junk[:], xt[:], AF.Square, accum=ss[:])
                rstd = col(st)
                rsqrt_col(rstd[:], ss[:], 1024.0, st)
                ot = o_rot.next()
                P.stt(ot[:], xt[:], rstd[:, 0:1], gfin[:], ALU.mult, ALU.mult)
                P.ld(out_d[i * 128:(i + 1) * 128, :], ot[:], wt=["out:%d" % i])
            P.barrier()
            P.emit()
    else:
        P.barrier()
        P.emit()
    P.stack.close()
    return nc


_CONSTS = None


def make_in_maps(inputs, n_cores=8):
    global _CONSTS
    if _CONSTS is None:
        _CONSTS = host_consts()
    maps = []
    f = lambda a: np.ascontiguousarray(np.asarray(a, dtype=np.float32))
    shared = {n: f(inputs[n]) for n in IN_SHAPES if n not in ('x', 'c', 'ctx', 'c_ctx', 'g_final')}
    shared['c_ctx'] = f(inputs['c_ctx']).reshape(1, 1024)
    shared['g_final'] = f(inputs['g_final']).reshape(1, 1024)
    shared.update(_CONSTS)
    for core in range(n_cores):
        b = core % 4
        m = dict(shared)
        m['x'] = f(inputs['x'][b])
        m['c'] = f(inputs['c'][b]).reshape(1, 1024)
        m['ctx'] = f(inputs['ctx'][b])
        maps.append(m)
    return maps


def kernel(**inputs):
    nc = build()
    maps = make_in_maps(inputs)
    res = run_bass_kernel_spmd(nc, maps, core_ids=list(range(8)))
    out = np.stack([np.asarray(res.results[b]["out"], dtype=np.float32) for b in range(4)], axis=0)
    return out
```
